# Optimizing a Trainium2 kernel written in Bass

```python
import jax
import jax.numpy as jnp
from jax import lax
import numpy as np

D_MODEL = 1024
BATCH = 4
SEQ = 4096
DEPTH = 2

N_EVEN = (DEPTH + 1) // 2
N_ODD = DEPTH // 2
NORM_EPS = 1e-6

GLA_HEADS = 4
GLA_DK = 64
GLA_DV = 128
GLA_LOWRANK = 16
GLA_GATE_NORM = 16.0
GLA_CHUNK = 16
GLA_WIDTH = GLA_HEADS * GLA_DV
GLA_SPLITS = (GLA_HEADS * GLA_DK, GLA_HEADS * GLA_DK, GLA_WIDTH, GLA_WIDTH, GLA_LOWRANK)
GLA_COLS = 2 * GLA_HEADS * GLA_DK + 2 * GLA_WIDTH + GLA_LOWRANK

RWKV_HEADS = 8
RWKV_N = 64
RWKV_W_LORA = 64
RWKV_A_LORA = 64
RWKV_G_LORA = 128
RWKV_GN_EPS = 64e-5
RWKV_WIDTH = RWKV_HEADS * RWKV_N
RWKV_SPLITS = (RWKV_WIDTH, RWKV_W_LORA, RWKV_WIDTH, RWKV_WIDTH, RWKV_A_LORA, RWKV_G_LORA)
RWKV_COLS = 3 * RWKV_WIDTH + RWKV_W_LORA + RWKV_A_LORA + RWKV_G_LORA
EVEN_COLS = GLA_COLS + RWKV_COLS
EVEN_MIX_WIDTH = GLA_WIDTH + RWKV_WIDTH

NSA_HEADS = 16
NSA_GROUPS = 4
NSA_REP = NSA_HEADS // NSA_GROUPS
NSA_DH = 64
NSA_WIDTH = NSA_HEADS * NSA_DH
NSA_KV = NSA_GROUPS * NSA_DH
NSA_CMP_LEN = 32
NSA_CMP_STRIDE = 16
NSA_CMP_HIDDEN = 64
NSA_SEL_LEN = 64
NSA_N_SEL = 8
NSA_N_LOCAL = 2
NSA_WINDOW = 512
NSA_Q_BLOCK = 128
NSA_FORCE = 100.0
ODD_SPLITS = (NSA_WIDTH, NSA_KV, NSA_KV, NSA_KV, NSA_KV, NSA_KV, NSA_KV, 3 * NSA_HEADS)
ODD_COLS = NSA_WIDTH + 6 * NSA_KV + 3 * NSA_HEADS

FFN_HIDDEN = 2816
CONV_WIDTH = 3

kernel_name = 'hybrid_gla_rwkv7_nsa_convffn_adaln'


def _split(t, sizes):
    return jnp.split(t, np.cumsum(sizes)[:-1].tolist(), axis=-1)


def _rmsnorm(x, g, eps=NORM_EPS):
    x32 = x.astype(jnp.float32)
    y = x32 * lax.rsqrt(jnp.mean(x32 * x32, axis=-1, keepdims=True) + eps)
    return (y * g).astype(x.dtype)


def _masked_softmax(s, mask):
    s = jnp.where(mask, s, -jnp.inf)
    m = jnp.max(s, axis=-1, keepdims=True)
    m = jnp.where(jnp.isfinite(m), m, 0.0)
    e = jnp.exp(s - m)
    den = jnp.sum(e, axis=-1, keepdims=True)
    return e / jnp.where(den > 0, den, 1.0)


def _gla_chunked(q, k, v, log_a):
    B, T, H, DK = q.shape
    DV = v.shape[-1]
    C = GLA_CHUNK
    N = T // C

    def chunks(t):
        return t.astype(jnp.float32).reshape(B, N, C, H, t.shape[-1]).transpose(0, 3, 1, 2, 4)

    q, k, v, log_a = chunks(q * DK ** -0.5), chunks(k), chunks(v), chunks(log_a)
    b = jnp.cumsum(log_a, axis=3)
    causal = jnp.tril(jnp.ones((C, C), dtype=bool))
    rel = jnp.where(causal[:, :, None], b[:, :, :, :, None, :] - b[:, :, :, None, :, :], -jnp.inf)
    scores = jnp.einsum('bhntk,bhnsk,bhntsk->bhnts', q, k, jnp.exp(rel))
    o_intra = jnp.einsum('bhnts,bhnsv->bhntv', scores, v)
    b_end = b[:, :, :, -1]
    upd = jnp.einsum('bhnsk,bhnsv->bhnkv', k * jnp.exp(b_end[:, :, :, None] - b), v)

    def step(state, inp):
        decay, u = inp
        return decay[..., None] * state + u, state

    s0 = jnp.zeros((B, H, DK, DV), jnp.float32)
    _, s_prev = lax.scan(step, s0, (jnp.moveaxis(jnp.exp(b_end), 2, 0), jnp.moveaxis(upd, 2, 0)))
    s_prev = jnp.moveaxis(s_prev, 0, 2)
    o_inter = jnp.einsum('bhntk,bhnkv->bhntv', q * jnp.exp(b), s_prev)
    return (o_intra + o_inter).transpose(0, 2, 3, 1, 4).reshape(B, T, H, DV)


def _rwkv7_scan(r, w, k, v, kk, a):
    B, T, H, N = r.shape

    def step(state, inp):
        r_t, w_t, k_t, v_t, kk_t, a_t = inp
        removed = jnp.einsum('bhvk,bhk->bhv', state, kk_t)
        state = (state * w_t[:, :, None, :]
                 - removed[..., None] * (kk_t * a_t)[:, :, None, :]
                 + v_t[..., None] * k_t[:, :, None, :])
        return state, jnp.einsum('bhvk,bhk->bhv', state, r_t)

    s0 = jnp.zeros((B, H, N, N), jnp.float32)
    _, y = lax.scan(step, s0, tuple(jnp.moveaxis(t, 1, 0) for t in (r, w, k, v, kk, a)))
    return jnp.moveaxis(y, 0, 1)


def _even_mixer(h, w_in, shift_mu, a_up, a_b, gla_g, w0, w2, a0, a2, g2, k_k, k_a, r_k,
                gn_w, gn_b, w_out):
    B, T, _ = h.shape
    hd = lambda t, d: t.reshape(B, T, -1, d)
    p = (h @ w_in).astype(jnp.float32)
    p_gla, p_rw = p[..., :GLA_COLS], p[..., GLA_COLS:]
    q, k, v, og, lr = _split(p_gla, GLA_SPLITS)
    log_a = jax.nn.log_sigmoid(lr @ a_up + a_b) / GLA_GATE_NORM
    o = _gla_chunked(hd(q, GLA_DK), hd(k, GLA_DK), hd(v, GLA_DV), hd(log_a, GLA_DK))
    o_gla = (_rmsnorm(o, gla_g) * jax.nn.silu(hd(og, GLA_DV))).reshape(B, T, GLA_WIDTH)
    p_prev = jnp.concatenate([jnp.zeros_like(p_rw[:, :1]), p_rw[:, :-1]], axis=1)
    p_rw = p_rw + (p_prev - p_rw) * shift_mu
    r, wl, k, v, al, gl = _split(p_rw, RWKV_SPLITS)
    w = jnp.exp(-jnp.exp(-jax.nn.softplus(-(w0 + jnp.tanh(wl) @ w2)) - 0.5))
    a = jax.nn.sigmoid(a0 + al @ a2)
    g = jax.nn.sigmoid(gl) @ g2
    kk = hd(k * k_k, RWKV_N)
    kk = kk * lax.rsqrt(jnp.maximum(jnp.sum(kk * kk, axis=-1, keepdims=True), 1e-24))
    k = k * (1.0 + (a - 1.0) * k_a)
    rh, kh, vh = hd(r, RWKV_N), hd(k, RWKV_N), hd(v, RWKV_N)
    y = _rwkv7_scan(rh, hd(w, RWKV_N), kh, vh, kk, hd(a, RWKV_N))
    mu = jnp.mean(y, axis=-1, keepdims=True)
    var = jnp.mean(jnp.square(y - mu), axis=-1, keepdims=True)
    y = ((y - mu) * lax.rsqrt(var + RWKV_GN_EPS)).reshape(B, T, RWKV_WIDTH) * gn_w + gn_b
    bonus = (jnp.sum(rh * kh * r_k, axis=-1, keepdims=True) * vh).reshape(B, T, RWKV_WIDTH)
    o_rw = (y + bonus) * g
    out = jnp.concatenate([o_gla, o_rw], axis=-1) @ w_out
    return out.astype(h.dtype)


def _compress(t, pe, w1, w2, cmp_start):
    B, T, G, DH = t.shape
    idx = cmp_start[:, None] + jnp.arange(NSA_CMP_LEN)[None, :]
    blocks = t[:, idx] + pe[:, None, :]
    flat = blocks.transpose(0, 1, 3, 2, 4).reshape(B, idx.shape[0], G, NSA_CMP_LEN * DH)
    return jax.nn.gelu(flat @ w1) @ w2


def _nsa_mixer(h, w_in, pe_k, w1_k, w2_k, pe_v, w1_v, w2_v, w_out):
    B, T, _ = h.shape
    G, R, DH = NSA_GROUPS, NSA_REP, NSA_DH
    f32 = jnp.float32
    p = h @ w_in
    q, kc, vc, ks, vs, kw, vw, gt = _split(p, ODD_SPLITS)
    q = q.reshape(B, T, G, R, DH) * DH ** -0.5
    kc, vc, ks, vs, kw, vw = (t.reshape(B, T, G, DH) for t in (kc, vc, ks, vs, kw, vw))
    gates = jax.nn.sigmoid(gt.astype(f32)).reshape(B, T, G, R, 3)
    slopes = (2.0 ** (-8.0 * jnp.arange(1, NSA_HEADS + 1, dtype=f32) / NSA_HEADS)).reshape(G, R)
    n_cmp = (T - NSA_CMP_LEN) // NSA_CMP_STRIDE + 1
    cmp_start = jnp.arange(n_cmp) * NSA_CMP_STRIDE
    cmp_end = cmp_start + NSA_CMP_LEN - 1
    kc = _compress(kc, pe_k, w1_k, w2_k, cmp_start)
    vc = _compress(vc, pe_v, w1_v, w2_v, cmp_start)
    n_sel = T // NSA_SEL_LEN
    k_sel = min(NSA_N_SEL, n_sel)
    sel_start = jnp.arange(n_sel) * NSA_SEL_LEN
    overlap = ((cmp_start[:, None] <= sel_start[None, :] + NSA_SEL_LEN - 1)
               & (cmp_end[:, None] >= sel_start[None, :])).astype(f32)
    ks_blk = ks.reshape(B, n_sel, NSA_SEL_LEN, G, DH).transpose(0, 3, 1, 2, 4)
    vs_blk = vs.reshape(B, n_sel, NSA_SEL_LEN, G, DH).transpose(0, 3, 1, 2, 4)
    pad = ((0, 0), (NSA_WINDOW, 0), (0, 0), (0, 0))
    kw_pad, vw_pad = jnp.pad(kw, pad), jnp.pad(vw, pad)
    b_idx = jnp.arange(B)[:, None, None, None]
    g_idx = jnp.arange(G)[None, :, None, None]
    QB = NSA_Q_BLOCK

    def query_block(qi):
        q0 = qi * QB
        qb = lax.dynamic_slice_in_dim(q, q0, QB, axis=1)
        t = q0 + jnp.arange(QB)
        dist_c = t[:, None] - cmp_end[None, :]
        s = jnp.einsum('bqgrd,bngd->bgrqn', qb, kc).astype(f32) - slopes[:, :, None, None] * dist_c
        p_c = _masked_softmax(s, dist_c >= 0)
        o_c = jnp.einsum('bgrqn,bngd->bqgrd', p_c.astype(vc.dtype), vc)
        imp = jnp.einsum('bgrqn,nj->bgqj', p_c, overlap)
        blk = jnp.arange(n_sel)
        ahead = (t // NSA_SEL_LEN)[:, None] - blk[None, :]
        valid = ahead >= 0
        forced = (blk[None, :] == 0) | (valid & (ahead < NSA_N_LOCAL))
        score = jnp.where(valid, imp + jnp.where(forced, NSA_FORCE, 0.0), -NSA_FORCE)
        _, sel = lax.top_k(score, k_sel)
        tok = (sel[..., None] * NSA_SEL_LEN + jnp.arange(NSA_SEL_LEN)).reshape(B, G, QB, k_sel * NSA_SEL_LEN)
        dist_s = t[:, None] - tok
        k_g = ks_blk[b_idx, g_idx, sel].reshape(B, G, QB, k_sel * NSA_SEL_LEN, DH)
        v_g = vs_blk[b_idx, g_idx, sel].reshape(B, G, QB, k_sel * NSA_SEL_LEN, DH)
        s = (jnp.einsum('bqgrd,bgqmd->bgrqm', qb, k_g).astype(f32)
             - slopes[:, :, None, None] * dist_s[:, :, None])
        p_s = _masked_softmax(s, dist_s[:, :, None] >= 0)
        o_s = jnp.einsum('bgrqm,bgqmd->bqgrd', p_s.astype(v_g.dtype), v_g)
        k_w = lax.dynamic_slice_in_dim(kw_pad, q0, QB + NSA_WINDOW, axis=1)
        v_w = lax.dynamic_slice_in_dim(vw_pad, q0, QB + NSA_WINDOW, axis=1)
        pos = q0 - NSA_WINDOW + jnp.arange(QB + NSA_WINDOW)
        dist_w = t[:, None] - pos[None, :]
        mask_w = (dist_w >= 0) & (dist_w < NSA_WINDOW) & (pos[None, :] >= 0)
        s = jnp.einsum('bqgrd,bkgd->bgrqk', qb, k_w).astype(f32) - slopes[:, :, None, None] * dist_w
        p_w = _masked_softmax(s, mask_w)
        o_w = jnp.einsum('bgrqk,bkgd->bqgrd', p_w.astype(v_w.dtype), v_w)
        gb = lax.dynamic_slice_in_dim(gates, q0, QB, axis=1)
        return gb[..., 0:1] * o_c + gb[..., 1:2] * o_s + gb[..., 2:3] * o_w

    out = lax.map(query_block, jnp.arange(T // QB))
    out = jnp.moveaxis(out, 0, 1).reshape(B, T, NSA_WIDTH)
    return (out @ w_out).astype(h.dtype)


def _conv_ffn(h, w_up, conv_w, conv_b, w_down):
    u, v = jnp.split(h @ w_up, 2, axis=-1)
    u = lax.conv_general_dilated(u, conv_w[:, None, :].astype(u.dtype), window_strides=(1,),
                                 padding=[(CONV_WIDTH - 1, 0)],
                                 dimension_numbers=('NWC', 'WIO', 'NWC'),
                                 feature_group_count=FFN_HIDDEN) + conv_b
    return ((jax.nn.gelu(u) * v) @ w_down).astype(h.dtype)


def setup_inputs(seed: int = 0) -> dict:
    key = jax.random.key(seed)
    keys = iter(jax.random.split(key, 40))
    nrm = lambda shape, scale: jax.random.normal(next(keys), shape, jnp.float32) * scale
    uni = lambda shape, lo, hi: jax.random.uniform(next(keys), shape, jnp.float32, lo, hi)
    D, F = D_MODEL, FFN_HIDDEN
    L = NSA_CMP_LEN * NSA_DH
    return {
        'x': nrm((BATCH, SEQ, D), 1.0),
        'c': nrm((BATCH, D), 1.0),
        'ada_w': nrm((DEPTH, D, 6 * D), D ** -0.5),
        'ada_b': nrm((DEPTH, 6 * D), 0.02),
        'norm1_g': 1.0 + nrm((DEPTH, D), 0.02),
        'norm2_g': 1.0 + nrm((DEPTH, D), 0.02),
        'ffn_w_up': nrm((DEPTH, D, 2 * F), D ** -0.5),
        'ffn_conv_w': nrm((DEPTH, CONV_WIDTH, F), CONV_WIDTH ** -0.5),
        'ffn_conv_b': nrm((DEPTH, F), 0.02),
        'ffn_w_down': nrm((DEPTH, F, D), F ** -0.5),
        'ev_w_in': nrm((N_EVEN, D, EVEN_COLS), D ** -0.5),
        'ev_shift_mu': uni((N_EVEN, RWKV_COLS), 0.0, 1.0),
        'gla_a_up': nrm((N_EVEN, GLA_LOWRANK, GLA_HEADS * GLA_DK), GLA_LOWRANK ** -0.5),
        'gla_a_b': nrm((N_EVEN, GLA_HEADS * GLA_DK), 0.1),
        'gla_norm_g': 1.0 + nrm((N_EVEN, GLA_DV), 0.02),
        'rw_w0': uni((N_EVEN, RWKV_WIDTH), -5.0, 1.0),
        'rw_w2': nrm((N_EVEN, RWKV_W_LORA, RWKV_WIDTH), RWKV_W_LORA ** -0.5),
        'rw_a0': nrm((N_EVEN, RWKV_WIDTH), 0.1),
        'rw_a2': nrm((N_EVEN, RWKV_A_LORA, RWKV_WIDTH), RWKV_A_LORA ** -0.5),
        'rw_g2': nrm((N_EVEN, RWKV_G_LORA, RWKV_WIDTH), RWKV_G_LORA ** -0.5),
        'rw_k_k': 0.85 + nrm((N_EVEN, RWKV_WIDTH), 0.02),
        'rw_k_a': 1.0 + nrm((N_EVEN, RWKV_WIDTH), 0.02),
        'rw_r_k': nrm((N_EVEN, RWKV_HEADS, RWKV_N), 0.1),
        'rw_gn_w': 1.0 + nrm((N_EVEN, RWKV_WIDTH), 0.02),
        'rw_gn_b': nrm((N_EVEN, RWKV_WIDTH), 0.02),
        'ev_w_out': nrm((N_EVEN, EVEN_MIX_WIDTH, D), EVEN_MIX_WIDTH ** -0.5),
        'od_w_in': nrm((N_ODD, D, ODD_COLS), D ** -0.5),
        'cmp_pe_k': nrm((N_ODD, NSA_CMP_LEN, NSA_DH), 0.1),
        'cmp_w1_k': nrm((N_ODD, L, NSA_CMP_HIDDEN), L ** -0.5),
        'cmp_w2_k': nrm((N_ODD, NSA_CMP_HIDDEN, NSA_DH), NSA_CMP_HIDDEN ** -0.5),
        'cmp_pe_v': nrm((N_ODD, NSA_CMP_LEN, NSA_DH), 0.1),
        'cmp_w1_v': nrm((N_ODD, L, NSA_CMP_HIDDEN), L ** -0.5),
        'cmp_w2_v': nrm((N_ODD, NSA_CMP_HIDDEN, NSA_DH), NSA_CMP_HIDDEN ** -0.5),
        'od_w_out': nrm((N_ODD, NSA_WIDTH, D), NSA_WIDTH ** -0.5),
        'final_norm_g': 1.0 + nrm((D,), 0.02),
    }


def reference(x, c, ada_w, ada_b, norm1_g, norm2_g, ffn_w_up, ffn_conv_w, ffn_conv_b, ffn_w_down,
              ev_w_in, ev_shift_mu, gla_a_up, gla_a_b, gla_norm_g, rw_w0, rw_w2, rw_a0, rw_a2,
              rw_g2, rw_k_k, rw_k_a, rw_r_k, rw_gn_w, rw_gn_b, ev_w_out,
              od_w_in, cmp_pe_k, cmp_w1_k, cmp_w2_k, cmp_pe_v, cmp_w1_v, cmp_w2_v, od_w_out,
              final_norm_g):
    cond = jax.nn.silu(c)
    for layer in range(DEPTH):
        mod = cond @ ada_w[layer] + ada_b[layer]
        sh1, sc1, g1, sh2, sc2, g2 = jnp.split(mod[:, None, :], 6, axis=-1)
        hn = _rmsnorm(x, norm1_g[layer]) * (1.0 + sc1) + sh1
        i = layer // 2
        if layer % 2 == 0:
            mix = _even_mixer(hn, ev_w_in[i], ev_shift_mu[i], gla_a_up[i], gla_a_b[i], gla_norm_g[i],
                              rw_w0[i], rw_w2[i], rw_a0[i], rw_a2[i], rw_g2[i], rw_k_k[i], rw_k_a[i],
                              rw_r_k[i], rw_gn_w[i], rw_gn_b[i], ev_w_out[i])
        else:
            mix = _nsa_mixer(hn, od_w_in[i], cmp_pe_k[i], cmp_w1_k[i], cmp_w2_k[i],
                             cmp_pe_v[i], cmp_w1_v[i], cmp_w2_v[i], od_w_out[i])
        x = x + g1 * mix
        hn = _rmsnorm(x, norm2_g[layer]) * (1.0 + sc2) + sh2
        x = x + g2 * _conv_ffn(hn, ffn_w_up[layer], ffn_conv_w[layer], ffn_conv_b[layer], ffn_w_down[layer])
    return _rmsnorm(x, final_norm_g)
```

```python
import contextlib
import numpy as np
import concourse.bass as bass
import concourse.mybir as mybir
from concourse.bass_utils import run_bass_kernel_spmd

F32 = mybir.dt.float32
BF16 = mybir.dt.bfloat16
AF = mybir.ActivationFunctionType
ALU = mybir.AluOpType
AX = mybir.AxisListType

ENGS = ("pe", "act", "dve", "pool", "sp")


class Tl:
    _n = 0

    def __init__(self, t, space, key=None):
        self.t = t
        self.space = space
        Tl._n += 1
        self.key = key if key is not None else ("t", Tl._n)

    def __getitem__(self, idx):
        return V(self, self.t[idx])

    def ap(self):
        return V(self, self.t[:])

    def part(self, sub):
        return Tl(self.t, self.space, key=(self.key, sub))


class V:
    def __init__(self, tile, ap):
        self.tile = tile
        self.ap = ap

    def __getitem__(self, idx):
        return V(self.tile, self.ap[idx])

    def rearrange(self, *a, **kw):
        return V(self.tile, self.ap.rearrange(*a, **kw))

    def bitcast(self, dt):
        return V(self.tile, self.ap.bitcast(dt))

    @property
    def shape(self):
        return self.ap.shape


class Op:
    __slots__ = ("eng", "fn", "waits", "inc", "kind")


class K:
    def __init__(self, nc, n_dma_slots=(("sp", 40), ("pool", 16), ("act", 12)), same_engine_raw=True):
        self.nc = nc
        self.stack = contextlib.ExitStack()
        self.ops = {e: [] for e in ENGS}
        self.count = {e: 0 for e in ENGS}
        self.sem = {}
        for e in ENGS:
            self.sem[e] = self.stack.enter_context(nc.semaphore("s_" + e))
        self.slots = {}
        for q, n in n_dma_slots:
            self.slots[q] = [[self.stack.enter_context(nc.semaphore(f"d_{q}{i}")), 0] for i in range(n)]
        self.slot_rr = {q: 0 for q, _ in n_dma_slots}
        self.waited = {e: {} for e in ENGS}
        self.last_w = {}
        self.readers = {}
        self.same_engine_raw = same_engine_raw
        self.n_ops = 0
        self.stacks = [self.stack]
        self.dram_w = {}
        self.pe_rg = {}
        self.uid = 0

    def push(self):
        self.stacks.append(contextlib.ExitStack())

    def pop(self):
        self.barrier()
        self.stacks.pop().close()

    def sb(self, name, shape, dtype=F32):
        self.uid += 1
        t = self.stacks[-1].enter_context(self.nc.sbuf_tensor(f"{name}_{self.uid}", list(shape), dtype))
        return Tl(t, "sb")

    def ps(self, name, shape, dtype=F32):
        self.uid += 1
        t = self.stacks[-1].enter_context(self.nc.psum_tensor(f"{name}_{self.uid}", list(shape), dtype))
        return Tl(t, "ps")

    def dram(self, name, shape, dtype=F32, kind="Internal"):
        t = self.nc.dram_tensor(name, list(shape), dtype, kind=kind).ap()
        return Tl(t, "dram")

    def _need(self, eng, deps, sem, val, src_eng):
        if sem is None:
            return
        if src_eng == eng and eng == "pe":
            return
        cur = deps.get(id(sem))
        if cur is None or cur[1] < val:
            deps[id(sem)] = (sem, val)

    def _record(self, eng, fn, reads, writes, dma_q=None, rg=None):
        deps = {}
        rkeys = []
        wkeys = []
        for v in reads:
            if v is None:
                continue
            tl = v.tile if isinstance(v, V) else v
            rkeys.append((tl.key, tl.space))
        for v in writes:
            tl = v.tile if isinstance(v, V) else v
            wkeys.append((tl.key, tl.space))
        is_dma = dma_q is not None
        for key, space in rkeys:
            if space == "dram":
                for (sem, val, seng, wdma) in self.dram_w.get(key, []):
                    self._need(eng, deps, sem, val, None)
                continue
            lw = self.last_w.get(key)
            if lw is not None:
                sem, val, seng, wdma = lw
                if seng == eng and not wdma and not is_dma and not self.same_engine_raw and eng != "pe":
                    pass
                else:
                    self._need(eng, deps, sem, val, seng if not (wdma or is_dma) else None)
            if space == "ps":
                for (sem, val, seng, rdma) in self.readers.get(key, []):
                    if seng != eng:
                        self._need(eng, deps, sem, val, seng)
        for key, space in wkeys:
            if rg is not None and space == "ps":
                prev = self.pe_rg.get(key)
                if prev is not None and prev[0] != rg:
                    self._need(eng, deps, prev[1][0], prev[1][1], None)
            lw = self.last_w.get(key) if space != "dram" else None
            if lw is not None:
                sem, val, seng, wdma = lw
                if seng != eng or wdma or is_dma:
                    self._need(eng, deps, sem, val, None if (wdma or is_dma) else seng)
            for (sem, val, seng, rdma) in self.readers.get(key, []):
                if seng != eng or rdma or is_dma:
                    self._need(eng, deps, sem, val, None if (rdma or is_dma) else seng)
        if is_dma:
            sl = self.slots[dma_q]
            i = self.slot_rr[dma_q]
            self.slot_rr[dma_q] = (i + 1) % len(sl)
            sem, uses = sl[i]
            if uses > 0:
                self._need(eng, deps, sem, 16 * uses, None)
            sl[i][1] = uses + 1
            tok = (sem, 16 * (uses + 1), eng, True)
            inc = (sem, 16)
        else:
            self.count[eng] += 1
            tok = (self.sem[eng], self.count[eng], eng, False)
            inc = (self.sem[eng], 1)
        waits = []
        wd = self.waited[eng]
        for sid, (sem, val) in deps.items():
            if wd.get(sid, 0) >= val:
                continue
            wd[sid] = val
            waits.append((sem, val))
        op = Op()
        op.eng = eng
        op.fn = fn
        op.waits = waits
        op.inc = inc
        self.ops[eng].append(op)
        self.n_ops += 1
        for key, space in rkeys:
            lst = self.readers.setdefault(key, [])
            for n_, t_ in enumerate(lst):
                if t_[0] is tok[0]:
                    lst[n_] = tok
                    break
            else:
                lst.append(tok)
        for key, space in wkeys:
            if space == "dram":
                self.dram_w.setdefault(key, []).append(tok)
                continue
            if rg is not None and space == "ps":
                self.pe_rg[key] = (rg, tok)
            self.last_w[key] = tok
            self.readers[key] = []
        return tok

    def barrier(self):
        for e in ENGS:
            waits = []
            wd = self.waited[e]
            for o in ENGS:
                if o == e or self.count[o] == 0:
                    continue
                sem = self.sem[o]
                if wd.get(id(sem), 0) < self.count[o]:
                    wd[id(sem)] = self.count[o]
                    waits.append((sem, self.count[o]))
            for q, sl in self.slots.items():
                for sem, uses in sl:
                    if uses > 0 and wd.get(id(sem), 0) < 16 * uses:
                        wd[id(sem)] = 16 * uses
                        waits.append((sem, 16 * uses))
            if waits:
                op = Op()
                op.eng = e
                op.fn = None
                op.waits = waits
                op.inc = None
                self.ops[e].append(op)
        self.last_w = {}
        self.readers = {}
        self.dram_w = {}

    @staticmethod
    def _a(x):
        return x.ap if isinstance(x, V) else x

    def mm(self, out, lhsT, rhs, start=True, stop=True, **kw):
        o, l, r = out.ap, lhsT.ap, rhs.ap
        rg = (int(l.start_partition()), int(l.shape[0]))
        return self._record("pe", lambda e: e.matmul(o, l, r, start=start, stop=stop, **kw), [lhsT, rhs], [out], rg=rg)

    def tr(self, out, in_, ident):
        o, i, d = out.ap, in_.ap, ident.ap
        rg = (int(i.start_partition()), int(i.shape[0]))
        return self._record("pe", lambda e: e.transpose(o, i, d), [in_, ident], [out], rg=rg)

    def act(self, out, in_, func, bias=None, scale=1.0, accum=None, eng="act"):
        o, i = out.ap, in_.ap
        kw = {}
        reads = [in_]
        if bias is not None:
            kw["bias"] = self._a(bias)
            if isinstance(bias, V):
                reads.append(bias)
        if isinstance(scale, V):
            reads.append(scale)
        kw["scale"] = self._a(scale)
        writes = [out]
        if accum is not None:
            kw["accum_out"] = accum.ap
            writes.append(accum)
        return self._record(eng, lambda e: e.activation(o, i, func, **kw), reads, writes)

    def tt(self, out, a, b, op, eng="dve"):
        o, x, y = out.ap, a.ap, b.ap
        return self._record(eng, lambda e: e.tensor_tensor(o, x, y, op), [a, b], [out])

    def ts(self, out, a, s1, s2=None, op0=ALU.mult, op1=None, eng="dve", accum=None):
        o, x = out.ap, a.ap
        reads = [a]
        for s in (s1, s2):
            if isinstance(s, V):
                reads.append(s)
        a1, a2 = self._a(s1), self._a(s2)
        kw = {}
        writes = [out]
        if accum is not None:
            kw["accum_out"] = accum.ap
            writes.append(accum)
        if op1 is None:
            return self._record(eng, lambda e: e.tensor_scalar(o, x, a1, None, op0, **kw), reads, writes)
        return self._record(eng, lambda e: e.tensor_scalar(o, x, a1, a2, op0, op1, **kw), reads, writes)

    def stt(self, out, a, scalar, b, op0, op1, eng="dve"):
        o, x, y = out.ap, a.ap, b.ap
        reads = [a, b]
        if isinstance(scalar, V):
            reads.append(scalar)
        s = self._a(scalar)
        return self._record(eng, lambda e: e.scalar_tensor_tensor(o, x, s, y, op0, op1), reads, [out])

    def copy(self, out, in_, eng="dve"):
        o, i = out.ap, in_.ap
        if eng == "act":
            return self._record(eng, lambda e: e.copy(o, i), [in_], [out])
        return self._record(eng, lambda e: e.tensor_copy(o, i), [in_], [out])

    def memset(self, out, val, eng="dve"):
        o = out.ap
        return self._record(eng, lambda e: e.memset(o, val), [], [out])

    def reduce(self, out, in_, op, axis=AX.X, eng="dve"):
        o, i = out.ap, in_.ap
        return self._record(eng, lambda e: e.tensor_reduce(o, i, axis, op), [in_], [out])

    def recip(self, out, in_):
        o, i = out.ap, in_.ap
        return self._record("dve", lambda e: e.reciprocal(o, i), [in_], [out])

    def generic(self, eng, fn, reads, writes):
        return self._record(eng, fn, reads, writes)

    def dma(self, out, in_, q="sp", **kw):
        o, i = out.ap, in_.ap
        return self._record(q, lambda e: e.dma_start(o, i, **kw), [in_], [out], dma_q=q)

    def check_deadlock(self):
        ptr = {e: 0 for e in ENGS}
        semv = {}
        progress = True
        while progress:
            progress = False
            for e in ENGS:
                lst = self.ops[e]
                while ptr[e] < len(lst):
                    op = lst[ptr[e]]
                    if all(semv.get(id(sem), 0) >= val for sem, val in op.waits):
                        if op.fn is not None:
                            semv[id(op.inc[0])] = semv.get(id(op.inc[0]), 0) + op.inc[1]
                        ptr[e] += 1
                        progress = True
                    else:
                        break
        stuck = {e: (ptr[e], len(self.ops[e])) for e in ENGS if ptr[e] < len(self.ops[e])}
        if stuck:
            raise RuntimeError(f"semaphore deadlock: {stuck}")

    def emit(self):
        nc = self.nc
        self.barrier()
        self.check_deadlock()
        ops = self.ops
        with nc.Block() as block:
            def run(eng_obj, lst):
                for op in lst:
                    for sem, val in op.waits:
                        eng_obj.wait_ge(sem, val)
                    if op.fn is not None:
                        ins = op.fn(eng_obj)
                        ins.then_inc(op.inc[0], op.inc[1])

            @block.tensor
            def _(e):
                run(e, ops["pe"])

            @block.scalar
            def _(e):
                run(e, ops["act"])

            @block.vector
            def _(e):
                run(e, ops["dve"])

            @block.gpsimd
            def _(e):
                run(e, ops["pool"])

            @block.sync
            def _(e):
                run(e, ops["sp"])
        self.stack.close()

T = 4096
D = 1024
NT = T // 128
EPS = 1e-6
EV_COLS = 3344
OD_COLS = 2608
FF = 2816
NKC = D // 128


class Ctx:
    pass


def _rows(ap2d):
    return ap2d.rearrange("(kc p) n -> p kc n", p=128)


def phase_consts(k, cx):
    cx.ident = k.sb("ident", [128, 128], BF16)
    cx.identf = k.sb("identf", [128, 128], F32)
    k.dma(cx.identf[:], cx.inp["c_ident"][:])
    k.copy(cx.ident[:], cx.identf[:])
    cx.ones_row = k.sb("ones_row", [1, 128], F32)
    k.memset(cx.ones_row[:], 1.0)


def phase_mod(k, cx, layer):
    inp = cx.inp
    bc = {}
    for nm in ("gsc1", "sh1", "g1", "gsc2", "sh2", "g2"):
        bc[nm] = k.sb(f"bc_{nm}", [128, D], F32)
    k.push()
    ccol = k.sb("ccol", [128, NKC], F32)
    k.dma(ccol[:], inp["c_col"][:])
    cond = k.sb("cond", [128, NKC], F32)
    k.act(cond[:], ccol[:], AF.Silu)
    modrow = k.sb("modrow", [1, 6 * D], F32)
    brow = k.sb("brow", [1, 6 * D], F32)
    k.dma(brow[:], inp["ada_b"][layer:layer + 1, :])
    g12 = k.sb("g12", [1, 2 * D], F32)
    k.dma(g12[:, 0:D], inp["norm1_g"][layer:layer + 1, :])
    k.dma(g12[:, D:2 * D], inp["norm2_g"][layer:layer + 1, :])
    wst = [k.sb(f"adast{i}", [128, NKC, 512], F32) for i in range(2)]
    pm = [k.ps(f"pmod{i}", [128, 512], F32) for i in range(2)]
    aw = _rows(inp["ada_w"][layer])
    for blk in range(12):
        w = wst[blk % 2]
        k.dma(w[:], aw[:, :, blk * 512:(blk + 1) * 512])
        p = pm[blk % 2]
        for kc in range(NKC):
            k.mm(p[0:1, :], cond[:, kc:kc + 1], w[:, kc, :], start=(kc == 0), stop=(kc == NKC - 1))
        k.tt(modrow[:, blk * 512:(blk + 1) * 512], p[0:1, :], brow[:, blk * 512:(blk + 1) * 512], ALU.add)
    for which, (sc_off, goff) in enumerate(((1, 0), (4, 1))):
        k.ts(modrow[:, sc_off * D:(sc_off + 1) * D], modrow[:, sc_off * D:(sc_off + 1) * D], 1.0, None, ALU.add)
        k.tt(modrow[:, sc_off * D:(sc_off + 1) * D], modrow[:, sc_off * D:(sc_off + 1) * D], g12[:, goff * D:(goff + 1) * D], ALU.mult)
    order = ("sh1", "gsc1", "g1", "sh2", "gsc2", "g2")
    i = 0
    for j, nm in enumerate(order):
        for half in range(2):
            p = pm[i % 2]
            i += 1
            k.mm(p[:], cx.ones_row[:], modrow[:, j * D + half * 512: j * D + (half + 1) * 512])
            k.copy(bc[nm][:, half * 512:(half + 1) * 512], p[:], eng=("act" if half else "dve"))
    k.pop()
    return bc


def load_weight_bf16(k, w_rows, ncols, name, col0=0):
    nkc = w_rows.shape[1]
    wb = k.sb(name, [128, nkc, ncols], BF16)
    k.push()
    st = [k.sb(f"{name}_st{i}", [128, ncols], F32) for i in range(2)]
    engs = ("dve", "pool", "act")
    for kc in range(nkc):
        s = st[kc % 2]
        k.dma(s[:], w_rows[:, kc, col0:col0 + ncols])
        k.copy(wb[:, kc, :], s[:], eng=engs[kc % 3])
    k.pop()
    return wb


class NormFront:
    def __init__(self, k, cx, gsc, sh):
        self.k, self.cx, self.gsc, self.sh = k, cx, gsc, sh
        self.xt = [k.sb(f"nf_x{i}", [128, D], F32) for i in range(2)]
        self.xm = [k.sb(f"nf_xm{i}", [128, D], F32) for i in range(2)]
        self.xb = [k.sb(f"nf_xb{i}", [128, D], BF16) for i in range(2)]
        self.junk = k.sb("nf_junk", [128, D], BF16)
        self.ss = [k.sb(f"nf_ss{i}", [128, 2], F32) for i in range(2)]
        self.pT = [k.ps(f"nf_pT{i}", [128, D], BF16) for i in range(2)]
        self.i = 0

    def tile(self, x_src, dst):
        k, cx = self.k, self.cx
        i = self.i % 2
        self.i += 1
        xt, xm, xb, ss, pT = self.xt[i], self.xm[i], self.xb[i], self.ss[i], self.pT[i]
        k.dma(xt[:], x_src)
        k.memset(ss[:], 0.0, eng="pool")
        k.act(self.junk[:], xt[:], AF.Square, accum=ss[:, 0:1])
        k.act(ss[:, 1:2], ss[:, 0:1], AF.Sqrt, bias=EPS, scale=1.0 / D)
        k.recip(ss[:, 1:2], ss[:, 1:2])
        k.stt(xm[:], xt[:], ss[:, 1:2], self.gsc[:], ALU.mult, ALU.mult)
        k.tt(xb[:], xm[:], self.sh[:], ALU.add, eng="pool")
        for kc in range(NKC):
            k.tr(pT[:, kc * 128:(kc + 1) * 128], xb[:, kc * 128:(kc + 1) * 128], cx.ident[:])
        k.copy(dst[:, 0:4, :], pT[:, 0:512].rearrange("p (k t) -> p k t", k=4), eng="act")
        k.copy(dst[:, 4:8, :], pT[:, 512:1024].rearrange("p (k t) -> p k t", k=4), eng="dve")
        return xt


class Evac:
    def __init__(self, k, name, shape, dtype, n=3):
        self.k = k
        self.bufs = [k.sb(f"{name}{i}", shape, dtype) for i in range(n)]
        self.i = 0

    def next(self):
        b = self.bufs[self.i % len(self.bufs)]
        self.i += 1
        return b


def phase_proj(k, cx, x_in, w_dram, ncols, bc_gsc, bc_sh, fm_specs, tm_specs, name):
    k.push()
    wb = load_weight_bf16(k, _rows(w_dram), ncols, name + "_wb")
    nf = NormFront(k, cx, bc_gsc, bc_sh)
    xnT = [k.sb(f"{name}_xnT{i}", [128, NKC, 512], BF16) for i in range(2)]
    pmm = [k.ps(f"{name}_pm{i}", [128, 512], F32) for i in range(4)]
    ev32 = Evac(k, name + "_ev32", [128, 512], F32, 3)
    ev16 = Evac(k, name + "_ev16", [128, 512], BF16, 3)
    pi = 0
    ei = 0
    engs = ("act", "dve")
    for tb in range(T // 512):
        xn = xnT[tb % 2]
        for j in range(4):
            t0 = tb * 512 + j * 128
            nf.tile(x_in[t0:t0 + 128, :], xn[:, :, j * 128:(j + 1) * 128])
        for (c0, n, dst, row0, dt, scale) in fm_specs:
            for cc in range(0, n, 128):
                m = min(128, n - cc)
                p = pmm[pi % 4]
                pi += 1
                for kc in range(NKC):
                    k.mm(p[0:m, :], wb[:, kc, c0 + cc:c0 + cc + m], xn[:, kc, :], start=(kc == 0), stop=(kc == NKC - 1))
                e = (ev32 if dt == F32 else ev16).next()
                eng = engs[ei % 2]
                ei += 1
                if eng == "act":
                    k.act(e[0:m, :], p[0:m, :], AF.Copy, scale=scale)
                else:
                    k.ts(e[0:m, :], p[0:m, :], scale, None, ALU.mult)
                k.dma(dst[row0 + cc:row0 + cc + m, tb * 512:(tb + 1) * 512], e[0:m, :], q="pool")
        for (c0, n, dst, col0, dt) in tm_specs:
            for j in range(4):
                t0 = tb * 512 + j * 128
                for cc in range(0, n, 512):
                    m = min(512, n - cc)
                    p = pmm[pi % 4]
                    pi += 1
                    for kc in range(NKC):
                        k.mm(p[:, 0:m], xn[:, kc, j * 128:(j + 1) * 128], wb[:, kc, c0 + cc:c0 + cc + m], start=(kc == 0), stop=(kc == NKC - 1))
                    e = (ev32 if dt == F32 else ev16).next()
                    k.copy(e[:, 0:m], p[:, 0:m], eng=engs[ei % 2])
                    ei += 1
                    k.dma(dst[t0:t0 + 128, col0 + cc:col0 + cc + m], e[:, 0:m], q="pool")
    k.pop()


def phase_gla(k, cx):
    inp = cx.inp
    k.push()
    a_up = k.sb("g_aup", [16, 256], F32)
    k.dma(a_up[:], inp["gla_a_up"][:])
    nab = k.sb("g_nab", [128, 2], F32)
    k.dma(nab[:], inp["gla_a_b_col"][:])
    k.ts(nab[:], nab[:], -1.0, None, ALU.mult)
    gbc = k.sb("g_gbc", [128, 128], F32)
    k.dma(gbc[:], inp["gla_norm_g_bc"][:])
    rmask = k.sb("g_rmask", [128, 512], F32)
    k.dma(rmask[:], inp["c_scan128"][:])
    tri = k.sb("g_tri", [128, 128], F32)
    k.dma(tri[:], inp["c_tri_incl"][:])
    S = [k.sb(f"g_S{i}", [128, 128], F32) for i in range(2)]
    Sbf = [k.sb(f"g_Sbf{i}", [128, 128], BF16) for i in range(2)]
    for i in range(2):
        k.memset(S[i][:], 0.0)
        k.memset(Sbf[i][:], 0.0)
    lrT = [k.sb(f"g_lrT{i}", [16, 512], F32) for i in range(2)]
    qT = [k.sb(f"g_qT{i}", [128, 512], F32) for i in range(2)]
    kT = [k.sb(f"g_kT{i}", [128, 512], F32) for i in range(2)]
    e1 = [k.sb(f"g_e1{i}", [128, 512], F32) for i in range(2)]
    sp = [k.sb(f"g_sp{i}", [128, 512], F32) for i in range(2)]
    bsp = [k.sb(f"g_bsp{i}", [128, 512], F32) for i in range(2)]
    eb = [k.sb(f"g_eb{i}", [128, 512], F32) for i in range(2)]
    ebi = [k.sb(f"g_ebi{i}", [128, 512], F32) for i in range(2)]
    qs = [k.sb(f"g_qs{i}", [128, 512], BF16) for i in range(2)]
    ks = [k.sb(f"g_ks{i}", [128, 512], BF16) for i in range(2)]
    kh = [k.sb(f"g_kh{i}", [128, 128], BF16) for i in range(2)]
    khT = [k.sb(f"g_khT{i}", [128, 128], BF16) for i in range(2)]
    vt = [k.sb(f"g_vt{i}", [128, 256], BF16) for i in range(3)]
    ogt = [k.sb(f"g_ogt{i}", [128, 256], F32) for i in range(3)]
    sil = [k.sb(f"g_sil{i}", [128, 256], F32) for i in range(2)]
    At = [k.sb(f"g_At{i}", [128, 128], BF16) for i in range(3)]
    t1 = [k.sb(f"g_t1{i}", [128, 128], F32) for i in range(2)]
    obuf = [k.sb(f"g_ob{i}", [128, 256], BF16) for i in range(2)]
    junk = k.sb("g_junk", [128, 128], BF16)
    ss = [k.sb(f"g_ss{i}", [128, 2], F32) for i in range(4)]
    pz = k.ps("g_pz", [128, 512], F32)
    pA = [k.ps(f"g_pA{i}", [128, 512], F32) for i in range(2)]
    po = [k.ps(f"g_po{i}", [128, 512], F32) for i in range(2)]
    pkv = k.ps("g_pkv", [128, 512], F32)
    pT = k.ps("g_pT", [128, 1024], BF16)
    ci = 0
    hi = 0
    for tb in range(T // 512):
        tsl = slice(tb * 512, (tb + 1) * 512)
        lr = lrT[tb % 2]
        k.dma(lr[:], cx.G_lr[0:16, tsl])
        for hp in range(2):
            b = (tb * 2 + hp) % 2
            k.dma(qT[b][:], cx.G_qk[hp * 128:(hp + 1) * 128, tsl])
            k.dma(kT[b][:], cx.G_qk[256 + hp * 128:256 + (hp + 1) * 128, tsl])
            k.mm(pz[:], a_up[:, hp * 128:(hp + 1) * 128], lr[:])
            k.act(e1[b][:], pz[:], AF.Exp, bias=nab[:, hp:hp + 1], scale=-1.0)
            k.act(sp[b][:], e1[b][:], AF.Ln, bias=1.0)
            o_, m_, s_ = bsp[b].t[:], rmask.t[:], sp[b].t[:]
            k.generic("dve", lambda e, o_=o_, m_=m_, s_=s_: e.tensor_tensor_scan(o_, m_, s_, 0.0, ALU.mult, ALU.add), [rmask, sp[b]], [bsp[b]])
            k.act(eb[b][:], bsp[b][:], AF.Exp, scale=-1.0 / 16)
            k.act(ebi[b][:], bsp[b][:], AF.Exp, scale=1.0 / 16)
            k.stt(qs[b][:], qT[b][:], 0.125, eb[b][:], ALU.mult, ALU.mult)
            k.tt(ks[b][:], kT[b][:], ebi[b][:], ALU.mult, eng="pool")
            for c in range(4):
                t0 = tb * 512 + c * 128
                cs = slice(c * 128, (c + 1) * 128)
                v = vt[ci % 3]
                og = ogt[ci % 3]
                k.dma(v[:], cx.G_v[t0:t0 + 128, hp * 256:(hp + 1) * 256])
                k.dma(og[:], cx.G_og[t0:t0 + 128, hp * 256:(hp + 1) * 256])
                sl_ = sil[ci % 2]
                k.act(sl_[:], og[:], AF.Silu)
                khc = kh[ci % 2]
                k.ts(khc[:], ks[b][:, cs], eb[b][:, c * 128 + 127:c * 128 + 128], None, ALU.mult)
                k.tr(pT[:, 0:128], khc[:], cx.ident[:])
                kht = khT[ci % 2]
                k.copy(kht[:], pT[:, 0:128], eng="act")
                ob = obuf[ci % 2]
                for hh in range(2):
                    hb = hh * 64
                    a_ps = pA[hi % 2]
                    o_ps = po[hi % 2]
                    at = At[hi % 3]
                    s2 = ss[hi % 4]
                    tt1 = t1[hi % 2]
                    hi += 1
                    k.mm(a_ps[:, 0:128], ks[b][hb:hb + 64, cs], qs[b][hb:hb + 64, cs])
                    k.tt(at[:], a_ps[:, 0:128], tri[:], ALU.mult)
                    k.mm(o_ps[:, 0:128], at[:], v[:, hh * 128:(hh + 1) * 128], start=True, stop=False)
                    k.mm(o_ps[:, 0:128], qs[b][hb:hb + 64, cs], Sbf[hp][hb:hb + 64, :], start=False, stop=True)
                    k.memset(s2[:], 0.0, eng="pool")
                    k.act(junk[:], o_ps[:, 0:128], AF.Square, accum=s2[:, 0:1])
                    k.act(s2[:, 1:2], s2[:, 0:1], AF.Sqrt, bias=EPS, scale=1.0 / 128)
                    k.recip(s2[:, 1:2], s2[:, 1:2])
                    k.stt(tt1[:], o_ps[:, 0:128], s2[:, 1:2], gbc[:], ALU.mult, ALU.mult)
                    k.tt(ob[:, hh * 128:(hh + 1) * 128], tt1[:], sl_[:, hh * 128:(hh + 1) * 128], ALU.mult, eng="pool")
                k.dma(cx.O_mix[t0:t0 + 128, hp * 256:(hp + 1) * 256], ob[:], q="pool")
                k.mm(pkv[:, 0:256], kht[:], v[:])
                ebc = eb[b][:, c * 128 + 127:c * 128 + 128]
                k.stt(S[hp][0:64, :], S[hp][0:64, :], ebc[0:64, :], pkv[0:64, 0:128], ALU.mult, ALU.add)
                k.stt(S[hp][64:128, :], S[hp][64:128, :], ebc[64:128, :], pkv[64:128, 128:256], ALU.mult, ALU.add)
                k.copy(Sbf[hp][:], S[hp][:], eng="act")
                ci += 1
    k.pop()


def phase_wout(k, cx, x_in, x_out, w_dram, g_bc, src, src_mode, name):
    k.push()
    wb = load_weight_bf16(k, _rows(w_dram), D, name + "_wb")
    oT = [k.sb(f"{name}_oT{i}", [128, NKC, 512], BF16) for i in range(2)]
    ot = [k.sb(f"{name}_ot{i}", [128, D], BF16) for i in range(2)]
    xt = [k.sb(f"{name}_x{i}", [128, D], F32) for i in range(2)]
    tmp = [k.sb(f"{name}_tmp{i}", [128, D], F32) for i in range(2)]
    xo = [k.sb(f"{name}_xo{i}", [128, D], F32) for i in range(2)]
    pT = [k.ps(f"{name}_pT{i}", [128, D], BF16) for i in range(2)]
    pm = [k.ps(f"{name}_pm{i}", [128, 512], F32) for i in range(4)]
    ti = 0
    pi = 0
    for tb in range(T // 512):
        o = oT[tb % 2]
        if src_mode == "fm":
            k.dma(o[:], src[:, tb * 512:(tb + 1) * 512].rearrange("(kc p) t -> p kc t", p=128))
        else:
            for j in range(4):
                t0 = tb * 512 + j * 128
                a = ot[(tb * 4 + j) % 2]
                p = pT[(tb * 4 + j) % 2]
                k.dma(a[:], src[t0:t0 + 128, :])
                for kc in range(NKC):
                    k.tr(p[:, kc * 128:(kc + 1) * 128], a[:, kc * 128:(kc + 1) * 128], cx.ident[:])
                k.copy(o[:, 0:4, j * 128:(j + 1) * 128], p[:, 0:512].rearrange("p (k t) -> p k t", k=4), eng="act")
                k.copy(o[:, 4:8, j * 128:(j + 1) * 128], p[:, 512:1024].rearrange("p (k t) -> p k t", k=4), eng="dve")
        for j in range(4):
            t0 = tb * 512 + j * 128
            x = xt[ti % 2]
            tm_ = tmp[ti % 2]
            xo_ = xo[ti % 2]
            ti += 1
            k.dma(x[:], x_in[t0:t0 + 128, :])
            for half in range(2):
                p = pm[pi % 4]
                pi += 1
                for kc in range(NKC):
                    k.mm(p[:], o[:, kc, j * 128:(j + 1) * 128], wb[:, kc, half * 512:(half + 1) * 512], start=(kc == 0), stop=(kc == NKC - 1))
                k.tt(tm_[:, half * 512:(half + 1) * 512], p[:], g_bc[:, half * 512:(half + 1) * 512], ALU.mult)
            k.tt(xo_[:], tm_[:], x[:], ALU.add, eng="pool")
            k.dma(x_out[t0:t0 + 128, :], xo_[:], q="pool")
    k.pop()


def phase_ffn_up(k, cx, layer, x_in, bc):
    inp = cx.inp
    TB = 512
    NF = FF // 128
    k.push()
    wup = load_weight_bf16(k, _rows(inp["ffn_w_up"][layer]), 2 * FF, "f_wup")
    cw = k.sb("f_cw", [128, NF, 4], F32)
    k.dma(cw[:], inp["ffn_conv_col"][layer])
    nf = NormFront(k, cx, bc["gsc2"], bc["sh2"])
    xnT = [k.sb(f"f_xnT{i}", [128, NKC, TB], BF16) for i in range(2)]
    halo = k.sb("f_halo", [128, NF, 2], F32)
    k.memset(halo[:], 0.0)
    ub = [k.sb(f"f_ub{i}", [128, TB + 2], F32) for i in range(3)]
    cv = [k.sb(f"f_cv{i}", [128, TB], F32) for i in range(3)]
    ge = [k.sb(f"f_ge{i}", [128, TB], F32) for i in range(3)]
    hb = [k.sb(f"f_hb{i}", [128, TB], BF16) for i in range(3)]
    pu = [k.ps(f"f_pu{i}", [128, 512], F32) for i in range(3)]
    pv = [k.ps(f"f_pv{i}", [128, 512], F32) for i in range(3)]
    ui = 0
    for tb in range(T // TB):
        xn = xnT[tb % 2]
        for j in range(TB // 128):
            t0 = tb * TB + j * 128
            nf.tile(x_in[t0:t0 + 128, :], xn[:, :, j * 128:(j + 1) * 128])
        pend = None

        def tail(st):
            fc, u, c_, g_, h_, p_v = st
            k.ts(c_[:], u[:, 2:TB + 2], cw[:, fc, 2:3], cw[:, fc, 3:4], ALU.mult, ALU.add, eng="pool")
            k.stt(c_[:], u[:, 1:TB + 1], cw[:, fc, 1:2], c_[:], ALU.mult, ALU.add)
            k.stt(c_[:], u[:, 0:TB], cw[:, fc, 0:1], c_[:], ALU.mult, ALU.add)
            k.act(g_[:], c_[:], AF.Gelu)
            k.tt(h_[:], g_[:], p_v[:, 0:TB], ALU.mult)
            k.dma(cx.H_fm[fc * 128:(fc + 1) * 128, tb * TB:(tb + 1) * TB], h_[:], q="pool")

        for fc in range(NF):
            p_u = pu[ui % 3]
            p_v = pv[ui % 3]
            u = ub[ui % 3]
            c_ = cv[ui % 3]
            g_ = ge[ui % 3]
            h_ = hb[ui % 3]
            ui += 1
            for kc in range(NKC):
                k.mm(p_u[:, 0:TB], wup[:, kc, fc * 128:(fc + 1) * 128], xn[:, kc, :], start=(kc == 0), stop=(kc == NKC - 1))
            for kc in range(NKC):
                k.mm(p_v[:, 0:TB], wup[:, kc, FF + fc * 128:FF + (fc + 1) * 128], xn[:, kc, :], start=(kc == 0), stop=(kc == NKC - 1))
            k.copy(u[:, 0:2], halo[:, fc, :], eng="pool")
            k.copy(u[:, 2:TB + 2], p_u[:, 0:TB], eng="act")
            k.copy(halo[:, fc, :], u[:, TB:TB + 2], eng="pool")
            if pend is not None:
                tail(pend)
            pend = (fc, u, c_, g_, h_, p_v)
        tail(pend)
    k.pop()


def phase_ffn_down(k, cx, layer, x_in, x_out, bc, final=False):
    inp = cx.inp
    TB = 512
    NF = FF // 128
    k.push()
    wdn = load_weight_bf16(k, _rows(inp["ffn_w_down"][layer]), D, "f_wdn")
    hT = [k.sb(f"f_hT{i}", [128, NF, TB], BF16) for i in range(2)]
    pd = [k.ps(f"f_pd{i}", [128, 512], F32) for i in range(4)]
    xt = [k.sb(f"f_x{i}", [128, D], F32) for i in range(2)]
    tmp = [k.sb(f"f_tmp{i}", [128, D], F32) for i in range(2)]
    if final:
        fg = k.sb("f_fg", [128, D], F32)
        k.dma(fg[:], inp["final_norm_g_bc"][:])
        fss = [k.sb(f"f_fss{i}", [128, 2], F32) for i in range(2)]
        fjunk = k.sb("f_fjunk", [128, D], BF16)
    ti = 0
    di = 0
    for tb in range(T // TB):
        h = hT[tb % 2]
        k.dma(h[:], cx.H_fm[:, tb * TB:(tb + 1) * TB].rearrange("(fc p) t -> p fc t", p=128))
        for j in range(TB // 128):
            t0 = tb * TB + j * 128
            x = xt[ti % 2]
            tm_ = tmp[ti % 2]
            k.dma(x[:], x_in[t0:t0 + 128, :])
            for half in range(2):
                p = pd[di % 4]
                di += 1
                for fc in range(NF):
                    k.mm(p[:], h[:, fc, j * 128:(j + 1) * 128], wdn[:, fc, half * 512:(half + 1) * 512], start=(fc == 0), stop=(fc == NF - 1))
                k.tt(tm_[:, half * 512:(half + 1) * 512], p[:], bc["g2"][:, half * 512:(half + 1) * 512], ALU.mult)
            k.tt(x[:], tm_[:], x[:], ALU.add, eng="pool")
            if not final:
                k.dma(x_out[t0:t0 + 128, :], x[:], q="pool")
            else:
                s2 = fss[ti % 2]
                k.memset(s2[:], 0.0, eng="pool")
                k.act(fjunk[:], x[:], AF.Square, accum=s2[:, 0:1])
                k.act(s2[:, 1:2], s2[:, 0:1], AF.Sqrt, bias=EPS, scale=1.0 / D)
                k.recip(s2[:, 1:2], s2[:, 1:2])
                k.stt(tm_[:], x[:], s2[:, 1:2], fg[:], ALU.mult, ALU.mult)
                k.dma(x_out[t0:t0 + 128, :], tm_[:], q="pool")
            ti += 1
    k.pop()


def phase_rwkv(k, cx):
    inp = cx.inp
    LG = 0.6065306597126334
    GN_EPS = 64e-5
    k.push()

    def const(name, shape, dtype=F32, src=None):
        t = k.sb("rc_" + name, shape, F32)
        k.dma(t[:], inp[src or ("rw_" + name)][:])
        if dtype == F32:
            return t
        tb_ = k.sb("rcb_" + name, shape, dtype)
        k.copy(tb_[:], t[:])
        return tb_

    mu = const("mu_col", [128, 10])
    omu = k.sb("rc_omu", [128, 10], F32)
    k.ts(omu[:], mu[:], -1.0, 1.0, ALU.mult, ALU.add)
    mu_k = const("mu_k_col", [128, 4])
    omu_k = k.sb("rc_omu_k", [128, 4], F32)
    k.ts(omu_k[:], mu_k[:], -1.0, 1.0, ALU.mult, ALU.add)
    mu_al = const("mu_al_col", [64, 1])
    omu_al = k.sb("rc_omu_al", [64, 1], F32)
    k.ts(omu_al[:], mu_al[:], -1.0, 1.0, ALU.mult, ALU.add)
    muv = const("muv_bc", [128, 512])
    omuv = k.sb("rc_omuv", [128, 512], F32)
    k.ts(omuv[:], muv[:], -1.0, 1.0, ALU.mult, ALU.add)
    w0 = const("w0_col", [128, 4])
    a0 = const("a0_col", [128, 4])
    kkc = const("k_k_col", [128, 4])
    kac = const("k_a_col", [128, 4])
    okac = k.sb("rc_okac", [128, 4], F32)
    k.ts(okac[:], kac[:], -1.0, 1.0, ALU.mult, ALU.add)
    w2 = const("w2", [64, 512])
    a2 = const("a2", [64, 512])
    g2b = const("g2", [128, 512], BF16)
    rkcol = const("rkcol", [128, 4, 2])
    gnw = const("gn_w_bc", [128, 512])
    gnb = const("gn_b_bc", [128, 512])
    bones = const("blockones", [128, 128], src="c_blockones")
    negblk = const("negblock", [128, 128], src="c_negblock")
    scan64 = const("scan64", [128, 512], src="c_scan64")
    MK1 = const("mk1", [128, 256], src="c_mk1")
    MK2 = const("mk2", [128, 256], src="c_mk2")
    NMT = const("nmt", [128, 128], src="c_nmt")
    identf = cx.identf
    ident = cx.ident

    def f32t(name, shape=(128, 512)):
        return k.sb("r_" + name, list(shape), F32)

    rraw, kraw = f32t("rraw", (128, 513)), f32t("kraw", (128, 513))
    wlraw, alraw, glraw = f32t("wlraw", (64, 513)), f32t("alraw", (64, 513)), f32t("glraw", (128, 513))
    wls, als, gls, twl = f32t("wls", (64, 512)), f32t("als", (64, 512)), f32t("gls", (128, 512)), f32t("twl", (64, 512))
    sgl = k.sb("r_sgl", [128, 512], BF16)
    rs, ks, sg, csg, dd = f32t("rs"), f32t("ks"), f32t("sg"), f32t("csg"), f32t("dd")
    E1, E2, E3, aa = f32t("E1"), f32t("E2"), f32t("E3"), f32t("aa")
    kk, sq, rn, kkn, t1, kmod, beta, rk = f32t("kk"), f32t("sq"), f32t("rn"), f32t("kkn"), f32t("t1"), f32t("kmod"), f32t("beta"), f32t("rk")
    AR = k.sb("r_AR", [128, 4, 2, 128], BF16)
    kbT = k.sb("r_kbT", [128, 512], BF16)
    bbT = k.sb("r_bbT", [128, 512], BF16)
    KG = k.sb("r_KG", [128, 512], BF16)
    BG = k.sb("r_BG", [128, 512], BF16)
    vraw = [f32t(f"vraw{i}") for i in range(2)]
    vprev = [f32t(f"vprev{i}") for i in range(2)]
    vs = [k.sb(f"r_vs{i}", [128, 4, 512], F32) for i in range(2)]
    vb = [k.sb(f"r_vb{i}", [128, 4, 512], BF16) for i in range(2)]
    RZ = [k.sb(f"r_RZ{i}", [128, 4, 2, 128], BF16) for i in range(2)]
    for t_ in RZ:
        k.memset(t_[:], 0.0)
    Y0s = [k.sb(f"r_Y0s{i}", [128, 4, 2, 64], F32) for i in range(2)]
    Gs = [k.sb(f"r_Gs{i}", [128, 8, 64], F32) for i in range(2)]
    MT = [k.sb(f"r_MT{i}", [128, 8, 128], BF16) for i in range(2)]
    Hst = [k.sb(f"r_Hst{i}", [128, 8, 64], BF16) for i in range(2)]
    Hcur = [k.sb(f"r_Hcur{i}", [128, 64], BF16) for i in range(4)]
    for t_ in Hcur:
        k.memset(t_[:], 0.0)
    bsc = [k.sb(f"r_bsc{i}", [128, 4, 2], F32) for i in range(2)]
    tokT = [k.sb(f"r_tokT{i}", [128, 2, 128], BF16) for i in range(2)]
    AZ = [[k.sb(f"r_AZ{i}{h}", [128, 128], BF16) for h in range(2)] for i in range(2)]
    C1 = [k.sb(f"r_C1{h}", [128, 256], BF16) for h in range(2)]
    C2 = [k.sb(f"r_C2{h}", [128, 256], BF16) for h in range(2)]
    Ln = [[k.sb(f"r_Ln{h}{i}", [128, 128], BF16) for i in range(2)] for h in range(2)]
    LTb = [[k.sb(f"r_LT{h}{i}", [128, 128], BF16) for i in range(2)] for h in range(2)]
    QT = [[k.sb(f"r_QT{h}{i}", [128, 128], BF16) for i in range(2)] for h in range(2)]
    WUp = [k.sb(f"r_WU{i}", [128, 2, 2, 64], BF16) for i in range(2)]
    tmpM = k.sb("r_tmpM", [128, 128], F32)
    yt = [k.sb(f"r_yt{i}", [128, 2, 64], F32) for i in range(2)]
    ysq = k.sb("r_ysq", [128, 2, 64], F32)
    st = [k.sb(f"r_st{i}", [128, 8], F32) for i in range(2)]
    yn = [k.sb(f"r_yn{i}", [128, 2, 64], F32) for i in range(2)]
    ob = [k.sb(f"r_ob{i}", [128, 128], BF16) for i in range(2)]
    B = [k.ps(f"r_B{i}", [128, 512], F32) for i in range(7)]
    BT = k.ps("r_BT", [128, 1024], BF16)
    wi = 0
    oi = 0
    lim_tb, lim_hp, lim_stage = getattr(cx, "rw_limit", (T // 512, 4, 99))
    for tb in range(min(T // 512, lim_tb)):
        t00 = tb * 512
        def load_halo(dst, row0, nrows):
            if tb == 0:
                k.memset(dst[0:nrows, 0:1], 0.0)
                k.dma(dst[0:nrows, 1:513], cx.R_fm[row0:row0 + nrows, 0:512])
            else:
                k.dma(dst[0:nrows, :], cx.R_fm[row0:row0 + nrows, t00 - 1:t00 + 512])

        def shift(dst, raw, n, mcol):
            k.ts(dst[0:n, :], raw[0:n, 1:513], omu[0:n, mcol:mcol + 1], None, ALU.mult, eng="pool")
            k.stt(dst[0:n, :], raw[0:n, 0:512], mu[0:n, mcol:mcol + 1], dst[0:n, :], ALU.mult, ALU.add)

        load_halo(wlraw, 512, 64)
        load_halo(alraw, 1088, 64)
        load_halo(glraw, 1152, 128)
        k.ts(wls[:], wlraw[:, 1:513], omu[0:64, 4:5], None, ALU.mult, eng="pool")
        k.stt(wls[:], wlraw[:, 0:512], mu[0:64, 4:5], wls[:], ALU.mult, ALU.add)
        k.ts(als[:], alraw[:, 1:513], omu_al[:, 0:1], None, ALU.mult, eng="pool")
        k.stt(als[:], alraw[:, 0:512], mu_al[:, 0:1], als[:], ALU.mult, ALU.add)
        shift(gls, glraw, 128, 9)
        k.act(twl[:], wls[:], AF.Tanh)
        k.act(sgl[:], gls[:], AF.Sigmoid)
        vsb, vbb = vs[tb % 2], vb[tb % 2]
        for j in range(4):
            t0 = t00 + j * 128
            vr, vp = vraw[j % 2], vprev[j % 2]
            k.dma(vr[:], cx.R_v[t0:t0 + 128, :])
            if t0 == 0:
                k.memset(vp[0:1, :], 0.0)
                k.dma(vp[1:128, :], cx.R_v[0:127, :])
            else:
                k.dma(vp[:], cx.R_v[t0 - 1:t0 + 127, :])
            k.tt(vsb[:, j, :], vr[:], omuv[:], ALU.mult, eng="pool")
            k.tt(vp[:], vp[:], muv[:], ALU.mult, eng="pool")
            k.tt(vsb[:, j, :], vsb[:, j, :], vp[:], ALU.add, eng="pool")
            k.copy(vbb[:, j, :], vsb[:, j, :], eng="act")
        for hp in range(min(4, lim_hp)):
            w = wi % 2
            wi += 1
            rz, y0s, gs, mt, hst, bs = RZ[w], Y0s[w], Gs[w], MT[w], Hst[w], bsc[w]
            load_halo(rraw, hp * 128, 128)
            load_halo(kraw, 576 + hp * 128, 128)
            k.ts(rs[:], rraw[:, 1:513], omu[:, hp:hp + 1], None, ALU.mult)
            k.stt(rs[:], rraw[:, 0:512], mu[:, hp:hp + 1], rs[:], ALU.mult, ALU.add)
            k.ts(ks[:], kraw[:, 1:513], omu_k[:, hp:hp + 1], None, ALU.mult)
            k.stt(ks[:], kraw[:, 0:512], mu_k[:, hp:hp + 1], ks[:], ALU.mult, ALU.add)
            k.mm(B[0][:], w2[:, hp * 128:(hp + 1) * 128], twl[:])
            k.act(sg[:], B[0][:], AF.Sigmoid, bias=w0[:, hp:hp + 1])
            o_, m_, s_ = csg.t[:], scan64.t[:], sg.t[:]
            k.generic("dve", lambda e, o_=o_, m_=m_, s_=s_: e.tensor_tensor_scan(o_, m_, s_, 0.0, ALU.mult, ALU.add), [scan64, sg], [csg])
            k.act(E1[:], csg[:], AF.Exp, scale=-LG)
            k.act(E2[:], csg[:], AF.Exp, scale=LG)
            k.tt(dd[:], csg[:], sg[:], ALU.subtract, eng="pool")
            k.act(E3[:], dd[:], AF.Exp, scale=-LG)
            k.mm(B[0][:], a2[:, hp * 128:(hp + 1) * 128], als[:])
            k.act(aa[:], B[0][:], AF.Sigmoid, bias=a0[:, hp:hp + 1])
            k.ts(kk[:], ks[:], kkc[:, hp:hp + 1], None, ALU.mult)
            k.tt(sq[:], kk[:], kk[:], ALU.mult, eng="pool")
            k.mm(B[0][:], bones[:], sq[:])
            k.ts(rn[:], B[0][:], 1e-24, None, ALU.max)
            k.act(rn[:], rn[:], AF.Ln)
            k.act(rn[:], rn[:], AF.Exp, scale=-0.5)
            k.tt(kkn[:], kk[:], rn[:], ALU.mult)
            k.ts(t1[:], aa[:], kac[:, hp:hp + 1], okac[:, hp:hp + 1], ALU.mult, ALU.add, eng="pool")
            k.tt(kmod[:], ks[:], t1[:], ALU.mult, eng="pool")
            k.tt(beta[:], aa[:], kkn[:], ALU.mult, eng="pool")
            k.tt(AR[:, :, 0, :], kkn[:].rearrange("p (j t) -> p j t", j=4), E3[:].rearrange("p (j t) -> p j t", j=4), ALU.mult)
            k.tt(AR[:, :, 1, :], rs[:].rearrange("p (j t) -> p j t", j=4), E1[:].rearrange("p (j t) -> p j t", j=4), ALU.mult)
            k.tt(kbT[:], kmod[:], E2[:], ALU.mult)
            k.tt(bbT[:], beta[:], E2[:], ALU.mult, eng="pool")
            for c in range(8):
                csl = slice(c * 64, (c + 1) * 64)
                gcol = E1[:, c * 64 + 63:c * 64 + 64]
                k.ts(KG[:, csl], kbT[:, csl], gcol, None, ALU.mult, eng="pool")
                k.ts(BG[:, csl], bbT[:, csl], gcol, None, ALU.mult, eng="pool")
            k.tt(rk[:], rs[:], kmod[:], ALU.mult, eng="pool")
            for j in range(4):
                jsl = slice(j * 128, (j + 1) * 128)
                k.mm(B[0][:, j * 2:j * 2 + 2], rk[:, jsl], rkcol[:, hp, :])
            k.copy(bs[:], B[0][:, 0:8].rearrange("p (j h) -> p j h", j=4), eng="act")
            if lim_stage < 2:
                continue
            for j in range(4):
                jsl = slice(j * 128, (j + 1) * 128)
                tk = tokT[j % 2]
                az = AZ[j % 2]
                wu = WUp[j % 2]
                k.tr(BT[:, 0:128], AR[:, j, 0, :], ident[:])
                k.tr(BT[:, 128:256], KG[:, jsl], ident[:])
                k.tr(BT[:, 256:384], BG[:, jsl], ident[:])
                k.copy(az[0][:, 0:64], BT[:, 0:64], eng="act")
                k.copy(az[1][:, 0:64], BT[:, 64:128], eng="act")
                k.copy(tk[:], BT[:, 128:384].rearrange("p (a b) -> p a b", a=2), eng="act")
                def head_stream(h):
                    hb = h * 64
                    bk = B[1 + h]
                    bi = B[3 + h]
                    vh = vbb[:, j, hp * 128 + h * 64:hp * 128 + (h + 1) * 64]
                    k.mm(bk[:, 0:256], kbT[hb:hb + 64, jsl], AR[hb:hb + 64, j, :, :].rearrange("p a t -> p (a t)"))
                    k.tt(C1[h][:], bk[:, 0:256], MK1[:], ALU.mult)
                    k.mm(bk[:, 256:512], bbT[hb:hb + 64, jsl], AR[hb:hb + 64, j, :, :].rearrange("p a t -> p (a t)"))
                    k.tt(C2[h][:], bk[:, 256:512], MK2[:], ALU.mult)
                    k.mm(bi[:, 0:128], AR[hb:hb + 64, j, 0, :], bbT[hb:hb + 64, jsl])
                    k.tt(Ln[h][0][:], bi[:, 0:128], NMT[:], ALU.mult)
                    yield
                    if lim_stage < 3:
                        return
                    lt = C2[h][:, 0:128]
                    qt = QT[h][0]
                    k.tt(qt[:], lt, ident[:], ALU.add, eng="pool")
                    lp = Ln[h][0][:]
                    lpt = lt
                    for i in range(5):
                        lp2 = Ln[h][(i + 1) % 2]
                        k.mm(bi[:, 128:256], lpt, lp)
                        if i < 4:
                            lpt2 = LTb[h][i % 2]
                            k.mm(bi[:, 256:384], lp, lpt)
                        k.copy(lp2[:], bi[:, 128:256], eng="act")
                        if i < 4:
                            k.copy(lpt2[:], bi[:, 256:384], eng="act")
                        yield
                        qn = QT[h][(i + 1) % 2]
                        k.mm(bi[:, 384:512], lp2[:], qt[:])
                        k.tt(qn[:], bi[:, 384:512], qt[:], ALU.add)
                        yield
                        qt = qn
                        lp = lp2[:]
                        if i < 4:
                            lpt = lpt2[:]
                    if lim_stage < 4:
                        return
                    k.mm(bk[:, 0:64], C1[h][:, 0:128], vh)
                    k.copy(az[h][:, 64:128], bk[:, 0:64], eng="act")
                    yield
                    k.mm(bk[:, 64:192], qt[:], az[h][:])
                    k.copy(wu[:, 0, h, :], bk[:, 64:128], eng="act")
                    k.ts(wu[:, 1, h, :], bk[:, 128:192], -1.0, None, ALU.mult)
                    yield
                    k.mm(bk[:, 192:256], C1[h][:, 128:256], vh, start=True, stop=False)
                    k.mm(bk[:, 192:256], C2[h][:, 128:256], wu[:, 1, h, :], start=False, stop=True)
                    k.copy(y0s[:, j, h, :], bk[:, 192:256], eng="act")

                gens = [head_stream(0), head_stream(1)]
                while gens:
                    for g_ in list(gens):
                        try:
                            next(g_)
                        except StopIteration:
                            gens.remove(g_)
                if lim_stage < 5:
                    continue
                bp = B[5]
                wa = wu[:, 0, :, :].rearrange("p h k -> p (h k)")
                k.mm(bp[:, 0:128], wa, C2[0][:, 128:256])
                k.mm(bp[:, 128:256], wa, C2[1][:, 128:256])
                for h in range(2):
                    hb = h * 64
                    for c in range(2):
                        cs = slice(c * 64, (c + 1) * 64)
                        k.tt(rz[hb:hb + 64, j, c, cs], AR[hb:hb + 64, j, 1, cs], bp[hb:hb + 64, h * 128 + c * 64:h * 128 + (c + 1) * 64], ALU.subtract)
                for c in range(2):
                    cb = c * 64
                    cc = j * 2 + c
                    k.mm(bp[:, 256:384], tk[cb:cb + 64, 0, :], vbb[cb:cb + 64, j, hp * 128:(hp + 1) * 128], start=True, stop=False)
                    k.mm(bp[:, 256:384], tk[cb:cb + 64, 1, :], wu[cb:cb + 64, 1, :, :].rearrange("p h k -> p (h k)"), start=False, stop=True)
                    k.copy(gs[0:64, cc, :], bp[0:64, 256:320], eng="act")
                    k.copy(gs[64:128, cc, :], bp[64:128, 320:384], eng="act")
                    k.mm(bp[:, 384:512], wu[cb:cb + 64, 0, :, :].rearrange("p h k -> p (h k)"), tk[cb:cb + 64, 1, :])
                    k.tt(tmpM[:], bp[:, 384:512], negblk[:], ALU.mult)
                    k.stt(mt[:, cc, :], identf[:], E1[:, cc * 64 + 63:cc * 64 + 64], tmpM[:], ALU.mult, ALU.add)
            if lim_stage < 6:
                continue
            bh = B[6]
            k.copy(hst[:, 0, :], Hcur[hp][:], eng="pool")
            for c in range(8):
                k.mm(bh[:, 0:64], mt[:, c, :], hst[:, c, :])
                if c < 7:
                    k.tt(hst[:, c + 1, :], bh[:, 0:64], gs[:, c, :], ALU.add)
                else:
                    k.tt(Hcur[hp][:], bh[:, 0:64], gs[:, c, :], ALU.add)
            for j in range(4 if lim_stage >= 7 else 0):
                t0 = t00 + j * 128
                jsl = slice(j * 128, (j + 1) * 128)
                y = yt[oi % 2]
                s_ = st[oi % 2]
                yn_ = yn[oi % 2]
                o_b = ob[oi % 2]
                oi += 1
                for h in range(2):
                    hb = h * 64
                    k.mm(bh[:, 64 + h * 64:128 + h * 64], rz[hb:hb + 64, j, 0, :], hst[hb:hb + 64, 2 * j, :], start=True, stop=False)
                    k.mm(bh[:, 64 + h * 64:128 + h * 64], rz[hb:hb + 64, j, 1, :], hst[hb:hb + 64, 2 * j + 1, :], start=False, stop=True)
                k.tt(y[:], bh[:, 64:192].rearrange("p (h v) -> p h v", h=2), y0s[:, j, :, :], ALU.add)
                if lim_stage < 8:
                    continue
                k.reduce(s_[:, 0:2], y[:], ALU.add)
                k.tt(ysq[:], y[:], y[:], ALU.mult, eng="pool")
                k.reduce(s_[:, 2:4], ysq[:], ALU.add)
                k.ts(s_[:, 0:2], s_[:, 0:2], 1.0 / 64, None, ALU.mult)
                k.tt(s_[:, 4:6], s_[:, 0:2], s_[:, 0:2], ALU.mult)
                k.stt(s_[:, 2:4], s_[:, 2:4], 1.0 / 64, s_[:, 4:6], ALU.mult, ALU.subtract)
                k.act(s_[:, 2:4], s_[:, 2:4], AF.Sqrt, bias=GN_EPS)
                k.recip(s_[:, 2:4], s_[:, 2:4])
                if lim_stage < 9:
                    continue
                for h in range(2):
                    k.ts(yn_[:, h, :], y[:, h, :], s_[:, h:h + 1], s_[:, 2 + h:3 + h], ALU.subtract, ALU.mult)
                ynf = yn_[:].rearrange("p h v -> p (h v)")
                k.tt(ynf, ynf, gnw[:, hp * 128:(hp + 1) * 128], ALU.mult, eng="pool")
                k.tt(ynf, ynf, gnb[:, hp * 128:(hp + 1) * 128], ALU.add, eng="pool")
                for h in range(2):
                    k.stt(yn_[:, h, :], vsb[:, j, hp * 128 + h * 64:hp * 128 + (h + 1) * 64], bs[:, j, h:h + 1], yn_[:, h, :], ALU.mult, ALU.add)
                if lim_stage < 10:
                    continue
                k.mm(bh[:, 256:384], sgl[:, jsl], g2b[:, hp * 128:(hp + 1) * 128])
                k.tt(o_b[:], ynf, bh[:, 256:384], ALU.mult)
                k.dma(cx.O_mix[t0:t0 + 128, 512 + hp * 128:512 + (hp + 1) * 128], o_b[:], q="pool")
    k.pop()


NSA_SLOPES = [2.0 ** (-8.0 * (i + 1) / 16) for i in range(16)]


def phase_nsa(k, cx):
    inp = cx.inp
    k.push()

    def cload(name, shape, dtype=F32, parts=None):
        t = k.sb("nc_" + name, shape, F32)
        if parts is None:
            k.dma(t[:], inp[name][:])
        else:
            k.dma(t[parts[0]:parts[1]], inp[name][:])
        if dtype == F32:
            return t
        tb_ = k.sb("ncb_" + name, shape, dtype)
        if parts is None:
            k.copy(tb_[:], t[:])
        else:
            k.copy(tb_[parts[0]:parts[1]], t[parts[0]:parts[1]])
        return tb_

    ident = cx.ident
    diagD = cload("c_diagD", [128, 4, 512])
    winD = cload("c_winD", [128, 4, 512])
    kbias = cload("c_kbias", [128, 16 * 28])
    ov = cload("c_ov", [128, 2, 65], BF16)
    selg = cload("c_selg", [48, 48, 64], BF16)
    w2k = cload("nsa_w2k", [64, 64], BF16)
    w2v = cload("nsa_w2v", [64, 64], BF16)
    peT = cload("nsa_peT", [64, 2, 32], BF16)
    w1 = []
    for i, nm in enumerate(("nsa_w1k", "nsa_w1v")):
        wt = k.sb(f"n_w1_{i}", [64, 32, 64], BF16)
        k.push()
        st = k.sb("n_w1st", [64, 32, 64], F32)
        k.dma(st[:], inp[nm][:].rearrange("(l d) h -> d l h", d=64))
        k.copy(wt[:], st[:])
        k.pop()
        w1.append(wt)
    KA_sel = k.sb("n_KAs", [128, 32, 128], BF16)
    KA_win = k.sb("n_KAw", [128, 32, 128], BF16)
    k.push()
    st = k.sb("n_kaugst", [128, 32, 128], F32)
    k.dma(st[64:128], inp["c_kaug_sel"][:])
    k.copy(KA_sel[64:128], st[64:128])
    k.dma(st[64:128], inp["c_kaug_win"][:])
    k.copy(KA_win[64:128], st[64:128])
    k.pop()
    QA = [[k.sb(f"n_QA{i}{r}", [128, 512], BF16) for r in range(4)] for i in range(2)]
    VA_sel = k.sb("n_VAs", [128, 32, 128], BF16)
    VA_win = k.sb("n_VAw", [128, 32, 128], BF16)
    k.memset(VA_sel[:, :, 64:128], 1.0)
    k.memset(VA_win[:, :, 64:128], 1.0, eng="pool")
    VCA = k.sb("n_VCA", [128, 2, 128], BF16)
    k.memset(VCA[:], 0.0)
    k.memset(VCA[:, :, 64:128], 1.0)
    KC = k.sb("n_KC", [64, 256], BF16)
    XC = [k.sb(f"n_XC{i}", [64, T], BF16) for i in range(2)]
    h1 = [k.sb(f"n_h1{i}", [64, 256], BF16) for i in range(2)]
    cpe = k.sb("n_cpe", [64, 2], F32)
    Ec = k.sb("n_Ec", [128, 4, 2, 512], BF16)
    cD = [k.sb(f"n_cD{i}", [128, 512], F32) for i in range(4)]
    sc = [k.sb(f"n_sc{i}", [128, 512], F32) for i in range(3)]
    Pb = [k.sb(f"n_P{i}", [128, 512], BF16) for i in range(6)]
    gts = k.sb("n_gts", [48, 512], BF16)
    gtr = k.sb("n_gtr", [48, 512], F32)
    rden = [k.sb(f"n_rden{i}", [64, 512], F32) for i in range(2)]
    ff_ = [k.sb(f"n_f{i}", [64, 512], F32) for i in range(2)]
    acc = [k.sb(f"n_acc{i}", [64, 512], F32) for i in range(4)]
    tmpa = [k.sb(f"n_tmpa{i}", [64, 512], F32) for i in range(2)]
    ob = [k.sb(f"n_ob{i}", [64, 512], BF16) for i in range(2)]
    sval = [k.sb(f"n_sval{i}", [128, 64], F32) for i in range(2)]
    sadd = [k.sb(f"n_sadd{i}", [128, 64], F32) for i in range(2)]
    imp = [k.sb(f"n_imp{i}", [128, 64], F32) for i in range(2)]
    rec = [k.sb(f"n_rec{i}", [128, 1], F32) for i in range(4)]
    top8 = [k.sb(f"n_top8{i}", [128, 8], F32) for i in range(2)]
    msk = [k.sb(f"n_msk{i}", [128, 64], F32) for i in range(2)]
    MTk = [k.sb(f"n_MTk{i}", [128, 128], BF16) for i in range(2)]
    for t_ in MTk:
        k.memset(t_[:], 0.0)
    S = [k.ps(f"n_S{i}", [128, 512], F32) for i in range(4)]
    O = [k.ps(f"n_O{i}", [128, 512], F32) for i in range(2)]
    PG = k.ps("n_PG", [128, 512], F32)
    PI = PG
    PT = k.ps("n_PT", [128, 1024], BF16)

    qaug_st = k.sb("n_qaugst", [128, 512], F32)

    for i in range(2):
        for l in range(32):
            k.mm(PG[0:64, i:i + 1], w1[i][:, l, :], peT[:, i, l:l + 1], start=(l == 0), stop=(l == 31))
    k.copy(cpe[:], PG[0:64, 0:2])

    si = 0
    pi_ = 0
    oi = 0
    fi = 0
    for g in range(4):
        k.dma(KA_sel[0:64, :, :], cx.N_ks[g * 64:(g + 1) * 64, :].rearrange("d (kt s) -> d kt s", s=128))
        k.dma(KA_win[0:64, :, :], cx.N_kw[g * 64:(g + 1) * 64, :].rearrange("d (kt s) -> d kt s", s=128))
        k.dma(VA_sel[:, :, 0:64], cx.N_vs[:, g * 64:(g + 1) * 64].rearrange("(kt p) c -> p kt c", p=128))
        k.dma(VA_win[:, :, 0:64], cx.N_vw[:, g * 64:(g + 1) * 64].rearrange("(kt p) c -> p kt c", p=128))
        for r in range(4):
            h = g * 4 + r
            k.dma(qaug_st[64:128, :], inp["c_qaug"][h])
            for i in range(2):
                k.copy(QA[i][r][64:128, :], qaug_st[64:128, :])
        for i in range(2):
            k.dma(XC[i][:], cx.N_c[i * 256 + g * 64:i * 256 + (g + 1) * 64, :])
            for l in range(32):
                k.mm(PG[0:64, 0:255], w1[i][:, l, :], XC[i][:, l:l + 4065:16], start=(l == 0), stop=(l == 31))
            k.act(h1[i][:, 0:255], PG[0:64, 0:255], AF.Gelu, bias=cpe[:, i:i + 1])
        k.mm(PG[0:64, 256:511], w2k[:], h1[0][:, 0:255])
        k.copy(KC[:, 0:255], PG[0:64, 256:511])
        for nt, nn in ((0, 128), (1, 127)):
            k.mm(PI[0:nn, 0:64], h1[1][:, nt * 128:nt * 128 + nn], w2v[:])
            k.copy(VCA[0:nn, nt, 0:64], PI[0:nn, 0:64])
        for qb in range(T // 512):
            qsl = slice(qb * 512, (qb + 1) * 512)
            qa = QA[qb % 2]
            for r in range(4):
                h = g * 4 + r
                k.dma(qa[r][0:64, :], cx.N_q[h * 64:(h + 1) * 64, qsl])
            k.dma(gtr[:], cx.N_gt[:, qsl])
            k.act(gts[:], gtr[:], AF.Sigmoid)
            nts = ((0, 128),) if qb < 4 else ((0, 128), (1, 127))
            cds = {}
            for nt, nn in nts:
                cd = cD[(qb * 2 + nt) % 4]
                k.dma(cd[:], inp["c_cmpD"][(qb if nt == 0 else 8 + qb - 4)])
                cds[nt] = cd

            def finalize(o_ps, h, br, first):
                nonlocal fi
                rd, f_, tm_ = rden[fi % 2], ff_[fi % 2], tmpa[fi % 2]
                fi += 1
                if br == 0:
                    k.ts(rd[:], o_ps[64:128, :], 1e-30, None, ALU.max)
                    k.act(rd[:], rd[:], AF.Ln)
                else:
                    k.act(rd[:], o_ps[64:128, :], AF.Ln)
                k.act(rd[:], rd[:], AF.Exp, scale=-1.0)
                k.mm(PG[0:64, :], selg[:, h * 3 + br, :], gts[:])
                k.tt(f_[:], PG[0:64, :], rd[:], ALU.mult)
                a = acc[h % 4]
                if first:
                    k.tt(a[:], o_ps[0:64, :], f_[:], ALU.mult)
                else:
                    k.tt(tm_[:], o_ps[0:64, :], f_[:], ALU.mult)
                    k.tt(a[:], a[:], tm_[:], ALU.add, eng="pool")

            for r in range(4):
                h = g * 4 + r
                slope = NSA_SLOPES[h]
                o_ps = O[oi % 2]
                oi += 1
                for idx, (nt, nn) in enumerate(nts):
                    s_ps = S[si % 3]
                    s_sb = sc[si % 3]
                    si += 1
                    k.mm(s_ps[0:nn, :], KC[:, nt * 128:nt * 128 + nn], qa[r][0:64, :])
                    k.stt(s_sb[0:nn, :], cds[nt][0:nn, :], slope, s_ps[0:nn, :], ALU.mult, ALU.add)
                    k.act(Ec[0:nn, r, nt, :], s_sb[0:nn, :], AF.Exp)
                for idx, (nt, nn) in enumerate(nts):
                    k.mm(o_ps[:, :], VCA[0:nn, nt, :], Ec[0:nn, r, nt, :], start=(idx == 0), stop=(idx == len(nts) - 1))
                finalize(o_ps, h, 0, True)
            for qt in range(4):
                t0 = qb * 512 + qt * 128
                sv, sa = sval[qt % 2], sadd[qt % 2]
                im, t8, mk, mtk = imp[qt % 2], top8[qt % 2], msk[qt % 2], MTk[qt % 2]
                k.dma(sv[:], inp["c_selvalid"][t0:t0 + 128, :])
                k.dma(sa[:], inp["c_seladd"][t0:t0 + 128, :])
                for r in range(4):
                    rc = rec[r]
                    for idx, (nt, nn) in enumerate(nts):
                        k.mm(PI[:, r * 65:(r + 1) * 65], Ec[0:nn, r, nt, qt * 128:(qt + 1) * 128], ov[0:nn, nt, :], start=(idx == 0), stop=(idx == len(nts) - 1))
                    k.ts(rc[:], PI[:, r * 65 + 64:r * 65 + 65], 1e-30, None, ALU.max)
                    k.recip(rc[:], rc[:])
                    if r == 0:
                        k.ts(im[:], PI[:, 0:64], rc[:, 0:1], None, ALU.mult)
                    else:
                        k.stt(im[:], PI[:, r * 65:r * 65 + 64], rc[:, 0:1], im[:], ALU.mult, ALU.add)
                k.tt(im[:], im[:], sv[:], ALU.mult)
                k.tt(im[:], im[:], sa[:], ALU.add)
                i_, o_ = im.t[:], t8.t[:]
                k.generic("dve", lambda e, i_=i_, o_=o_: e.max(o_, i_), [im], [t8])
                k.ts(mk[:], im[:], t8[:, 7:8], None, ALU.is_ge)
                k.ts(mtk[:, 64:126], mk[:, 1:63], 30000.0, -30000.0, ALU.mult, ALU.add)
                k.tr(PT[:, 0:128], mtk[:], ident[:])
                for r in range(4):
                    k.copy(qa[r][64:126, qt * 128:(qt + 1) * 128], PT[64:126, 0:128], eng=("act" if r % 2 else "dve"))
            def stream(br, r, o_ps):
                nonlocal si, pi_
                KA, VA = (KA_sel, VA_sel) if br == 1 else (KA_win, VA_win)
                h = g * 4 + r
                slope = NSA_SLOPES[h]
                tiles = []
                if br == 1:
                    tiles += [("fast", kt, kt - 4 * qb) for kt in range(4 * qb)]
                elif qb > 0:
                    tiles += [("far", 4 * qb - 4 + j, j) for j in range(4)]
                tiles += [("diag", 4 * qb + j, j) for j in range(4)]
                pend = None
                for idx, (kind, kt, j) in enumerate(tiles):
                    s_ps = S[si % 4]
                    s_sb = sc[si % 3]
                    si += 1
                    p_ = Pb[pi_ % 6]
                    pi_ += 1
                    k.mm(s_ps[:], KA[:, kt, :], qa[r][:, :])
                    if kind == "fast":
                        col = h * 28 + (j + 28)
                        k.act(p_[:], s_ps[:], AF.Exp, bias=kbias[:, col:col + 1])
                    else:
                        dt_ = diagD if kind == "diag" else winD
                        k.stt(s_sb[:], dt_[:, j, :], slope, s_ps[:], ALU.mult, ALU.add)
                        k.act(p_[:], s_sb[:], AF.Exp)
                    yield
                    if pend is not None:
                        k.mm(o_ps[:], VA[:, pend[1], :], pend[0][:], start=(pend[2] == 0), stop=False)
                    pend = (p_, kt, idx)
                k.mm(o_ps[:], VA[:, pend[1], :], pend[0][:], start=(pend[2] == 0), stop=True)
                yield
                finalize(o_ps, h, br, False)

            for r in range(4):
                h = g * 4 + r
                gens = [stream(1, r, O[0]), stream(2, r, O[1])]
                while gens:
                    for g_ in list(gens):
                        try:
                            next(g_)
                        except StopIteration:
                            gens.remove(g_)
                o_b = ob[h % 2]
                k.copy(o_b[:], acc[h % 4][:], eng="act")
                k.dma(cx.O_fm[h * 64:(h + 1) * 64, qsl], o_b[:], q="pool")
    k.pop()


INPUT_SHAPES = {
    "x": [T, D], "c_col": [128, NKC], "ada_w": [2, D, 6 * D], "ada_b": [2, 6 * D],
    "norm1_g": [2, D], "norm2_g": [2, D], "final_norm_g_bc": [128, D],
    "ffn_w_up": [2, D, 2 * FF], "ffn_conv_col": [2, 128, FF // 128, 4], "ffn_w_down": [2, FF, D],
    "ev_w_in": [1, D, EV_COLS], "ev_w_out": [1, D, D], "od_w_in": [1, D, OD_COLS], "od_w_out": [1, D, D],
    "gla_a_up": [16, 256], "gla_a_b_col": [128, 2], "gla_norm_g_bc": [128, 128],
    "c_ident": [128, 128], "c_scan128": [128, 512], "c_tri_incl": [128, 128],
    "rw_mu_col": [128, 10], "rw_mu_k_col": [128, 4], "rw_mu_al_col": [64, 1], "rw_muv_bc": [128, 512],
    "rw_w0_col": [128, 4], "rw_a0_col": [128, 4], "rw_k_k_col": [128, 4], "rw_k_a_col": [128, 4],
    "rw_w2": [64, 512], "rw_a2": [64, 512], "rw_g2": [128, 512], "rw_rkcol": [128, 4, 2],
    "rw_gn_w_bc": [128, 512], "rw_gn_b_bc": [128, 512],
    "c_blockones": [128, 128], "c_negblock": [128, 128], "c_scan64": [128, 512],
    "c_mk1": [128, 256], "c_mk2": [128, 256], "c_nmt": [128, 128],
    "c_diagD": [128, 4, 512], "c_winD": [128, 4, 512], "c_kbias": [128, 16 * 28], "c_ov": [128, 2, 65],
    "c_selg": [48, 48, 64], "nsa_w2k": [64, 64], "nsa_w2v": [64, 64], "nsa_peT": [64, 2, 32],
    "nsa_w1k": [2048, 64], "nsa_w1v": [2048, 64], "c_kaug_sel": [64, 32, 128], "c_kaug_win": [64, 32, 128],
    "c_qaug": [16, 64, 512], "c_cmpD": [12, 128, 512], "c_selvalid": [T, 64], "c_seladd": [T, 64],
}


def build(stop=None, dbg=(), rw_limit=None, skip=()):
    nc = bass.Bass("TRN2", target_bir_lowering=False)
    k = K(nc)
    cx = Ctx()
    if rw_limit is not None:
        cx.rw_limit = rw_limit
    cx.inp = {}
    for name, shape in INPUT_SHAPES.items():
        cx.inp[name] = k.dram(name, shape, F32, kind="ExternalInput")

    def scratch(name, shape, dtype=F32):
        return k.dram(name, shape, dtype, kind=("ExternalOutput" if name in dbg else "Internal"))

    out = k.dram("out", [T, D], F32, kind="ExternalOutput")
    cx.G_qk = scratch("G_qk", [512, T])
    cx.G_lr = scratch("G_lr", [16, T])
    cx.G_v = scratch("G_v", [T, 512], BF16)
    cx.G_og = scratch("G_og", [T, 512])
    cx.R_fm = scratch("R_fm", [1280, T])
    cx.R_v = scratch("R_v", [T, 512])
    cx.O_mix = scratch("O_mix", [T, D], BF16)
    cx.O_fm = scratch("O_fm", [D, T], BF16)
    cx.H_fm = scratch("H_fm", [FF, T], BF16)
    cx.XA = scratch("XA", [T, D])
    cx.XB = scratch("XB", [T, D])
    cx.XC = scratch("XC", [T, D])
    cx.N_q = scratch("N_q", [D, T], BF16)
    cx.N_c = scratch("N_c", [512, T], BF16)
    cx.N_ks = scratch("N_ks", [256, T], BF16)
    cx.N_kw = scratch("N_kw", [256, T], BF16)
    cx.N_gt = scratch("N_gt", [48, T])
    cx.N_vs = scratch("N_vs", [T, 256], BF16)
    cx.N_vw = scratch("N_vw", [T, 256], BF16)
    x = cx.inp["x"]

    def done(stage):
        return stop is not None and stage == stop

    phase_consts(k, cx)
    k.push()
    bc = phase_mod(k, cx, 0)
    if not done("mod0") and "l0" not in skip:
        fm = [(0, 512, cx.G_qk, 0, F32, 1.0), (1536, 16, cx.G_lr, 0, F32, 1.0),
              (1552, 1088, cx.R_fm, 0, F32, 1.0), (1552 + 1600, 192, cx.R_fm, 1088, F32, 1.0)]
        tm = [(512, 512, cx.G_v, 0, BF16), (1024, 512, cx.G_og, 0, F32), (1552 + 1088, 512, cx.R_v, 0, F32)]
        phase_proj(k, cx, x, cx.inp["ev_w_in"][0], EV_COLS, bc["gsc1"], bc["sh1"], fm, tm, "pj0")
    stages = ["mod0", "proj0", "gla", "rwkv", "wout0", "ffn0u", "ffn0d", "proj1", "nsa", "wout1", "ffn1u", "ffn1d"]
    def upto(stage):
        return stop is None or stop not in stages or stages.index(stop) >= stages.index(stage)
    if "l0" in skip:
        stages_l0_off = True
    if upto("gla") and "gla" not in skip and "l0" not in skip:
        phase_gla(k, cx)
    if upto("rwkv") and "l0" not in skip:
        phase_rwkv(k, cx)
    if upto("wout0") and "l0" not in skip:
        phase_wout(k, cx, x, cx.XA, cx.inp["ev_w_out"][0], bc["g1"], cx.O_mix, "tm", "wo0")
    if upto("ffn0u") and "l0" not in skip:
        phase_ffn_up(k, cx, 0, cx.XA, bc)
    if upto("ffn0d") and "l0" not in skip:
        phase_ffn_down(k, cx, 0, cx.XA, cx.XB, bc)
    k.pop()
    if upto("proj1"):
        xin1 = cx.XB if "l0" not in skip else x
        k.push()
        bc = phase_mod(k, cx, 1)
        fm = [(0, 1024, cx.N_q, 0, BF16, 0.125), (1024, 512, cx.N_c, 0, BF16, 1.0), (1536, 256, cx.N_ks, 0, BF16, 1.0),
              (2048, 256, cx.N_kw, 0, BF16, 1.0), (2560, 48, cx.N_gt, 0, F32, 1.0)]
        tm = [(1792, 256, cx.N_vs, 0, BF16), (2304, 256, cx.N_vw, 0, BF16)]
        phase_proj(k, cx, xin1, cx.inp["od_w_in"][0], OD_COLS, bc["gsc1"], bc["sh1"], fm, tm, "pj1")
        if upto("nsa"):
            phase_nsa(k, cx)
        if upto("wout1"):
            phase_wout(k, cx, xin1, cx.XC, cx.inp["od_w_out"][0], bc["g1"], cx.O_fm, "fm", "wo1")
        if upto("ffn1u"):
            phase_ffn_up(k, cx, 1, cx.XC, bc)
        if upto("ffn1d"):
            phase_ffn_down(k, cx, 1, cx.XC, out, bc, final=True)
        k.pop()
    build.stats = {e: len(k.ops[e]) for e in ENGS}
    k.emit()
    return nc


def host_inputs(inputs, b):
    f = np.float32
    m = {}
    m["x"] = np.ascontiguousarray(inputs["x"][b], dtype=f)
    m["c_col"] = np.ascontiguousarray(inputs["c"][b].reshape(NKC, 128).T, dtype=f)
    for nm in ("ada_w", "ada_b", "norm1_g", "norm2_g", "ffn_w_up", "ffn_w_down", "ev_w_in", "ev_w_out", "od_w_in", "od_w_out"):
        m[nm] = np.ascontiguousarray(inputs[nm], dtype=f)
    m["final_norm_g_bc"] = np.ascontiguousarray(np.broadcast_to(inputs["final_norm_g"][None, :], (128, D)), dtype=f)
    cw = np.concatenate([inputs["ffn_conv_w"], inputs["ffn_conv_b"][:, None, :]], axis=1)
    m["ffn_conv_col"] = np.ascontiguousarray(cw.reshape(2, 4, FF // 128, 128).transpose(0, 3, 2, 1), dtype=f)
    m["gla_a_up"] = np.ascontiguousarray(inputs["gla_a_up"][0], dtype=f)
    m["gla_a_b_col"] = np.ascontiguousarray(inputs["gla_a_b"][0].reshape(2, 128).T, dtype=f)
    m["gla_norm_g_bc"] = np.ascontiguousarray(np.broadcast_to(inputs["gla_norm_g"][0][None, :], (128, 128)), dtype=f)
    m["c_ident"] = np.eye(128, dtype=f)
    sc = np.ones((128, 512), f)
    sc[:, 0::128] = 0.0
    m["c_scan128"] = sc
    i = np.arange(128)
    m["c_tri_incl"] = (i[:, None] <= i[None, :]).astype(f)
    smu = inputs["ev_shift_mu"][0]
    mu_fm = np.concatenate([smu[0:1088], smu[1600:1792]])
    m["rw_mu_col"] = np.ascontiguousarray(mu_fm.reshape(10, 128).T, dtype=f)
    m["rw_mu_k_col"] = np.ascontiguousarray(smu[576:1088].reshape(4, 128).T, dtype=f)
    m["rw_mu_al_col"] = np.ascontiguousarray(smu[1600:1664].reshape(64, 1), dtype=f)
    m["rw_muv_bc"] = np.ascontiguousarray(np.broadcast_to(smu[1088:1600][None, :], (128, 512)), dtype=f)
    for nm in ("w0", "a0", "k_k", "k_a"):
        m[f"rw_{nm}_col"] = np.ascontiguousarray(inputs[f"rw_{nm}"][0].reshape(4, 128).T, dtype=f)
    m["rw_w2"] = np.ascontiguousarray(inputs["rw_w2"][0], dtype=f)
    m["rw_a2"] = np.ascontiguousarray(inputs["rw_a2"][0], dtype=f)
    m["rw_g2"] = np.ascontiguousarray(inputs["rw_g2"][0], dtype=f)
    rk = inputs["rw_r_k"][0]
    rkcol = np.zeros((128, 4, 2), f)
    for hp in range(4):
        rkcol[0:64, hp, 0] = rk[2 * hp]
        rkcol[64:128, hp, 1] = rk[2 * hp + 1]
    m["rw_rkcol"] = rkcol
    m["rw_gn_w_bc"] = np.ascontiguousarray(np.broadcast_to(inputs["rw_gn_w"][0][None, :], (128, 512)), dtype=f)
    m["rw_gn_b_bc"] = np.ascontiguousarray(np.broadcast_to(inputs["rw_gn_b"][0][None, :], (128, 512)), dtype=f)
    same = (i[:, None] // 64) == (i[None, :] // 64)
    m["c_blockones"] = same.astype(f)
    m["c_negblock"] = -same.astype(f)
    s64 = np.ones((128, 512), f)
    s64[:, 0::64] = 0.0
    m["c_scan64"] = s64
    mstrict = (same & (i[:, None] < i[None, :])).astype(f)
    mincl = (same & (i[:, None] <= i[None, :])).astype(f)
    m["c_mk1"] = np.concatenate([mstrict, mincl], axis=1)
    m["c_mk2"] = np.concatenate([-mstrict, mincl], axis=1)
    m["c_nmt"] = np.ascontiguousarray(-mstrict.T)
    s_ = np.arange(128)[:, None].astype(np.float64)
    q_ = np.arange(512)[None, :].astype(np.float64)
    NEG = -1.0e6
    dd = np.zeros((128, 4, 512), f)
    wd = np.zeros((128, 4, 512), f)
    for j in range(4):
        dd[:, j, :] = np.where(128 * j + s_ <= q_, 128 * j + s_ - 511.0, NEG)
        wd[:, j, :] = np.where(128 * j + s_ > q_, 128 * j - 512.0 + s_ - 511.0, NEG)
    m["c_diagD"] = dd
    m["c_winD"] = wd
    slopes = np.array(NSA_SLOPES, np.float64)
    kb = np.zeros((128, 16 * 28), f)
    for h in range(16):
        for jj in range(28):
            kb[:, h * 28 + jj] = (slopes[h] * (np.arange(128) + 128.0 * (jj - 28) - 511.0)).astype(f)
    m["c_kbias"] = kb
    n_ = np.arange(256)
    cstart = n_ * 16
    cend = cstart + 31
    sstart = np.arange(64) * 64
    ovl = ((cstart[:, None] <= sstart[None, :] + 63) & (cend[:, None] >= sstart[None, :])).astype(f)
    ovl[255] = 0.0
    ov = np.zeros((128, 2, 65), f)
    ov[:, 0, :64] = ovl[:128]
    ov[:, 1, :64] = ovl[128:]
    ov[:, :, 64] = 1.0
    ov[127, 1, :] = 0.0
    m["c_ov"] = ov
    sg = np.zeros((48, 48, 64), f)
    for i_ in range(48):
        sg[i_, i_, :] = 1.0
    m["c_selg"] = sg
    m["nsa_w2k"] = np.ascontiguousarray(inputs["cmp_w2_k"][0], dtype=f)
    m["nsa_w2v"] = np.ascontiguousarray(inputs["cmp_w2_v"][0], dtype=f)
    m["nsa_peT"] = np.ascontiguousarray(np.stack([inputs["cmp_pe_k"][0].T, inputs["cmp_pe_v"][0].T], axis=1), dtype=f)
    m["nsa_w1k"] = np.ascontiguousarray(inputs["cmp_w1_k"][0], dtype=f)
    m["nsa_w1v"] = np.ascontiguousarray(inputs["cmp_w1_v"][0], dtype=f)
    ka_s = np.zeros((64, 32, 128), f)
    ka_w = np.zeros((64, 32, 128), f)
    for kt in range(32):
        for half in range(2):
            b_ = 2 * kt + half
            if 1 <= b_ <= 62:
                ka_s[b_ - 1, kt, half * 64:(half + 1) * 64] = 1.0
    ka_s[62:64] = 1.0
    ka_w[62:64] = 1.0
    m["c_kaug_sel"] = ka_s
    m["c_kaug_win"] = ka_w
    import ml_dtypes
    qa = np.zeros((16, 64, 512), f)
    for h in range(16):
        rv = slopes[h] * (511.0 - np.arange(512))
        hi = rv.astype(f).astype(ml_dtypes.bfloat16).astype(np.float64)
        lo = (rv - hi).astype(f).astype(ml_dtypes.bfloat16).astype(np.float64)
        qa[h, 62] = hi
        qa[h, 63] = lo
    m["c_qaug"] = qa
    cD = np.zeros((12, 128, 512), f)
    for qb in range(8):
        for nt in range(2):
            if nt == 1 and qb < 4:
                continue
            e_n = 16.0 * (128 * nt + np.arange(128)[:, None]) + 31.0
            t_q = 512.0 * qb + np.arange(512)[None, :]
            cD[qb if nt == 0 else 8 + qb - 4] = np.where(t_q >= e_n, -(t_q - e_n), NEG)
    m["c_cmpD"] = cD
    t_ = np.arange(T)
    ahead = (t_ // 64)[:, None] - np.arange(64)[None, :]
    valid = ahead >= 0
    forced = (np.arange(64)[None, :] == 0) | (valid & (ahead < 2))
    m["c_selvalid"] = valid.astype(f)
    m["c_seladd"] = np.where(valid, np.where(forced, 100.0, 0.0), -100.0).astype(f)
    return m


_CACHE = {}


def kernel(**inputs):
    inputs = {k_: np.asarray(v) for k_, v in inputs.items()}
    if "nc" not in _CACHE:
        _CACHE["nc"] = build()
    nc = _CACHE["nc"]
    B = inputs["x"].shape[0]
    maps = [host_inputs(inputs, b) for b in range(B)]
    zero = dict(maps[0])
    zero["x"] = np.zeros_like(maps[0]["x"])
    in_maps = maps + [zero] * (8 - B)
    res = run_bass_kernel_spmd(nc, in_maps, core_ids=list(range(8)))
    out = np.stack([np.asarray(res.results[b]["out"], dtype=np.float32) for b in range(B)], axis=0)
    return out
```

```python
import contextlib
import numpy as np
import concourse.bass as bass
import concourse.mybir as mybir
from concourse.bass_utils import run_bass_kernel_spmd

F32 = mybir.dt.float32
BF16 = mybir.dt.bfloat16
AF = mybir.ActivationFunctionType
ALU = mybir.AluOpType
AX = mybir.AxisListType

ENGS = ("pe", "act", "dve", "pool", "sp")


class Tl:
    _n = 0

    def __init__(self, t, space, key=None):
        self.t = t
        self.space = space
        Tl._n += 1
        self.key = key if key is not None else ("t", Tl._n)

    def __getitem__(self, idx):
        return V(self, self.t[idx])

    def ap(self):
        return V(self, self.t[:])

    def part(self, sub):
        return Tl(self.t, self.space, key=(self.key, sub))


class V:
    def __init__(self, tile, ap):
        self.tile = tile
        self.ap = ap

    def __getitem__(self, idx):
        return V(self.tile, self.ap[idx])

    def rearrange(self, *a, **kw):
        return V(self.tile, self.ap.rearrange(*a, **kw))

    def bitcast(self, dt):
        return V(self.tile, self.ap.bitcast(dt))

    @property
    def shape(self):
        return self.ap.shape


class Op:
    __slots__ = ("eng", "fn", "waits", "inc", "kind")


class K:
    def __init__(self, nc, n_dma_slots=(("sp", 40), ("pool", 16), ("act", 12)), same_engine_raw=True):
        self.nc = nc
        self.stack = contextlib.ExitStack()
        self.ops = {e: [] for e in ENGS}
        self.count = {e: 0 for e in ENGS}
        self.sem = {}
        for e in ENGS:
            self.sem[e] = self.stack.enter_context(nc.semaphore("s_" + e))
        self.slots = {}
        for q, n in n_dma_slots:
            self.slots[q] = [[self.stack.enter_context(nc.semaphore(f"d_{q}{i}")), 0] for i in range(n)]
        self.slot_rr = {q: 0 for q, _ in n_dma_slots}
        self.waited = {e: {} for e in ENGS}
        self.last_w = {}
        self.readers = {}
        self.same_engine_raw = same_engine_raw
        self.n_ops = 0
        self.stacks = [self.stack]
        self.dram_w = {}
        self.pe_rg = {}
        self.uid = 0

    def push(self):
        self.stacks.append(contextlib.ExitStack())

    def pop(self):
        self.barrier()
        self.stacks.pop().close()

    def sb(self, name, shape, dtype=F32):
        self.uid += 1
        t = self.stacks[-1].enter_context(self.nc.sbuf_tensor(f"{name}_{self.uid}", list(shape), dtype))
        return Tl(t, "sb")

    def ps(self, name, shape, dtype=F32):
        self.uid += 1
        t = self.stacks[-1].enter_context(self.nc.psum_tensor(f"{name}_{self.uid}", list(shape), dtype))
        return Tl(t, "ps")

    def dram(self, name, shape, dtype=F32, kind="Internal"):
        t = self.nc.dram_tensor(name, list(shape), dtype, kind=kind).ap()
        return Tl(t, "dram")

    def _need(self, eng, deps, sem, val, src_eng):
        if sem is None:
            return
        if src_eng == eng and eng == "pe":
            return
        cur = deps.get(id(sem))
        if cur is None or cur[1] < val:
            deps[id(sem)] = (sem, val)

    def _record(self, eng, fn, reads, writes, dma_q=None, rg=None):
        deps = {}
        rkeys = []
        wkeys = []
        for v in reads:
            if v is None:
                continue
            tl = v.tile if isinstance(v, V) else v
            rkeys.append((tl.key, tl.space))
        for v in writes:
            tl = v.tile if isinstance(v, V) else v
            wkeys.append((tl.key, tl.space))
        is_dma = dma_q is not None
        for key, space in rkeys:
            if space == "dram":
                for (sem, val, seng, wdma) in self.dram_w.get(key, []):
                    self._need(eng, deps, sem, val, None)
                continue
            lw = self.last_w.get(key)
            if lw is not None:
                sem, val, seng, wdma = lw
                if seng == eng and not wdma and not is_dma and not self.same_engine_raw and eng != "pe":
                    pass
                else:
                    self._need(eng, deps, sem, val, seng if not (wdma or is_dma) else None)
            if space == "ps":
                for (sem, val, seng, rdma) in self.readers.get(key, []):
                    if seng != eng:
                        self._need(eng, deps, sem, val, seng)
        for key, space in wkeys:
            if rg is not None and space == "ps":
                prev = self.pe_rg.get(key)
                if prev is not None and prev[0] != rg:
                    self._need(eng, deps, prev[1][0], prev[1][1], None)
            lw = self.last_w.get(key) if space != "dram" else None
            if lw is not None:
                sem, val, seng, wdma = lw
                if seng != eng or wdma or is_dma:
                    self._need(eng, deps, sem, val, None if (wdma or is_dma) else seng)
            for (sem, val, seng, rdma) in self.readers.get(key, []):
                if seng != eng or rdma or is_dma:
                    self._need(eng, deps, sem, val, None if (rdma or is_dma) else seng)
        if is_dma:
            sl = self.slots[dma_q]
            i = self.slot_rr[dma_q]
            self.slot_rr[dma_q] = (i + 1) % len(sl)
            sem, uses = sl[i]
            if uses > 0:
                self._need(eng, deps, sem, 16 * uses, None)
            sl[i][1] = uses + 1
            tok = (sem, 16 * (uses + 1), eng, True)
            inc = (sem, 16)
        else:
            self.count[eng] += 1
            tok = (self.sem[eng], self.count[eng], eng, False)
            inc = (self.sem[eng], 1)
        waits = []
        wd = self.waited[eng]
        for sid, (sem, val) in deps.items():
            if wd.get(sid, 0) >= val:
                continue
            wd[sid] = val
            waits.append((sem, val))
        op = Op()
        op.eng = eng
        op.fn = fn
        op.waits = waits
        op.inc = inc
        self.ops[eng].append(op)
        self.n_ops += 1
        for key, space in rkeys:
            lst = self.readers.setdefault(key, [])
            for n_, t_ in enumerate(lst):
                if t_[0] is tok[0]:
                    lst[n_] = tok
                    break
            else:
                lst.append(tok)
        for key, space in wkeys:
            if space == "dram":
                self.dram_w.setdefault(key, []).append(tok)
                continue
            if rg is not None and space == "ps":
                self.pe_rg[key] = (rg, tok)
            self.last_w[key] = tok
            self.readers[key] = []
        return tok

    def barrier(self):
        for e in ENGS:
            waits = []
            wd = self.waited[e]
            for o in ENGS:
                if o == e or self.count[o] == 0:
                    continue
                sem = self.sem[o]
                if wd.get(id(sem), 0) < self.count[o]:
                    wd[id(sem)] = self.count[o]
                    waits.append((sem, self.count[o]))
            for q, sl in self.slots.items():
                for sem, uses in sl:
                    if uses > 0 and wd.get(id(sem), 0) < 16 * uses:
                        wd[id(sem)] = 16 * uses
                        waits.append((sem, 16 * uses))
            if waits:
                op = Op()
                op.eng = e
                op.fn = None
                op.waits = waits
                op.inc = None
                self.ops[e].append(op)
        self.last_w = {}
        self.readers = {}
        self.dram_w = {}

    @staticmethod
    def _a(x):
        return x.ap if isinstance(x, V) else x

    def mm(self, out, lhsT, rhs, start=True, stop=True, **kw):
        o, l, r = out.ap, lhsT.ap, rhs.ap
        rg = (int(l.start_partition()), int(l.shape[0]))
        return self._record("pe", lambda e: e.matmul(o, l, r, start=start, stop=stop, **kw), [lhsT, rhs], [out], rg=rg)

    def tr(self, out, in_, ident):
        o, i, d = out.ap, in_.ap, ident.ap
        rg = (int(i.start_partition()), int(i.shape[0]))
        return self._record("pe", lambda e: e.transpose(o, i, d), [in_, ident], [out], rg=rg)

    def act(self, out, in_, func, bias=None, scale=1.0, accum=None, eng="act"):
        o, i = out.ap, in_.ap
        kw = {}
        reads = [in_]
        if bias is not None:
            kw["bias"] = self._a(bias)
            if isinstance(bias, V):
                reads.append(bias)
        if isinstance(scale, V):
            reads.append(scale)
        kw["scale"] = self._a(scale)
        writes = [out]
        if accum is not None:
            kw["accum_out"] = accum.ap
            writes.append(accum)
        return self._record(eng, lambda e: e.activation(o, i, func, **kw), reads, writes)

    def tt(self, out, a, b, op, eng="dve"):
        o, x, y = out.ap, a.ap, b.ap
        return self._record(eng, lambda e: e.tensor_tensor(o, x, y, op), [a, b], [out])

    def ts(self, out, a, s1, s2=None, op0=ALU.mult, op1=None, eng="dve", accum=None):
        o, x = out.ap, a.ap
        reads = [a]
        for s in (s1, s2):
            if isinstance(s, V):
                reads.append(s)
        a1, a2 = self._a(s1), self._a(s2)
        kw = {}
        writes = [out]
        if accum is not None:
            kw["accum_out"] = accum.ap
            writes.append(accum)
        if op1 is None:
            return self._record(eng, lambda e: e.tensor_scalar(o, x, a1, None, op0, **kw), reads, writes)
        return self._record(eng, lambda e: e.tensor_scalar(o, x, a1, a2, op0, op1, **kw), reads, writes)

    def stt(self, out, a, scalar, b, op0, op1, eng="dve"):
        o, x, y = out.ap, a.ap, b.ap
        reads = [a, b]
        if isinstance(scalar, V):
            reads.append(scalar)
        s = self._a(scalar)
        return self._record(eng, lambda e: e.scalar_tensor_tensor(o, x, s, y, op0, op1), reads, [out])

    def copy(self, out, in_, eng="dve"):
        o, i = out.ap, in_.ap
        if eng == "act":
            return self._record(eng, lambda e: e.copy(o, i), [in_], [out])
        return self._record(eng, lambda e: e.tensor_copy(o, i), [in_], [out])

    def memset(self, out, val, eng="dve"):
        o = out.ap
        return self._record(eng, lambda e: e.memset(o, val), [], [out])

    def reduce(self, out, in_, op, axis=AX.X, eng="dve"):
        o, i = out.ap, in_.ap
        return self._record(eng, lambda e: e.tensor_reduce(o, i, axis, op), [in_], [out])

    def recip(self, out, in_):
        o, i = out.ap, in_.ap
        return self._record("dve", lambda e: e.reciprocal(o, i), [in_], [out])

    def generic(self, eng, fn, reads, writes):
        return self._record(eng, fn, reads, writes)

    def dma(self, out, in_, q="sp", **kw):
        o, i = out.ap, in_.ap
        return self._record(q, lambda e: e.dma_start(o, i, **kw), [in_], [out], dma_q=q)

    def check_deadlock(self):
        ptr = {e: 0 for e in ENGS}
        semv = {}
        progress = True
        while progress:
            progress = False
            for e in ENGS:
                lst = self.ops[e]
                while ptr[e] < len(lst):
                    op = lst[ptr[e]]
                    if all(semv.get(id(sem), 0) >= val for sem, val in op.waits):
                        if op.fn is not None:
                            semv[id(op.inc[0])] = semv.get(id(op.inc[0]), 0) + op.inc[1]
                        ptr[e] += 1
                        progress = True
                    else:
                        break
        stuck = {e: (ptr[e], len(self.ops[e])) for e in ENGS if ptr[e] < len(self.ops[e])}
        if stuck:
            raise RuntimeError(f"semaphore deadlock: {stuck}")

    def emit(self):
        nc = self.nc
        self.barrier()
        self.check_deadlock()
        ops = self.ops
        with nc.Block() as block:
            def run(eng_obj, lst):
                for op in lst:
                    for sem, val in op.waits:
                        eng_obj.wait_ge(sem, val)
                    if op.fn is not None:
                        ins = op.fn(eng_obj)
                        ins.then_inc(op.inc[0], op.inc[1])

            @block.tensor
            def _(e):
                run(e, ops["pe"])

            @block.scalar
            def _(e):
                run(e, ops["act"])

            @block.vector
            def _(e):
                run(e, ops["dve"])

            @block.gpsimd
            def _(e):
                run(e, ops["pool"])

            @block.sync
            def _(e):
                run(e, ops["sp"])
        self.stack.close()

T = 4096
D = 1024
NT = T // 128
EPS = 1e-6
EV_COLS = 3344
OD_COLS = 2608
FF = 2816
NKC = D // 128


class Ctx:
    pass


def _rows(ap2d):
    return ap2d.rearrange("(kc p) n -> p kc n", p=128)


def phase_consts(k, cx):
    cx.ident = k.sb("ident", [128, 128], BF16)
    cx.identf = k.sb("identf", [128, 128], F32)
    k.dma(cx.identf[:], cx.inp["c_ident"][:])
    k.copy(cx.ident[:], cx.identf[:])
    cx.ones_row = k.sb("ones_row", [1, 128], F32)
    k.memset(cx.ones_row[:], 1.0)


def phase_mod(k, cx, layer):
    inp = cx.inp
    bc = {}
    for nm in ("gsc1", "sh1", "g1", "gsc2", "sh2", "g2"):
        bc[nm] = k.sb(f"bc_{nm}", [128, D], F32)
    k.push()
    ccol = k.sb("ccol", [128, NKC], F32)
    k.dma(ccol[:], inp["c_col"][:])
    cond = k.sb("cond", [128, NKC], F32)
    k.act(cond[:], ccol[:], AF.Silu)
    modrow = k.sb("modrow", [1, 6 * D], F32)
    brow = k.sb("brow", [1, 6 * D], F32)
    k.dma(brow[:], inp["ada_b"][layer:layer + 1, :])
    g12 = k.sb("g12", [1, 2 * D], F32)
    k.dma(g12[:, 0:D], inp["norm1_g"][layer:layer + 1, :])
    k.dma(g12[:, D:2 * D], inp["norm2_g"][layer:layer + 1, :])
    wst = [k.sb(f"adast{i}", [128, NKC, 512], F32) for i in range(2)]
    pm = [k.ps(f"pmod{i}", [128, 512], F32) for i in range(2)]
    aw = _rows(inp["ada_w"][layer])
    for blk in range(12):
        w = wst[blk % 2]
        k.dma(w[:], aw[:, :, blk * 512:(blk + 1) * 512])
        p = pm[blk % 2]
        for kc in range(NKC):
            k.mm(p[0:1, :], cond[:, kc:kc + 1], w[:, kc, :], start=(kc == 0), stop=(kc == NKC - 1))
        k.tt(modrow[:, blk * 512:(blk + 1) * 512], p[0:1, :], brow[:, blk * 512:(blk + 1) * 512], ALU.add)
    for which, (sc_off, goff) in enumerate(((1, 0), (4, 1))):
        k.ts(modrow[:, sc_off * D:(sc_off + 1) * D], modrow[:, sc_off * D:(sc_off + 1) * D], 1.0, None, ALU.add)
        k.tt(modrow[:, sc_off * D:(sc_off + 1) * D], modrow[:, sc_off * D:(sc_off + 1) * D], g12[:, goff * D:(goff + 1) * D], ALU.mult)
    order = ("sh1", "gsc1", "g1", "sh2", "gsc2", "g2")
    i = 0
    for j, nm in enumerate(order):
        for half in range(2):
            p = pm[i % 2]
            i += 1
            k.mm(p[:], cx.ones_row[:], modrow[:, j * D + half * 512: j * D + (half + 1) * 512])
            k.copy(bc[nm][:, half * 512:(half + 1) * 512], p[:], eng=("act" if half else "dve"))
    k.pop()
    return bc


def load_weight_bf16(k, w_rows, ncols, name, col0=0):
    nkc = w_rows.shape[1]
    wb = k.sb(name, [128, nkc, ncols], BF16)
    k.push()
    st = [k.sb(f"{name}_st{i}", [128, ncols], F32) for i in range(2)]
    engs = ("dve", "pool", "act")
    for kc in range(nkc):
        s = st[kc % 2]
        k.dma(s[:], w_rows[:, kc, col0:col0 + ncols])
        k.copy(wb[:, kc, :], s[:], eng=engs[kc % 3])
    k.pop()
    return wb


class NormFront:
    def __init__(self, k, cx, gsc, sh):
        self.k, self.cx, self.gsc, self.sh = k, cx, gsc, sh
        self.xt = [k.sb(f"nf_x{i}", [128, D], F32) for i in range(2)]
        self.xm = [k.sb(f"nf_xm{i}", [128, D], F32) for i in range(2)]
        self.xb = [k.sb(f"nf_xb{i}", [128, D], BF16) for i in range(2)]
        self.junk = k.sb("nf_junk", [128, D], BF16)
        self.ss = [k.sb(f"nf_ss{i}", [128, 2], F32) for i in range(2)]
        self.pT = [k.ps(f"nf_pT{i}", [128, D], BF16) for i in range(2)]
        self.i = 0

    def tile(self, x_src, dst):
        k, cx = self.k, self.cx
        i = self.i % 2
        self.i += 1
        xt, xm, xb, ss, pT = self.xt[i], self.xm[i], self.xb[i], self.ss[i], self.pT[i]
        k.dma(xt[:], x_src)
        k.memset(ss[:], 0.0, eng="pool")
        k.act(self.junk[:], xt[:], AF.Square, accum=ss[:, 0:1])
        k.act(ss[:, 1:2], ss[:, 0:1], AF.Sqrt, bias=EPS, scale=1.0 / D)
        k.recip(ss[:, 1:2], ss[:, 1:2])
        k.stt(xm[:], xt[:], ss[:, 1:2], self.gsc[:], ALU.mult, ALU.mult)
        k.tt(xb[:], xm[:], self.sh[:], ALU.add, eng="pool")
        for kc in range(NKC):
            k.tr(pT[:, kc * 128:(kc + 1) * 128], xb[:, kc * 128:(kc + 1) * 128], cx.ident[:])
        k.copy(dst[:, 0:4, :], pT[:, 0:512].rearrange("p (k t) -> p k t", k=4), eng="act")
        k.copy(dst[:, 4:8, :], pT[:, 512:1024].rearrange("p (k t) -> p k t", k=4), eng="dve")
        return xt


class Evac:
    def __init__(self, k, name, shape, dtype, n=3):
        self.k = k
        self.bufs = [k.sb(f"{name}{i}", shape, dtype) for i in range(n)]
        self.i = 0

    def next(self):
        b = self.bufs[self.i % len(self.bufs)]
        self.i += 1
        return b


def phase_proj(k, cx, x_in, w_dram, ncols, bc_gsc, bc_sh, fm_specs, tm_specs, name):
    k.push()
    wb = load_weight_bf16(k, _rows(w_dram), ncols, name + "_wb")
    nf = NormFront(k, cx, bc_gsc, bc_sh)
    xnT = [k.sb(f"{name}_xnT{i}", [128, NKC, 512], BF16) for i in range(2)]
    pmm = [k.ps(f"{name}_pm{i}", [128, 512], F32) for i in range(4)]
    ev32 = Evac(k, name + "_ev32", [128, 512], F32, 3)
    ev16 = Evac(k, name + "_ev16", [128, 512], BF16, 3)
    pi = 0
    ei = 0
    engs = ("act", "dve")
    for tb in range(T // 512):
        xn = xnT[tb % 2]
        for j in range(4):
            t0 = tb * 512 + j * 128
            nf.tile(x_in[t0:t0 + 128, :], xn[:, :, j * 128:(j + 1) * 128])
        for (c0, n, dst, row0, dt, scale) in fm_specs:
            for cc in range(0, n, 128):
                m = min(128, n - cc)
                p = pmm[pi % 4]
                pi += 1
                for kc in range(NKC):
                    k.mm(p[0:m, :], wb[:, kc, c0 + cc:c0 + cc + m], xn[:, kc, :], start=(kc == 0), stop=(kc == NKC - 1))
                e = (ev32 if dt == F32 else ev16).next()
                eng = engs[ei % 2]
                ei += 1
                if eng == "act":
                    k.act(e[0:m, :], p[0:m, :], AF.Copy, scale=scale)
                else:
                    k.ts(e[0:m, :], p[0:m, :], scale, None, ALU.mult)
                k.dma(dst[row0 + cc:row0 + cc + m, tb * 512:(tb + 1) * 512], e[0:m, :], q="pool")
        for (c0, n, dst, col0, dt) in tm_specs:
            for j in range(4):
                t0 = tb * 512 + j * 128
                for cc in range(0, n, 512):
                    m = min(512, n - cc)
                    p = pmm[pi % 4]
                    pi += 1
                    for kc in range(NKC):
                        k.mm(p[:, 0:m], xn[:, kc, j * 128:(j + 1) * 128], wb[:, kc, c0 + cc:c0 + cc + m], start=(kc == 0), stop=(kc == NKC - 1))
                    e = (ev32 if dt == F32 else ev16).next()
                    k.copy(e[:, 0:m], p[:, 0:m], eng=engs[ei % 2])
                    ei += 1
                    k.dma(dst[t0:t0 + 128, col0 + cc:col0 + cc + m], e[:, 0:m], q="pool")
    k.pop()


def phase_gla(k, cx):
    inp = cx.inp
    k.push()
    a_up = k.sb("g_aup", [16, 256], F32)
    k.dma(a_up[:], inp["gla_a_up"][:])
    nab = k.sb("g_nab", [128, 2], F32)
    k.dma(nab[:], inp["gla_a_b_col"][:])
    k.ts(nab[:], nab[:], -1.0, None, ALU.mult)
    gbc = k.sb("g_gbc", [128, 128], F32)
    k.dma(gbc[:], inp["gla_norm_g_bc"][:])
    rmask = k.sb("g_rmask", [128, 512], F32)
    k.dma(rmask[:], inp["c_scan128"][:])
    tri = k.sb("g_tri", [128, 128], F32)
    k.dma(tri[:], inp["c_tri_incl"][:])
    S = [k.sb(f"g_S{i}", [128, 128], F32) for i in range(2)]
    Sbf = [k.sb(f"g_Sbf{i}", [128, 128], BF16) for i in range(2)]
    for i in range(2):
        k.memset(S[i][:], 0.0)
        k.memset(Sbf[i][:], 0.0)
    lrT = [k.sb(f"g_lrT{i}", [16, 512], F32) for i in range(2)]
    qT = [k.sb(f"g_qT{i}", [128, 512], F32) for i in range(2)]
    kT = [k.sb(f"g_kT{i}", [128, 512], F32) for i in range(2)]
    e1 = [k.sb(f"g_e1{i}", [128, 512], F32) for i in range(2)]
    sp = [k.sb(f"g_sp{i}", [128, 512], F32) for i in range(2)]
    bsp = [k.sb(f"g_bsp{i}", [128, 512], F32) for i in range(2)]
    eb = [k.sb(f"g_eb{i}", [128, 512], F32) for i in range(2)]
    ebi = [k.sb(f"g_ebi{i}", [128, 512], F32) for i in range(2)]
    qs = [k.sb(f"g_qs{i}", [128, 512], BF16) for i in range(2)]
    ks = [k.sb(f"g_ks{i}", [128, 512], BF16) for i in range(2)]
    kh = [k.sb(f"g_kh{i}", [128, 128], BF16) for i in range(2)]
    khT = [k.sb(f"g_khT{i}", [128, 128], BF16) for i in range(2)]
    vt = [k.sb(f"g_vt{i}", [128, 256], BF16) for i in range(3)]
    ogt = [k.sb(f"g_ogt{i}", [128, 256], F32) for i in range(3)]
    sil = [k.sb(f"g_sil{i}", [128, 256], F32) for i in range(2)]
    At = [k.sb(f"g_At{i}", [128, 128], BF16) for i in range(3)]
    t1 = [k.sb(f"g_t1{i}", [128, 128], F32) for i in range(2)]
    obuf = [k.sb(f"g_ob{i}", [128, 256], BF16) for i in range(2)]
    junk = k.sb("g_junk", [128, 128], BF16)
    ss = [k.sb(f"g_ss{i}", [128, 2], F32) for i in range(4)]
    pz = k.ps("g_pz", [128, 512], F32)
    pA = [k.ps(f"g_pA{i}", [128, 512], F32) for i in range(2)]
    po = [k.ps(f"g_po{i}", [128, 512], F32) for i in range(2)]
    pkv = k.ps("g_pkv", [128, 512], F32)
    pT = k.ps("g_pT", [128, 1024], BF16)
    ci = 0
    hi = 0
    for tb in range(T // 512):
        tsl = slice(tb * 512, (tb + 1) * 512)
        lr = lrT[tb % 2]
        k.dma(lr[:], cx.G_lr[0:16, tsl])
        for hp in range(2):
            b = (tb * 2 + hp) % 2
            k.dma(qT[b][:], cx.G_qk[hp * 128:(hp + 1) * 128, tsl])
            k.dma(kT[b][:], cx.G_qk[256 + hp * 128:256 + (hp + 1) * 128, tsl])
            k.mm(pz[:], a_up[:, hp * 128:(hp + 1) * 128], lr[:])
            k.act(e1[b][:], pz[:], AF.Exp, bias=nab[:, hp:hp + 1], scale=-1.0)
            k.act(sp[b][:], e1[b][:], AF.Ln, bias=1.0)
            o_, m_, s_ = bsp[b].t[:], rmask.t[:], sp[b].t[:]
            k.generic("dve", lambda e, o_=o_, m_=m_, s_=s_: e.tensor_tensor_scan(o_, m_, s_, 0.0, ALU.mult, ALU.add), [rmask, sp[b]], [bsp[b]])
            k.act(eb[b][:], bsp[b][:], AF.Exp, scale=-1.0 / 16)
            k.act(ebi[b][:], bsp[b][:], AF.Exp, scale=1.0 / 16)
            k.stt(qs[b][:], qT[b][:], 0.125, eb[b][:], ALU.mult, ALU.mult)
            k.tt(ks[b][:], kT[b][:], ebi[b][:], ALU.mult, eng="pool")
            for c in range(4):
                t0 = tb * 512 + c * 128
                cs = slice(c * 128, (c + 1) * 128)
                v = vt[ci % 3]
                og = ogt[ci % 3]
                k.dma(v[:], cx.G_v[t0:t0 + 128, hp * 256:(hp + 1) * 256])
                k.dma(og[:], cx.G_og[t0:t0 + 128, hp * 256:(hp + 1) * 256])
                sl_ = sil[ci % 2]
                k.act(sl_[:], og[:], AF.Silu)
                khc = kh[ci % 2]
                k.ts(khc[:], ks[b][:, cs], eb[b][:, c * 128 + 127:c * 128 + 128], None, ALU.mult)
                k.tr(pT[:, 0:128], khc[:], cx.ident[:])
                kht = khT[ci % 2]
                k.copy(kht[:], pT[:, 0:128], eng="act")
                ob = obuf[ci % 2]
                for hh in range(2):
                    hb = hh * 64
                    a_ps = pA[hi % 2]
                    o_ps = po[hi % 2]
                    at = At[hi % 3]
                    s2 = ss[hi % 4]
                    tt1 = t1[hi % 2]
                    hi += 1
                    k.mm(a_ps[:, 0:128], ks[b][hb:hb + 64, cs], qs[b][hb:hb + 64, cs])
                    k.tt(at[:], a_ps[:, 0:128], tri[:], ALU.mult)
                    k.mm(o_ps[:, 0:128], at[:], v[:, hh * 128:(hh + 1) * 128], start=True, stop=False)
                    k.mm(o_ps[:, 0:128], qs[b][hb:hb + 64, cs], Sbf[hp][hb:hb + 64, :], start=False, stop=True)
                    k.memset(s2[:], 0.0, eng="pool")
                    k.act(junk[:], o_ps[:, 0:128], AF.Square, accum=s2[:, 0:1])
                    k.act(s2[:, 1:2], s2[:, 0:1], AF.Sqrt, bias=EPS, scale=1.0 / 128)
                    k.recip(s2[:, 1:2], s2[:, 1:2])
                    k.stt(tt1[:], o_ps[:, 0:128], s2[:, 1:2], gbc[:], ALU.mult, ALU.mult)
                    k.tt(ob[:, hh * 128:(hh + 1) * 128], tt1[:], sl_[:, hh * 128:(hh + 1) * 128], ALU.mult, eng="pool")
                k.dma(cx.O_mix[t0:t0 + 128, hp * 256:(hp + 1) * 256], ob[:], q="pool")
                k.mm(pkv[:, 0:256], kht[:], v[:])
                ebc = eb[b][:, c * 128 + 127:c * 128 + 128]
                k.stt(S[hp][0:64, :], S[hp][0:64, :], ebc[0:64, :], pkv[0:64, 0:128], ALU.mult, ALU.add)
                k.stt(S[hp][64:128, :], S[hp][64:128, :], ebc[64:128, :], pkv[64:128, 128:256], ALU.mult, ALU.add)
                k.copy(Sbf[hp][:], S[hp][:], eng="act")
                ci += 1
    k.pop()


def phase_wout(k, cx, x_in, x_out, w_dram, g_bc, src, src_mode, name):
    k.push()
    wb = load_weight_bf16(k, _rows(w_dram), D, name + "_wb")
    oT = [k.sb(f"{name}_oT{i}", [128, NKC, 512], BF16) for i in range(2)]
    ot = [k.sb(f"{name}_ot{i}", [128, D], BF16) for i in range(2)]
    xt = [k.sb(f"{name}_x{i}", [128, D], F32) for i in range(2)]
    tmp = [k.sb(f"{name}_tmp{i}", [128, D], F32) for i in range(2)]
    xo = [k.sb(f"{name}_xo{i}", [128, D], F32) for i in range(2)]
    pT = [k.ps(f"{name}_pT{i}", [128, D], BF16) for i in range(2)]
    pm = [k.ps(f"{name}_pm{i}", [128, 512], F32) for i in range(4)]
    ti = 0
    pi = 0
    for tb in range(T // 512):
        o = oT[tb % 2]
        if src_mode == "fm":
            k.dma(o[:], src[:, tb * 512:(tb + 1) * 512].rearrange("(kc p) t -> p kc t", p=128))
        else:
            for j in range(4):
                t0 = tb * 512 + j * 128
                a = ot[(tb * 4 + j) % 2]
                p = pT[(tb * 4 + j) % 2]
                k.dma(a[:], src[t0:t0 + 128, :])
                for kc in range(NKC):
                    k.tr(p[:, kc * 128:(kc + 1) * 128], a[:, kc * 128:(kc + 1) * 128], cx.ident[:])
                k.copy(o[:, 0:4, j * 128:(j + 1) * 128], p[:, 0:512].rearrange("p (k t) -> p k t", k=4), eng="act")
                k.copy(o[:, 4:8, j * 128:(j + 1) * 128], p[:, 512:1024].rearrange("p (k t) -> p k t", k=4), eng="dve")
        for j in range(4):
            t0 = tb * 512 + j * 128
            x = xt[ti % 2]
            tm_ = tmp[ti % 2]
            xo_ = xo[ti % 2]
            ti += 1
            k.dma(x[:], x_in[t0:t0 + 128, :])
            for half in range(2):
                p = pm[pi % 4]
                pi += 1
                for kc in range(NKC):
                    k.mm(p[:], o[:, kc, j * 128:(j + 1) * 128], wb[:, kc, half * 512:(half + 1) * 512], start=(kc == 0), stop=(kc == NKC - 1))
                k.tt(tm_[:, half * 512:(half + 1) * 512], p[:], g_bc[:, half * 512:(half + 1) * 512], ALU.mult)
            k.tt(xo_[:], tm_[:], x[:], ALU.add, eng="pool")
            k.dma(x_out[t0:t0 + 128, :], xo_[:], q="pool")
    k.pop()


def phase_ffn_up(k, cx, layer, x_in, bc):
    inp = cx.inp
    TB = 512
    NF = FF // 128
    k.push()
    wup = load_weight_bf16(k, _rows(inp["ffn_w_up"][layer]), 2 * FF, "f_wup")
    cw = k.sb("f_cw", [128, NF, 4], F32)
    k.dma(cw[:], inp["ffn_conv_col"][layer])
    nf = NormFront(k, cx, bc["gsc2"], bc["sh2"])
    xnT = [k.sb(f"f_xnT{i}", [128, NKC, TB], BF16) for i in range(2)]
    halo = k.sb("f_halo", [128, NF, 2], F32)
    k.memset(halo[:], 0.0)
    ub = [k.sb(f"f_ub{i}", [128, TB + 2], F32) for i in range(3)]
    cv = [k.sb(f"f_cv{i}", [128, TB], F32) for i in range(3)]
    ge = [k.sb(f"f_ge{i}", [128, TB], F32) for i in range(3)]
    hb = [k.sb(f"f_hb{i}", [128, TB], BF16) for i in range(3)]
    pu = [k.ps(f"f_pu{i}", [128, 512], F32) for i in range(3)]
    pv = [k.ps(f"f_pv{i}", [128, 512], F32) for i in range(3)]
    ui = 0
    for tb in range(T // TB):
        xn = xnT[tb % 2]
        for j in range(TB // 128):
            t0 = tb * TB + j * 128
            nf.tile(x_in[t0:t0 + 128, :], xn[:, :, j * 128:(j + 1) * 128])
        pend = None

        def tail(st):
            fc, u, c_, g_, h_, p_v = st
            k.ts(c_[:], u[:, 2:TB + 2], cw[:, fc, 2:3], cw[:, fc, 3:4], ALU.mult, ALU.add, eng="pool")
            k.stt(c_[:], u[:, 1:TB + 1], cw[:, fc, 1:2], c_[:], ALU.mult, ALU.add)
            k.stt(c_[:], u[:, 0:TB], cw[:, fc, 0:1], c_[:], ALU.mult, ALU.add)
            k.act(g_[:], c_[:], AF.Gelu)
            k.tt(h_[:], g_[:], p_v[:, 0:TB], ALU.mult)
            k.dma(cx.H_fm[fc * 128:(fc + 1) * 128, tb * TB:(tb + 1) * TB], h_[:], q="pool")

        for fc in range(NF):
            p_u = pu[ui % 3]
            p_v = pv[ui % 3]
            u = ub[ui % 3]
            c_ = cv[ui % 3]
            g_ = ge[ui % 3]
            h_ = hb[ui % 3]
            ui += 1
            for kc in range(NKC):
                k.mm(p_u[:, 0:TB], wup[:, kc, fc * 128:(fc + 1) * 128], xn[:, kc, :], start=(kc == 0), stop=(kc == NKC - 1))
            for kc in range(NKC):
                k.mm(p_v[:, 0:TB], wup[:, kc, FF + fc * 128:FF + (fc + 1) * 128], xn[:, kc, :], start=(kc == 0), stop=(kc == NKC - 1))
            k.copy(u[:, 0:2], halo[:, fc, :], eng="pool")
            k.copy(u[:, 2:TB + 2], p_u[:, 0:TB], eng="act")
            k.copy(halo[:, fc, :], u[:, TB:TB + 2], eng="pool")
            if pend is not None:
                tail(pend)
            pend = (fc, u, c_, g_, h_, p_v)
        tail(pend)
    k.pop()


def phase_ffn_down(k, cx, layer, x_in, x_out, bc, final=False):
    inp = cx.inp
    TB = 512
    NF = FF // 128
    k.push()
    wdn = load_weight_bf16(k, _rows(inp["ffn_w_down"][layer]), D, "f_wdn")
    hT = [k.sb(f"f_hT{i}", [128, NF, TB], BF16) for i in range(2)]
    pd = [k.ps(f"f_pd{i}", [128, 512], F32) for i in range(4)]
    xt = [k.sb(f"f_x{i}", [128, D], F32) for i in range(2)]
    tmp = [k.sb(f"f_tmp{i}", [128, D], F32) for i in range(2)]
    if final:
        fg = k.sb("f_fg", [128, D], F32)
        k.dma(fg[:], inp["final_norm_g_bc"][:])
        fss = [k.sb(f"f_fss{i}", [128, 2], F32) for i in range(2)]
        fjunk = k.sb("f_fjunk", [128, D], BF16)
    ti = 0
    di = 0
    for tb in range(T // TB):
        h = hT[tb % 2]
        k.dma(h[:], cx.H_fm[:, tb * TB:(tb + 1) * TB].rearrange("(fc p) t -> p fc t", p=128))
        for j in range(TB // 128):
            t0 = tb * TB + j * 128
            x = xt[ti % 2]
            tm_ = tmp[ti % 2]
            k.dma(x[:], x_in[t0:t0 + 128, :])
            for half in range(2):
                p = pd[di % 4]
                di += 1
                for fc in range(NF):
                    k.mm(p[:], h[:, fc, j * 128:(j + 1) * 128], wdn[:, fc, half * 512:(half + 1) * 512], start=(fc == 0), stop=(fc == NF - 1))
                k.tt(tm_[:, half * 512:(half + 1) * 512], p[:], bc["g2"][:, half * 512:(half + 1) * 512], ALU.mult)
            k.tt(x[:], tm_[:], x[:], ALU.add, eng="pool")
            if not final:
                k.dma(x_out[t0:t0 + 128, :], x[:], q="pool")
            else:
                s2 = fss[ti % 2]
                k.memset(s2[:], 0.0, eng="pool")
                k.act(fjunk[:], x[:], AF.Square, accum=s2[:, 0:1])
                k.act(s2[:, 1:2], s2[:, 0:1], AF.Sqrt, bias=EPS, scale=1.0 / D)
                k.recip(s2[:, 1:2], s2[:, 1:2])
                k.stt(tm_[:], x[:], s2[:, 1:2], fg[:], ALU.mult, ALU.mult)
                k.dma(x_out[t0:t0 + 128, :], tm_[:], q="pool")
            ti += 1
    k.pop()


def phase_rwkv(k, cx):
    inp = cx.inp
    LG = 0.6065306597126334
    GN_EPS = 64e-5
    k.push()

    def const(name, shape, dtype=F32, src=None):
        t = k.sb("rc_" + name, shape, F32)
        k.dma(t[:], inp[src or ("rw_" + name)][:])
        if dtype == F32:
            return t
        tb_ = k.sb("rcb_" + name, shape, dtype)
        k.copy(tb_[:], t[:])
        return tb_

    mu = const("mu_col", [128, 10])
    omu = k.sb("rc_omu", [128, 10], F32)
    k.ts(omu[:], mu[:], -1.0, 1.0, ALU.mult, ALU.add)
    mu_k = const("mu_k_col", [128, 4])
    omu_k = k.sb("rc_omu_k", [128, 4], F32)
    k.ts(omu_k[:], mu_k[:], -1.0, 1.0, ALU.mult, ALU.add)
    mu_al = const("mu_al_col", [64, 1])
    omu_al = k.sb("rc_omu_al", [64, 1], F32)
    k.ts(omu_al[:], mu_al[:], -1.0, 1.0, ALU.mult, ALU.add)
    muv = const("muv_bc", [128, 512])
    omuv = k.sb("rc_omuv", [128, 512], F32)
    k.ts(omuv[:], muv[:], -1.0, 1.0, ALU.mult, ALU.add)
    w0 = const("w0_col", [128, 4])
    a0 = const("a0_col", [128, 4])
    kkc = const("k_k_col", [128, 4])
    kac = const("k_a_col", [128, 4])
    okac = k.sb("rc_okac", [128, 4], F32)
    k.ts(okac[:], kac[:], -1.0, 1.0, ALU.mult, ALU.add)
    w2 = const("w2", [64, 512])
    a2 = const("a2", [64, 512])
    g2b = const("g2", [128, 512], BF16)
    rkcol = const("rkcol", [128, 4, 2])
    gnw = const("gn_w_bc", [128, 512])
    gnb = const("gn_b_bc", [128, 512])
    bones = const("blockones", [128, 128], src="c_blockones")
    negblk = const("negblock", [128, 128], src="c_negblock")
    scan64 = const("scan64", [128, 512], src="c_scan64")
    MK1 = const("mk1", [128, 256], src="c_mk1")
    MK2 = const("mk2", [128, 256], src="c_mk2")
    NMT = const("nmt", [128, 128], src="c_nmt")
    identf = cx.identf
    ident = cx.ident

    def f32t(name, shape=(128, 512)):
        return k.sb("r_" + name, list(shape), F32)

    rraw, kraw = f32t("rraw", (128, 513)), f32t("kraw", (128, 513))
    wlraw, alraw, glraw = f32t("wlraw", (64, 513)), f32t("alraw", (64, 513)), f32t("glraw", (128, 513))
    wls, als, gls, twl = f32t("wls", (64, 512)), f32t("als", (64, 512)), f32t("gls", (128, 512)), f32t("twl", (64, 512))
    sgls = [k.sb(f"r_sgl{i}", [128, 512], BF16) for i in range(2)]
    rs, ks, sg, csg, dd = f32t("rs"), f32t("ks"), f32t("sg"), f32t("csg"), f32t("dd")
    E1s = [f32t("E1a"), f32t("E1b")]
    E2, E3, aa = f32t("E2"), f32t("E3"), f32t("aa")
    kk, sq, rn, kkn, t1, kmod, beta, rk = f32t("kk"), f32t("sq"), f32t("rn"), f32t("kkn"), f32t("t1"), f32t("kmod"), f32t("beta"), f32t("rk")
    ARs = [k.sb(f"r_AR{i}", [128, 4, 2, 128], BF16) for i in range(2)]
    kbTs = [k.sb(f"r_kbT{i}", [128, 512], BF16) for i in range(2)]
    bbTs = [k.sb(f"r_bbT{i}", [128, 512], BF16) for i in range(2)]
    KGs = [k.sb(f"r_KG{i}", [128, 512], BF16) for i in range(2)]
    BGs = [k.sb(f"r_BG{i}", [128, 512], BF16) for i in range(2)]
    vraw = [f32t(f"vraw{i}") for i in range(2)]
    vprev = [f32t(f"vprev{i}") for i in range(2)]
    vs = [k.sb(f"r_vs{i}", [128, 4, 512], F32) for i in range(2)]
    vb = [k.sb(f"r_vb{i}", [128, 4, 512], BF16) for i in range(2)]
    RZ = [k.sb(f"r_RZ{i}", [128, 4, 2, 128], BF16) for i in range(2)]
    for t_ in RZ:
        k.memset(t_[:], 0.0)
    Y0s = [k.sb(f"r_Y0s{i}", [128, 4, 2, 64], F32) for i in range(2)]
    Gs = [k.sb(f"r_Gs{i}", [128, 8, 64], F32) for i in range(2)]
    MT = [k.sb(f"r_MT{i}", [128, 8, 128], BF16) for i in range(2)]
    Hst = [k.sb(f"r_Hst{i}", [128, 8, 64], BF16) for i in range(2)]
    Hcur = [k.sb(f"r_Hcur{i}", [128, 64], BF16) for i in range(4)]
    for t_ in Hcur:
        k.memset(t_[:], 0.0)
    bsc = [k.sb(f"r_bsc{i}", [128, 4, 2], F32) for i in range(2)]
    tokT = [k.sb(f"r_tokT{i}", [128, 2, 128], BF16) for i in range(2)]
    AZ = [[k.sb(f"r_AZ{i}{h}", [128, 128], BF16) for h in range(2)] for i in range(2)]
    C1 = [k.sb(f"r_C1{h}", [128, 256], BF16) for h in range(2)]
    C2 = [k.sb(f"r_C2{h}", [128, 256], BF16) for h in range(2)]
    Ln = [[k.sb(f"r_Ln{h}{i}", [128, 128], BF16) for i in range(2)] for h in range(2)]
    LTb = [[k.sb(f"r_LT{h}{i}", [128, 128], BF16) for i in range(2)] for h in range(2)]
    QT = [[k.sb(f"r_QT{h}{i}", [128, 128], BF16) for i in range(2)] for h in range(2)]
    WUp = [k.sb(f"r_WU{i}", [128, 2, 2, 64], BF16) for i in range(2)]
    tmpM = k.sb("r_tmpM", [128, 128], F32)
    yt = [k.sb(f"r_yt{i}", [128, 2, 64], F32) for i in range(2)]
    ysq = k.sb("r_ysq", [128, 2, 64], F32)
    st = [k.sb(f"r_st{i}", [128, 8], F32) for i in range(2)]
    yn = [k.sb(f"r_yn{i}", [128, 2, 64], F32) for i in range(2)]
    ob = [k.sb(f"r_ob{i}", [128, 128], BF16) for i in range(2)]
    B = [k.ps(f"r_B{i}", [128, 512], F32) for i in range(7)]
    BT = k.ps("r_BT", [128, 1024], BF16)
    oi = 0
    lim_tb, lim_hp, lim_stage = getattr(cx, "rw_limit", (T // 512, 4, 99))

    def shared_gen(tb):
        sgl = sgls[tb % 2]
        t00 = tb * 512
        def load_halo(dst, row0, nrows):
            if tb == 0:
                k.memset(dst[0:nrows, 0:1], 0.0)
                k.dma(dst[0:nrows, 1:513], cx.R_fm[row0:row0 + nrows, 0:512])
            else:
                k.dma(dst[0:nrows, :], cx.R_fm[row0:row0 + nrows, t00 - 1:t00 + 512])

        def shift(dst, raw, n, mcol):
            k.ts(dst[0:n, :], raw[0:n, 1:513], omu[0:n, mcol:mcol + 1], None, ALU.mult, eng="pool")
            k.stt(dst[0:n, :], raw[0:n, 0:512], mu[0:n, mcol:mcol + 1], dst[0:n, :], ALU.mult, ALU.add)

        load_halo(wlraw, 512, 64)
        load_halo(alraw, 1088, 64)
        load_halo(glraw, 1152, 128)
        k.ts(wls[:], wlraw[:, 1:513], omu[0:64, 4:5], None, ALU.mult, eng="pool")
        k.stt(wls[:], wlraw[:, 0:512], mu[0:64, 4:5], wls[:], ALU.mult, ALU.add)
        k.ts(als[:], alraw[:, 1:513], omu_al[:, 0:1], None, ALU.mult, eng="pool")
        k.stt(als[:], alraw[:, 0:512], mu_al[:, 0:1], als[:], ALU.mult, ALU.add)
        shift(gls, glraw, 128, 9)
        k.act(twl[:], wls[:], AF.Tanh)
        yield
        k.act(sgl[:], gls[:], AF.Sigmoid)
        yield
        vsb, vbb = vs[tb % 2], vb[tb % 2]
        for j in range(4):
            t0 = t00 + j * 128
            vr, vp = vraw[j % 2], vprev[j % 2]
            k.dma(vr[:], cx.R_v[t0:t0 + 128, :])
            if t0 == 0:
                k.memset(vp[0:1, :], 0.0)
                k.dma(vp[1:128, :], cx.R_v[0:127, :])
            else:
                k.dma(vp[:], cx.R_v[t0 - 1:t0 + 127, :])
            k.tt(vsb[:, j, :], vr[:], omuv[:], ALU.mult, eng="pool")
            k.tt(vp[:], vp[:], muv[:], ALU.mult, eng="pool")
            k.tt(vsb[:, j, :], vsb[:, j, :], vp[:], ALU.add, eng="pool")
            k.copy(vbb[:, j, :], vsb[:, j, :], eng="act")
            yield

    def pre_gen(tb, hp):
        t00 = tb * 512

        def load_halo(dst, row0, nrows):
            if tb == 0:
                k.memset(dst[0:nrows, 0:1], 0.0)
                k.dma(dst[0:nrows, 1:513], cx.R_fm[row0:row0 + nrows, 0:512])
            else:
                k.dma(dst[0:nrows, :], cx.R_fm[row0:row0 + nrows, t00 - 1:t00 + 512])
        w = (tb * 4 + hp) % 2
        rz, y0s, gs, mt, hst, bs = RZ[w], Y0s[w], Gs[w], MT[w], Hst[w], bsc[w]
        AR, kbT, bbT, KG, BG, E1 = ARs[w], kbTs[w], bbTs[w], KGs[w], BGs[w], E1s[w]
        sgl = sgls[tb % 2]
        vsb, vbb = vs[tb % 2], vb[tb % 2]
        load_halo(rraw, hp * 128, 128)
        load_halo(kraw, 576 + hp * 128, 128)
        k.ts(rs[:], rraw[:, 1:513], omu[:, hp:hp + 1], None, ALU.mult)
        k.stt(rs[:], rraw[:, 0:512], mu[:, hp:hp + 1], rs[:], ALU.mult, ALU.add)
        k.ts(ks[:], kraw[:, 1:513], omu_k[:, hp:hp + 1], None, ALU.mult)
        k.stt(ks[:], kraw[:, 0:512], mu_k[:, hp:hp + 1], ks[:], ALU.mult, ALU.add)
        yield
        k.mm(B[0][:], w2[:, hp * 128:(hp + 1) * 128], twl[:])
        k.act(sg[:], B[0][:], AF.Sigmoid, bias=w0[:, hp:hp + 1])
        o_, m_, s_ = csg.t[:], scan64.t[:], sg.t[:]
        k.generic("dve", lambda e, o_=o_, m_=m_, s_=s_: e.tensor_tensor_scan(o_, m_, s_, 0.0, ALU.mult, ALU.add), [scan64, sg], [csg])
        k.act(E1[:], csg[:], AF.Exp, scale=-LG)
        k.act(E2[:], csg[:], AF.Exp, scale=LG)
        yield
        k.tt(dd[:], csg[:], sg[:], ALU.subtract, eng="pool")
        k.act(E3[:], dd[:], AF.Exp, scale=-LG)
        k.mm(B[0][:], a2[:, hp * 128:(hp + 1) * 128], als[:])
        k.act(aa[:], B[0][:], AF.Sigmoid, bias=a0[:, hp:hp + 1])
        yield
        k.ts(kk[:], ks[:], kkc[:, hp:hp + 1], None, ALU.mult)
        k.tt(sq[:], kk[:], kk[:], ALU.mult, eng="pool")
        k.mm(B[0][:], bones[:], sq[:])
        k.ts(rn[:], B[0][:], 1e-24, None, ALU.max)
        yield
        k.act(rn[:], rn[:], AF.Ln)
        k.act(rn[:], rn[:], AF.Exp, scale=-0.5)
        k.tt(kkn[:], kk[:], rn[:], ALU.mult)
        k.ts(t1[:], aa[:], kac[:, hp:hp + 1], okac[:, hp:hp + 1], ALU.mult, ALU.add, eng="pool")
        yield
        k.tt(kmod[:], ks[:], t1[:], ALU.mult, eng="pool")
        k.tt(beta[:], aa[:], kkn[:], ALU.mult, eng="pool")
        k.tt(AR[:, :, 0, :], kkn[:].rearrange("p (j t) -> p j t", j=4), E3[:].rearrange("p (j t) -> p j t", j=4), ALU.mult)
        k.tt(AR[:, :, 1, :], rs[:].rearrange("p (j t) -> p j t", j=4), E1[:].rearrange("p (j t) -> p j t", j=4), ALU.mult)
        yield
        k.tt(kbT[:], kmod[:], E2[:], ALU.mult)
        k.tt(bbT[:], beta[:], E2[:], ALU.mult, eng="pool")
        for c in range(8):
            csl = slice(c * 64, (c + 1) * 64)
            gcol = E1[:, c * 64 + 63:c * 64 + 64]
            k.ts(KG[:, csl], kbT[:, csl], gcol, None, ALU.mult, eng="pool")
            k.ts(BG[:, csl], bbT[:, csl], gcol, None, ALU.mult, eng="pool")
        k.tt(rk[:], rs[:], kmod[:], ALU.mult, eng="pool")
        for j in range(4):
            jsl = slice(j * 128, (j + 1) * 128)
            k.mm(B[0][:, j * 2:j * 2 + 2], rk[:, jsl], rkcol[:, hp, :])
        k.copy(bs[:], B[0][:, 0:8].rearrange("p (j h) -> p j h", j=4), eng="act")
        yield
        yield

    def main_gen(tb, hp):
        nonlocal oi
        t00 = tb * 512
        w = (tb * 4 + hp) % 2
        rz, y0s, gs, mt, hst, bs = RZ[w], Y0s[w], Gs[w], MT[w], Hst[w], bsc[w]
        AR, kbT, bbT, KG, BG, E1 = ARs[w], kbTs[w], bbTs[w], KGs[w], BGs[w], E1s[w]
        sgl = sgls[tb % 2]
        vsb, vbb = vs[tb % 2], vb[tb % 2]
        for j in range(4):
            jsl = slice(j * 128, (j + 1) * 128)
            tk = tokT[j % 2]
            az = AZ[j % 2]
            wu = WUp[j % 2]
            k.tr(BT[:, 0:128], AR[:, j, 0, :], ident[:])
            k.tr(BT[:, 128:256], KG[:, jsl], ident[:])
            k.tr(BT[:, 256:384], BG[:, jsl], ident[:])
            k.copy(az[0][:, 0:64], BT[:, 0:64], eng="act")
            k.copy(az[1][:, 0:64], BT[:, 64:128], eng="act")
            k.copy(tk[:], BT[:, 128:384].rearrange("p (a b) -> p a b", a=2), eng="act")
            def head_stream(h):
                hb = h * 64
                bk = B[1 + h]
                bi = B[3 + h]
                vh = vbb[:, j, hp * 128 + h * 64:hp * 128 + (h + 1) * 64]
                k.mm(bk[:, 0:256], kbT[hb:hb + 64, jsl], AR[hb:hb + 64, j, :, :].rearrange("p a t -> p (a t)"))
                k.tt(C1[h][:], bk[:, 0:256], MK1[:], ALU.mult)
                k.mm(bk[:, 256:512], bbT[hb:hb + 64, jsl], AR[hb:hb + 64, j, :, :].rearrange("p a t -> p (a t)"))
                k.tt(C2[h][:], bk[:, 256:512], MK2[:], ALU.mult)
                k.mm(bi[:, 0:128], AR[hb:hb + 64, j, 0, :], bbT[hb:hb + 64, jsl])
                k.tt(Ln[h][0][:], bi[:, 0:128], NMT[:], ALU.mult)
                yield
                if lim_stage < 3:
                    return
                lt = C2[h][:, 0:128]
                qt = QT[h][0]
                k.tt(qt[:], lt, ident[:], ALU.add, eng="pool")
                lp = Ln[h][0][:]
                lpt = lt
                for i in range(5):
                    lp2 = Ln[h][(i + 1) % 2]
                    k.mm(bi[:, 128:256], lpt, lp)
                    if i < 4:
                        lpt2 = LTb[h][i % 2]
                        k.mm(bi[:, 256:384], lp, lpt)
                    k.copy(lp2[:], bi[:, 128:256], eng="act")
                    if i < 4:
                        k.copy(lpt2[:], bi[:, 256:384], eng="act")
                    yield
                    qn = QT[h][(i + 1) % 2]
                    k.mm(bi[:, 384:512], lp2[:], qt[:])
                    k.tt(qn[:], bi[:, 384:512], qt[:], ALU.add)
                    yield
                    qt = qn
                    lp = lp2[:]
                    if i < 4:
                        lpt = lpt2[:]
                if lim_stage < 4:
                    return
                k.mm(bk[:, 0:64], C1[h][:, 0:128], vh)
                k.copy(az[h][:, 64:128], bk[:, 0:64], eng="act")
                yield
                k.mm(bk[:, 64:192], qt[:], az[h][:])
                k.copy(wu[:, 0, h, :], bk[:, 64:128], eng="act")
                k.ts(wu[:, 1, h, :], bk[:, 128:192], -1.0, None, ALU.mult)
                yield
                k.mm(bk[:, 192:256], C1[h][:, 128:256], vh, start=True, stop=False)
                k.mm(bk[:, 192:256], C2[h][:, 128:256], wu[:, 1, h, :], start=False, stop=True)
                k.copy(y0s[:, j, h, :], bk[:, 192:256], eng="act")

            gens = [head_stream(0), head_stream(1)]
            while gens:
                for g_ in list(gens):
                    try:
                        next(g_)
                    except StopIteration:
                        gens.remove(g_)
                yield
            if lim_stage < 5:
                continue
            bp = B[5]
            wa = wu[:, 0, :, :].rearrange("p h k -> p (h k)")
            k.mm(bp[:, 0:128], wa, C2[0][:, 128:256])
            k.mm(bp[:, 128:256], wa, C2[1][:, 128:256])
            for h in range(2):
                hb = h * 64
                for c in range(2):
                    cs = slice(c * 64, (c + 1) * 64)
                    k.tt(rz[hb:hb + 64, j, c, cs], AR[hb:hb + 64, j, 1, cs], bp[hb:hb + 64, h * 128 + c * 64:h * 128 + (c + 1) * 64], ALU.subtract)
            for c in range(2):
                cb = c * 64
                cc = j * 2 + c
                k.mm(bp[:, 256:384], tk[cb:cb + 64, 0, :], vbb[cb:cb + 64, j, hp * 128:(hp + 1) * 128], start=True, stop=False)
                k.mm(bp[:, 256:384], tk[cb:cb + 64, 1, :], wu[cb:cb + 64, 1, :, :].rearrange("p h k -> p (h k)"), start=False, stop=True)
                k.copy(gs[0:64, cc, :], bp[0:64, 256:320], eng="act")
                k.copy(gs[64:128, cc, :], bp[64:128, 320:384], eng="act")
                k.mm(bp[:, 384:512], wu[cb:cb + 64, 0, :, :].rearrange("p h k -> p (h k)"), tk[cb:cb + 64, 1, :])
                k.tt(tmpM[:], bp[:, 384:512], negblk[:], ALU.mult)
                k.stt(mt[:, cc, :], identf[:], E1[:, cc * 64 + 63:cc * 64 + 64], tmpM[:], ALU.mult, ALU.add)
        if lim_stage < 6:
            return
        bh = B[6]
        k.copy(hst[:, 0, :], Hcur[hp][:], eng="pool")
        for c in range(8):
            k.mm(bh[:, 0:64], mt[:, c, :], hst[:, c, :])
            if c < 7:
                k.tt(hst[:, c + 1, :], bh[:, 0:64], gs[:, c, :], ALU.add)
            else:
                k.tt(Hcur[hp][:], bh[:, 0:64], gs[:, c, :], ALU.add)
        for j in range(4 if lim_stage >= 7 else 0):
            t0 = t00 + j * 128
            jsl = slice(j * 128, (j + 1) * 128)
            y = yt[oi % 2]
            s_ = st[oi % 2]
            yn_ = yn[oi % 2]
            o_b = ob[oi % 2]
            oi += 1
            for h in range(2):
                hb = h * 64
                k.mm(bh[:, 64 + h * 64:128 + h * 64], rz[hb:hb + 64, j, 0, :], hst[hb:hb + 64, 2 * j, :], start=True, stop=False)
                k.mm(bh[:, 64 + h * 64:128 + h * 64], rz[hb:hb + 64, j, 1, :], hst[hb:hb + 64, 2 * j + 1, :], start=False, stop=True)
            k.tt(y[:], bh[:, 64:192].rearrange("p (h v) -> p h v", h=2), y0s[:, j, :, :], ALU.add)
            if lim_stage < 8:
                continue
            k.reduce(s_[:, 0:2], y[:], ALU.add)
            k.tt(ysq[:], y[:], y[:], ALU.mult, eng="pool")
            k.reduce(s_[:, 2:4], ysq[:], ALU.add)
            k.ts(s_[:, 0:2], s_[:, 0:2], 1.0 / 64, None, ALU.mult)
            k.tt(s_[:, 4:6], s_[:, 0:2], s_[:, 0:2], ALU.mult)
            k.stt(s_[:, 2:4], s_[:, 2:4], 1.0 / 64, s_[:, 4:6], ALU.mult, ALU.subtract)
            k.act(s_[:, 2:4], s_[:, 2:4], AF.Sqrt, bias=GN_EPS)
            k.recip(s_[:, 2:4], s_[:, 2:4])
            if lim_stage < 9:
                continue
            for h in range(2):
                k.ts(yn_[:, h, :], y[:, h, :], s_[:, h:h + 1], s_[:, 2 + h:3 + h], ALU.subtract, ALU.mult)
            ynf = yn_[:].rearrange("p h v -> p (h v)")
            k.tt(ynf, ynf, gnw[:, hp * 128:(hp + 1) * 128], ALU.mult, eng="pool")
            k.tt(ynf, ynf, gnb[:, hp * 128:(hp + 1) * 128], ALU.add, eng="pool")
            for h in range(2):
                k.stt(yn_[:, h, :], vsb[:, j, hp * 128 + h * 64:hp * 128 + (h + 1) * 64], bs[:, j, h:h + 1], yn_[:, h, :], ALU.mult, ALU.add)
            if lim_stage < 10:
                continue
            k.mm(bh[:, 256:384], sgl[:, jsl], g2b[:, hp * 128:(hp + 1) * 128])
            k.tt(o_b[:], ynf, bh[:, 256:384], ALU.mult)
            k.dma(cx.O_mix[t0:t0 + 128, 512 + hp * 128:512 + (hp + 1) * 128], o_b[:], q="pool")


    def run_rr(gens):
        gens = list(gens)
        while gens:
            for g_ in list(gens):
                try:
                    next(g_)
                except StopIteration:
                    gens.remove(g_)

    def chain_gen(*gs_):
        for g_ in gs_:
            yield from g_

    n_tb = min(T // 512, lim_tb)
    n_hp = min(4, lim_hp)
    units = [(tb, hp) for tb in range(n_tb) for hp in range(n_hp)]
    run_rr([chain_gen(shared_gen(0), pre_gen(0, 0))])
    for ui_, (tb, hp) in enumerate(units):
        gens = [main_gen(tb, hp)] if lim_stage >= 2 else []
        if ui_ + 1 < len(units):
            ntb, nhp = units[ui_ + 1]
            if ntb != tb:
                gens.append(chain_gen(shared_gen(ntb), pre_gen(ntb, nhp)))
            else:
                gens.append(pre_gen(ntb, nhp))
        run_rr(gens)
    k.pop()


NSA_SLOPES = [2.0 ** (-8.0 * (i + 1) / 16) for i in range(16)]


def phase_nsa(k, cx):
    inp = cx.inp
    k.push()

    def cload(name, shape, dtype=F32, parts=None):
        t = k.sb("nc_" + name, shape, F32)
        if parts is None:
            k.dma(t[:], inp[name][:])
        else:
            k.dma(t[parts[0]:parts[1]], inp[name][:])
        if dtype == F32:
            return t
        tb_ = k.sb("ncb_" + name, shape, dtype)
        if parts is None:
            k.copy(tb_[:], t[:])
        else:
            k.copy(tb_[parts[0]:parts[1]], t[parts[0]:parts[1]])
        return tb_

    ident = cx.ident
    diagD = cload("c_diagD", [128, 4, 512])
    winD = cload("c_winD", [128, 4, 512])
    kbias = cload("c_kbias", [128, 16 * 28])
    ov = cload("c_ov", [128, 2, 65], BF16)
    selg = cload("c_selg", [48, 48, 64], BF16)
    w2k = cload("nsa_w2k", [64, 64], BF16)
    w2v = cload("nsa_w2v", [64, 64], BF16)
    peT = cload("nsa_peT", [64, 2, 32], BF16)
    w1 = []
    for i, nm in enumerate(("nsa_w1k", "nsa_w1v")):
        wt = k.sb(f"n_w1_{i}", [64, 32, 64], BF16)
        k.push()
        st = k.sb("n_w1st", [64, 32, 64], F32)
        k.dma(st[:], inp[nm][:].rearrange("(l d) h -> d l h", d=64))
        k.copy(wt[:], st[:])
        k.pop()
        w1.append(wt)
    KA_sel = k.sb("n_KAs", [128, 32, 128], BF16)
    KA_win = k.sb("n_KAw", [128, 32, 128], BF16)
    k.push()
    st = k.sb("n_kaugst", [128, 32, 128], F32)
    k.dma(st[64:128], inp["c_kaug_sel"][:])
    k.copy(KA_sel[64:128], st[64:128])
    k.dma(st[64:128], inp["c_kaug_win"][:])
    k.copy(KA_win[64:128], st[64:128])
    k.pop()
    QA = [[k.sb(f"n_QA{i}{r}", [128, 512], BF16) for r in range(4)] for i in range(2)]
    VA_sel = k.sb("n_VAs", [128, 32, 128], BF16)
    VA_win = k.sb("n_VAw", [128, 32, 128], BF16)
    k.memset(VA_sel[:, :, 64:128], 1.0)
    k.memset(VA_win[:, :, 64:128], 1.0, eng="pool")
    VCA = k.sb("n_VCA", [128, 2, 128], BF16)
    k.memset(VCA[:], 0.0)
    k.memset(VCA[:, :, 64:128], 1.0)
    KC = k.sb("n_KC", [64, 256], BF16)
    XC = [k.sb(f"n_XC{i}", [64, T], BF16) for i in range(2)]
    h1 = [k.sb(f"n_h1{i}", [64, 256], BF16) for i in range(2)]
    cpe = k.sb("n_cpe", [64, 2], F32)
    Ec = k.sb("n_Ec", [128, 4, 2, 512], BF16)
    cD = [k.sb(f"n_cD{i}", [128, 512], F32) for i in range(4)]
    sc = [k.sb(f"n_sc{i}", [128, 512], F32) for i in range(3)]
    Pb = [k.sb(f"n_P{i}", [128, 512], BF16) for i in range(6)]
    gts = k.sb("n_gts", [48, 512], BF16)
    gtr = k.sb("n_gtr", [48, 512], F32)
    rden = [k.sb(f"n_rden{i}", [64, 512], F32) for i in range(2)]
    ff_ = [k.sb(f"n_f{i}", [64, 512], F32) for i in range(2)]
    acc = [k.sb(f"n_acc{i}", [64, 512], F32) for i in range(4)]
    tmpa = [k.sb(f"n_tmpa{i}", [64, 512], F32) for i in range(2)]
    ob = [k.sb(f"n_ob{i}", [64, 512], BF16) for i in range(2)]
    sval = [k.sb(f"n_sval{i}", [128, 64], F32) for i in range(2)]
    sadd = [k.sb(f"n_sadd{i}", [128, 64], F32) for i in range(2)]
    imp = [k.sb(f"n_imp{i}", [128, 64], F32) for i in range(2)]
    rec = [k.sb(f"n_rec{i}", [128, 1], F32) for i in range(4)]
    top8 = [k.sb(f"n_top8{i}", [128, 8], F32) for i in range(2)]
    msk = [k.sb(f"n_msk{i}", [128, 64], F32) for i in range(2)]
    MTk = [k.sb(f"n_MTk{i}", [128, 128], BF16) for i in range(2)]
    for t_ in MTk:
        k.memset(t_[:], 0.0)
    S = [k.ps(f"n_S{i}", [128, 512], F32) for i in range(4)]
    O = [k.ps(f"n_O{i}", [128, 512], F32) for i in range(2)]
    PG = k.ps("n_PG", [128, 512], F32)
    PI = PG
    PT = k.ps("n_PT", [128, 1024], BF16)

    qaug_st = k.sb("n_qaugst", [128, 512], F32)

    for i in range(2):
        for l in range(32):
            k.mm(PG[0:64, i:i + 1], w1[i][:, l, :], peT[:, i, l:l + 1], start=(l == 0), stop=(l == 31))
    k.copy(cpe[:], PG[0:64, 0:2])

    si = 0
    pi_ = 0
    oi = 0
    fi = 0
    for g in range(4):
        k.dma(KA_sel[0:64, :, :], cx.N_ks[g * 64:(g + 1) * 64, :].rearrange("d (kt s) -> d kt s", s=128))
        k.dma(KA_win[0:64, :, :], cx.N_kw[g * 64:(g + 1) * 64, :].rearrange("d (kt s) -> d kt s", s=128))
        k.dma(VA_sel[:, :, 0:64], cx.N_vs[:, g * 64:(g + 1) * 64].rearrange("(kt p) c -> p kt c", p=128))
        k.dma(VA_win[:, :, 0:64], cx.N_vw[:, g * 64:(g + 1) * 64].rearrange("(kt p) c -> p kt c", p=128))
        for r in range(4):
            h = g * 4 + r
            k.dma(qaug_st[64:128, :], inp["c_qaug"][h])
            for i in range(2):
                k.copy(QA[i][r][64:128, :], qaug_st[64:128, :])
        for i in range(2):
            k.dma(XC[i][:], cx.N_c[i * 256 + g * 64:i * 256 + (g + 1) * 64, :])
            for l in range(32):
                k.mm(PG[0:64, 0:255], w1[i][:, l, :], XC[i][:, l:l + 4065:16], start=(l == 0), stop=(l == 31))
            k.act(h1[i][:, 0:255], PG[0:64, 0:255], AF.Gelu, bias=cpe[:, i:i + 1])
        k.mm(PG[0:64, 256:511], w2k[:], h1[0][:, 0:255])
        k.copy(KC[:, 0:255], PG[0:64, 256:511])
        for nt, nn in ((0, 128), (1, 127)):
            k.mm(PI[0:nn, 0:64], h1[1][:, nt * 128:nt * 128 + nn], w2v[:])
            k.copy(VCA[0:nn, nt, 0:64], PI[0:nn, 0:64])
        for qb in range(T // 512):
            qsl = slice(qb * 512, (qb + 1) * 512)
            qa = QA[qb % 2]
            for r in range(4):
                h = g * 4 + r
                k.dma(qa[r][0:64, :], cx.N_q[h * 64:(h + 1) * 64, qsl])
            k.dma(gtr[:], cx.N_gt[:, qsl])
            k.act(gts[:], gtr[:], AF.Sigmoid)
            nts = ((0, 128),) if qb < 4 else ((0, 128), (1, 127))
            cds = {}
            for nt, nn in nts:
                cd = cD[(qb * 2 + nt) % 4]
                k.dma(cd[:], inp["c_cmpD"][(qb if nt == 0 else 8 + qb - 4)])
                cds[nt] = cd

            def finalize(o_ps, h, br, first):
                nonlocal fi
                rd, f_, tm_ = rden[fi % 2], ff_[fi % 2], tmpa[fi % 2]
                fi += 1
                if br == 0:
                    k.ts(rd[:], o_ps[64:128, :], 1e-30, None, ALU.max)
                    k.act(rd[:], rd[:], AF.Ln)
                else:
                    k.act(rd[:], o_ps[64:128, :], AF.Ln)
                k.act(rd[:], rd[:], AF.Exp, scale=-1.0)
                k.mm(PG[0:64, :], selg[:, h * 3 + br, :], gts[:])
                k.tt(f_[:], PG[0:64, :], rd[:], ALU.mult)
                a = acc[h % 4]
                if first:
                    k.tt(a[:], o_ps[0:64, :], f_[:], ALU.mult)
                else:
                    k.tt(tm_[:], o_ps[0:64, :], f_[:], ALU.mult)
                    k.tt(a[:], a[:], tm_[:], ALU.add, eng="pool")

            for r in range(4):
                h = g * 4 + r
                slope = NSA_SLOPES[h]
                o_ps = O[oi % 2]
                oi += 1
                for idx, (nt, nn) in enumerate(nts):
                    s_ps = S[si % 3]
                    s_sb = sc[si % 3]
                    si += 1
                    k.mm(s_ps[0:nn, :], KC[:, nt * 128:nt * 128 + nn], qa[r][0:64, :])
                    k.stt(s_sb[0:nn, :], cds[nt][0:nn, :], slope, s_ps[0:nn, :], ALU.mult, ALU.add)
                    k.act(Ec[0:nn, r, nt, :], s_sb[0:nn, :], AF.Exp)
                for idx, (nt, nn) in enumerate(nts):
                    k.mm(o_ps[:, :], VCA[0:nn, nt, :], Ec[0:nn, r, nt, :], start=(idx == 0), stop=(idx == len(nts) - 1))
                finalize(o_ps, h, 0, True)
            for qt in range(4):
                t0 = qb * 512 + qt * 128
                sv, sa = sval[qt % 2], sadd[qt % 2]
                im, t8, mk, mtk = imp[qt % 2], top8[qt % 2], msk[qt % 2], MTk[qt % 2]
                k.dma(sv[:], inp["c_selvalid"][t0:t0 + 128, :])
                k.dma(sa[:], inp["c_seladd"][t0:t0 + 128, :])
                for r in range(4):
                    rc = rec[r]
                    for idx, (nt, nn) in enumerate(nts):
                        k.mm(PI[:, r * 65:(r + 1) * 65], Ec[0:nn, r, nt, qt * 128:(qt + 1) * 128], ov[0:nn, nt, :], start=(idx == 0), stop=(idx == len(nts) - 1))
                    k.ts(rc[:], PI[:, r * 65 + 64:r * 65 + 65], 1e-30, None, ALU.max)
                    k.recip(rc[:], rc[:])
                    if r == 0:
                        k.ts(im[:], PI[:, 0:64], rc[:, 0:1], None, ALU.mult)
                    else:
                        k.stt(im[:], PI[:, r * 65:r * 65 + 64], rc[:, 0:1], im[:], ALU.mult, ALU.add)
                k.tt(im[:], im[:], sv[:], ALU.mult)
                k.tt(im[:], im[:], sa[:], ALU.add)
                i_, o_ = im.t[:], t8.t[:]
                k.generic("dve", lambda e, i_=i_, o_=o_: e.max(o_, i_), [im], [t8])
                k.ts(mk[:], im[:], t8[:, 7:8], None, ALU.is_ge)
                k.ts(mtk[:, 64:126], mk[:, 1:63], 30000.0, -30000.0, ALU.mult, ALU.add)
                k.tr(PT[:, 0:128], mtk[:], ident[:])
                for r in range(4):
                    k.copy(qa[r][64:126, qt * 128:(qt + 1) * 128], PT[64:126, 0:128], eng=("act" if r % 2 else "dve"))
            def stream(br, r, o_ps):
                nonlocal si, pi_
                KA, VA = (KA_sel, VA_sel) if br == 1 else (KA_win, VA_win)
                h = g * 4 + r
                slope = NSA_SLOPES[h]
                tiles = []
                if br == 1:
                    tiles += [("fast", kt, kt - 4 * qb) for kt in range(4 * qb)]
                elif qb > 0:
                    tiles += [("far", 4 * qb - 4 + j, j) for j in range(4)]
                tiles += [("diag", 4 * qb + j, j) for j in range(4)]
                pend = None
                for idx, (kind, kt, j) in enumerate(tiles):
                    s_ps = S[si % 4]
                    s_sb = sc[si % 3]
                    si += 1
                    p_ = Pb[pi_ % 6]
                    pi_ += 1
                    if kind == "diag":
                        c0, c1 = 128 * j, 512
                    elif kind == "far":
                        c0, c1 = 0, 128 * (j + 1)
                    else:
                        c0, c1 = 0, 512
                    k.mm(s_ps[:, c0:c1], KA[:, kt, :], qa[r][:, c0:c1])
                    if kind == "fast":
                        col = h * 28 + (j + 28)
                        k.act(p_[:], s_ps[:], AF.Exp, bias=kbias[:, col:col + 1])
                    else:
                        dt_ = diagD if kind == "diag" else winD
                        k.stt(s_sb[:, c0:c1], dt_[:, j, c0:c1], slope, s_ps[:, c0:c1], ALU.mult, ALU.add)
                        k.act(p_[:, c0:c1], s_sb[:, c0:c1], AF.Exp)
                    yield
                    if pend is not None:
                        pp, pkt, pidx, pc0, pc1 = pend
                        k.mm(o_ps[:, pc0:pc1], VA[:, pkt, :], pp[:, pc0:pc1], start=(pidx == 0), stop=False)
                    pend = (p_, kt, idx, c0, c1)
                pp, pkt, pidx, pc0, pc1 = pend
                k.mm(o_ps[:, pc0:pc1], VA[:, pkt, :], pp[:, pc0:pc1], start=(pidx == 0), stop=True)
                yield
                finalize(o_ps, h, br, False)

            for r in range(4):
                h = g * 4 + r
                gens = [stream(1, r, O[0]), stream(2, r, O[1])]
                while gens:
                    for g_ in list(gens):
                        try:
                            next(g_)
                        except StopIteration:
                            gens.remove(g_)
                o_b = ob[h % 2]
                k.copy(o_b[:], acc[h % 4][:], eng="act")
                k.dma(cx.O_fm[h * 64:(h + 1) * 64, qsl], o_b[:], q="pool")
    k.pop()


INPUT_SHAPES = {
    "x": [T, D], "c_col": [128, NKC], "ada_w": [2, D, 6 * D], "ada_b": [2, 6 * D],
    "norm1_g": [2, D], "norm2_g": [2, D], "final_norm_g_bc": [128, D],
    "ffn_w_up": [2, D, 2 * FF], "ffn_conv_col": [2, 128, FF // 128, 4], "ffn_w_down": [2, FF, D],
    "ev_w_in": [1, D, EV_COLS], "ev_w_out": [1, D, D], "od_w_in": [1, D, OD_COLS], "od_w_out": [1, D, D],
    "gla_a_up": [16, 256], "gla_a_b_col": [128, 2], "gla_norm_g_bc": [128, 128],
    "c_ident": [128, 128], "c_scan128": [128, 512], "c_tri_incl": [128, 128],
    "rw_mu_col": [128, 10], "rw_mu_k_col": [128, 4], "rw_mu_al_col": [64, 1], "rw_muv_bc": [128, 512],
    "rw_w0_col": [128, 4], "rw_a0_col": [128, 4], "rw_k_k_col": [128, 4], "rw_k_a_col": [128, 4],
    "rw_w2": [64, 512], "rw_a2": [64, 512], "rw_g2": [128, 512], "rw_rkcol": [128, 4, 2],
    "rw_gn_w_bc": [128, 512], "rw_gn_b_bc": [128, 512],
    "c_blockones": [128, 128], "c_negblock": [128, 128], "c_scan64": [128, 512],
    "c_mk1": [128, 256], "c_mk2": [128, 256], "c_nmt": [128, 128],
    "c_diagD": [128, 4, 512], "c_winD": [128, 4, 512], "c_kbias": [128, 16 * 28], "c_ov": [128, 2, 65],
    "c_selg": [48, 48, 64], "nsa_w2k": [64, 64], "nsa_w2v": [64, 64], "nsa_peT": [64, 2, 32],
    "nsa_w1k": [2048, 64], "nsa_w1v": [2048, 64], "c_kaug_sel": [64, 32, 128], "c_kaug_win": [64, 32, 128],
    "c_qaug": [16, 64, 512], "c_cmpD": [12, 128, 512], "c_selvalid": [T, 64], "c_seladd": [T, 64],
}


def build(stop=None, dbg=(), rw_limit=None, skip=()):
    nc = bass.Bass("TRN2", target_bir_lowering=False)
    k = K(nc)
    cx = Ctx()
    if rw_limit is not None:
        cx.rw_limit = rw_limit
    cx.inp = {}
    for name, shape in INPUT_SHAPES.items():
        cx.inp[name] = k.dram(name, shape, F32, kind="ExternalInput")

    def scratch(name, shape, dtype=F32):
        return k.dram(name, shape, dtype, kind=("ExternalOutput" if name in dbg else "Internal"))

    out = k.dram("out", [T, D], F32, kind="ExternalOutput")
    cx.G_qk = scratch("G_qk", [512, T])
    cx.G_lr = scratch("G_lr", [16, T])
    cx.G_v = scratch("G_v", [T, 512], BF16)
    cx.G_og = scratch("G_og", [T, 512])
    cx.R_fm = scratch("R_fm", [1280, T])
    cx.R_v = scratch("R_v", [T, 512])
    cx.O_mix = scratch("O_mix", [T, D], BF16)
    cx.O_fm = scratch("O_fm", [D, T], BF16)
    cx.H_fm = scratch("H_fm", [FF, T], BF16)
    cx.XA = scratch("XA", [T, D])
    cx.XB = scratch("XB", [T, D])
    cx.XC = scratch("XC", [T, D])
    cx.N_q = scratch("N_q", [D, T], BF16)
    cx.N_c = scratch("N_c", [512, T], BF16)
    cx.N_ks = scratch("N_ks", [256, T], BF16)
    cx.N_kw = scratch("N_kw", [256, T], BF16)
    cx.N_gt = scratch("N_gt", [48, T])
    cx.N_vs = scratch("N_vs", [T, 256], BF16)
    cx.N_vw = scratch("N_vw", [T, 256], BF16)
    x = cx.inp["x"]

    def done(stage):
        return stop is not None and stage == stop

    phase_consts(k, cx)
    k.push()
    bc = phase_mod(k, cx, 0)
    if not done("mod0") and "l0" not in skip:
        fm = [(0, 512, cx.G_qk, 0, F32, 1.0), (1536, 16, cx.G_lr, 0, F32, 1.0),
              (1552, 1088, cx.R_fm, 0, F32, 1.0), (1552 + 1600, 192, cx.R_fm, 1088, F32, 1.0)]
        tm = [(512, 512, cx.G_v, 0, BF16), (1024, 512, cx.G_og, 0, F32), (1552 + 1088, 512, cx.R_v, 0, F32)]
        phase_proj(k, cx, x, cx.inp["ev_w_in"][0], EV_COLS, bc["gsc1"], bc["sh1"], fm, tm, "pj0")
    stages = ["mod0", "proj0", "gla", "rwkv", "wout0", "ffn0u", "ffn0d", "proj1", "nsa", "wout1", "ffn1u", "ffn1d"]
    def upto(stage):
        return stop is None or stop not in stages or stages.index(stop) >= stages.index(stage)
    if "l0" in skip:
        stages_l0_off = True
    if upto("gla") and "gla" not in skip and "l0" not in skip:
        phase_gla(k, cx)
    if upto("rwkv") and "l0" not in skip:
        phase_rwkv(k, cx)
    if upto("wout0") and "l0" not in skip:
        phase_wout(k, cx, x, cx.XA, cx.inp["ev_w_out"][0], bc["g1"], cx.O_mix, "tm", "wo0")
    if upto("ffn0u") and "l0" not in skip:
        phase_ffn_up(k, cx, 0, cx.XA, bc)
    if upto("ffn0d") and "l0" not in skip:
        phase_ffn_down(k, cx, 0, cx.XA, cx.XB, bc)
    k.pop()
    if upto("proj1"):
        xin1 = cx.XB if "l0" not in skip else x
        k.push()
        bc = phase_mod(k, cx, 1)
        fm = [(0, 1024, cx.N_q, 0, BF16, 0.125), (1024, 512, cx.N_c, 0, BF16, 1.0), (1536, 256, cx.N_ks, 0, BF16, 1.0),
              (2048, 256, cx.N_kw, 0, BF16, 1.0), (2560, 48, cx.N_gt, 0, F32, 1.0)]
        tm = [(1792, 256, cx.N_vs, 0, BF16), (2304, 256, cx.N_vw, 0, BF16)]
        phase_proj(k, cx, xin1, cx.inp["od_w_in"][0], OD_COLS, bc["gsc1"], bc["sh1"], fm, tm, "pj1")
        if upto("nsa"):
            phase_nsa(k, cx)
        if upto("wout1"):
            phase_wout(k, cx, xin1, cx.XC, cx.inp["od_w_out"][0], bc["g1"], cx.O_fm, "fm", "wo1")
        if upto("ffn1u"):
            phase_ffn_up(k, cx, 1, cx.XC, bc)
        if upto("ffn1d"):
            phase_ffn_down(k, cx, 1, cx.XC, out, bc, final=True)
        k.pop()
    build.stats = {e: len(k.ops[e]) for e in ENGS}
    k.emit()
    return nc


def host_inputs(inputs, b):
    f = np.float32
    m = {}
    m["x"] = np.ascontiguousarray(inputs["x"][b], dtype=f)
    m["c_col"] = np.ascontiguousarray(inputs["c"][b].reshape(NKC, 128).T, dtype=f)
    for nm in ("ada_w", "ada_b", "norm1_g", "norm2_g", "ffn_w_up", "ffn_w_down", "ev_w_in", "ev_w_out", "od_w_in", "od_w_out"):
        m[nm] = np.ascontiguousarray(inputs[nm], dtype=f)
    m["final_norm_g_bc"] = np.ascontiguousarray(np.broadcast_to(inputs["final_norm_g"][None, :], (128, D)), dtype=f)
    cw = np.concatenate([inputs["ffn_conv_w"], inputs["ffn_conv_b"][:, None, :]], axis=1)
    m["ffn_conv_col"] = np.ascontiguousarray(cw.reshape(2, 4, FF // 128, 128).transpose(0, 3, 2, 1), dtype=f)
    m["gla_a_up"] = np.ascontiguousarray(inputs["gla_a_up"][0], dtype=f)
    m["gla_a_b_col"] = np.ascontiguousarray(inputs["gla_a_b"][0].reshape(2, 128).T, dtype=f)
    m["gla_norm_g_bc"] = np.ascontiguousarray(np.broadcast_to(inputs["gla_norm_g"][0][None, :], (128, 128)), dtype=f)
    m["c_ident"] = np.eye(128, dtype=f)
    sc = np.ones((128, 512), f)
    sc[:, 0::128] = 0.0
    m["c_scan128"] = sc
    i = np.arange(128)
    m["c_tri_incl"] = (i[:, None] <= i[None, :]).astype(f)
    smu = inputs["ev_shift_mu"][0]
    mu_fm = np.concatenate([smu[0:1088], smu[1600:1792]])
    m["rw_mu_col"] = np.ascontiguousarray(mu_fm.reshape(10, 128).T, dtype=f)
    m["rw_mu_k_col"] = np.ascontiguousarray(smu[576:1088].reshape(4, 128).T, dtype=f)
    m["rw_mu_al_col"] = np.ascontiguousarray(smu[1600:1664].reshape(64, 1), dtype=f)
    m["rw_muv_bc"] = np.ascontiguousarray(np.broadcast_to(smu[1088:1600][None, :], (128, 512)), dtype=f)
    for nm in ("w0", "a0", "k_k", "k_a"):
        m[f"rw_{nm}_col"] = np.ascontiguousarray(inputs[f"rw_{nm}"][0].reshape(4, 128).T, dtype=f)
    m["rw_w2"] = np.ascontiguousarray(inputs["rw_w2"][0], dtype=f)
    m["rw_a2"] = np.ascontiguousarray(inputs["rw_a2"][0], dtype=f)
    m["rw_g2"] = np.ascontiguousarray(inputs["rw_g2"][0], dtype=f)
    rk = inputs["rw_r_k"][0]
    rkcol = np.zeros((128, 4, 2), f)
    for hp in range(4):
        rkcol[0:64, hp, 0] = rk[2 * hp]
        rkcol[64:128, hp, 1] = rk[2 * hp + 1]
    m["rw_rkcol"] = rkcol
    m["rw_gn_w_bc"] = np.ascontiguousarray(np.broadcast_to(inputs["rw_gn_w"][0][None, :], (128, 512)), dtype=f)
    m["rw_gn_b_bc"] = np.ascontiguousarray(np.broadcast_to(inputs["rw_gn_b"][0][None, :], (128, 512)), dtype=f)
    same = (i[:, None] // 64) == (i[None, :] // 64)
    m["c_blockones"] = same.astype(f)
    m["c_negblock"] = -same.astype(f)
    s64 = np.ones((128, 512), f)
    s64[:, 0::64] = 0.0
    m["c_scan64"] = s64
    mstrict = (same & (i[:, None] < i[None, :])).astype(f)
    mincl = (same & (i[:, None] <= i[None, :])).astype(f)
    m["c_mk1"] = np.concatenate([mstrict, mincl], axis=1)
    m["c_mk2"] = np.concatenate([-mstrict, mincl], axis=1)
    m["c_nmt"] = np.ascontiguousarray(-mstrict.T)
    s_ = np.arange(128)[:, None].astype(np.float64)
    q_ = np.arange(512)[None, :].astype(np.float64)
    NEG = -1.0e6
    dd = np.zeros((128, 4, 512), f)
    wd = np.zeros((128, 4, 512), f)
    for j in range(4):
        dd[:, j, :] = np.where(128 * j + s_ <= q_, 128 * j + s_ - 511.0, NEG)
        wd[:, j, :] = np.where(128 * j + s_ > q_, 128 * j - 512.0 + s_ - 511.0, NEG)
    m["c_diagD"] = dd
    m["c_winD"] = wd
    slopes = np.array(NSA_SLOPES, np.float64)
    kb = np.zeros((128, 16 * 28), f)
    for h in range(16):
        for jj in range(28):
            kb[:, h * 28 + jj] = (slopes[h] * (np.arange(128) + 128.0 * (jj - 28) - 511.0)).astype(f)
    m["c_kbias"] = kb
    n_ = np.arange(256)
    cstart = n_ * 16
    cend = cstart + 31
    sstart = np.arange(64) * 64
    ovl = ((cstart[:, None] <= sstart[None, :] + 63) & (cend[:, None] >= sstart[None, :])).astype(f)
    ovl[255] = 0.0
    ov = np.zeros((128, 2, 65), f)
    ov[:, 0, :64] = ovl[:128]
    ov[:, 1, :64] = ovl[128:]
    ov[:, :, 64] = 1.0
    ov[127, 1, :] = 0.0
    m["c_ov"] = ov
    sg = np.zeros((48, 48, 64), f)
    for i_ in range(48):
        sg[i_, i_, :] = 1.0
    m["c_selg"] = sg
    m["nsa_w2k"] = np.ascontiguousarray(inputs["cmp_w2_k"][0], dtype=f)
    m["nsa_w2v"] = np.ascontiguousarray(inputs["cmp_w2_v"][0], dtype=f)
    m["nsa_peT"] = np.ascontiguousarray(np.stack([inputs["cmp_pe_k"][0].T, inputs["cmp_pe_v"][0].T], axis=1), dtype=f)
    m["nsa_w1k"] = np.ascontiguousarray(inputs["cmp_w1_k"][0], dtype=f)
    m["nsa_w1v"] = np.ascontiguousarray(inputs["cmp_w1_v"][0], dtype=f)
    ka_s = np.zeros((64, 32, 128), f)
    ka_w = np.zeros((64, 32, 128), f)
    for kt in range(32):
        for half in range(2):
            b_ = 2 * kt + half
            if 1 <= b_ <= 62:
                ka_s[b_ - 1, kt, half * 64:(half + 1) * 64] = 1.0
    ka_s[62:64] = 1.0
    ka_w[62:64] = 1.0
    m["c_kaug_sel"] = ka_s
    m["c_kaug_win"] = ka_w
    import ml_dtypes
    qa = np.zeros((16, 64, 512), f)
    for h in range(16):
        rv = slopes[h] * (511.0 - np.arange(512))
        hi = rv.astype(f).astype(ml_dtypes.bfloat16).astype(np.float64)
        lo = (rv - hi).astype(f).astype(ml_dtypes.bfloat16).astype(np.float64)
        qa[h, 62] = hi
        qa[h, 63] = lo
    m["c_qaug"] = qa
    cD = np.zeros((12, 128, 512), f)
    for qb in range(8):
        for nt in range(2):
            if nt == 1 and qb < 4:
                continue
            e_n = 16.0 * (128 * nt + np.arange(128)[:, None]) + 31.0
            t_q = 512.0 * qb + np.arange(512)[None, :]
            cD[qb if nt == 0 else 8 + qb - 4] = np.where(t_q >= e_n, -(t_q - e_n), NEG)
    m["c_cmpD"] = cD
    t_ = np.arange(T)
    ahead = (t_ // 64)[:, None] - np.arange(64)[None, :]
    valid = ahead >= 0
    forced = (np.arange(64)[None, :] == 0) | (valid & (ahead < 2))
    m["c_selvalid"] = valid.astype(f)
    m["c_seladd"] = np.where(valid, np.where(forced, 100.0, 0.0), -100.0).astype(f)
    return m


_CACHE = {}


def kernel(**inputs):
    inputs = {k_: np.asarray(v) for k_, v in inputs.items()}
    if "nc" not in _CACHE:
        _CACHE["nc"] = build()
    nc = _CACHE["nc"]
    B = inputs["x"].shape[0]
    maps = [host_inputs(inputs, b) for b in range(B)]
    zero = dict(maps[0])
    zero["x"] = np.zeros_like(maps[0]["x"])
    in_maps = maps + [zero] * (8 - B)
    res = run_bass_kernel_spmd(nc, in_maps, core_ids=list(range(8)))
    out = np.stack([np.asarray(res.results[b]["out"], dtype=np.float32) for b in range(B)], axis=0)
    return out
```

```python
import contextlib
import numpy as np
import concourse.bass as bass
import concourse.mybir as mybir
from concourse.bass_utils import run_bass_kernel_spmd

F32 = mybir.dt.float32
BF16 = mybir.dt.bfloat16
AF = mybir.ActivationFunctionType
ALU = mybir.AluOpType
AX = mybir.AxisListType

ENGS = ("pe", "act", "dve", "pool", "sp")


class Tl:
    _n = 0

    def __init__(self, t, space, key=None):
        self.t = t
        self.space = space
        Tl._n += 1
        self.key = key if key is not None else ("t", Tl._n)

    def __getitem__(self, idx):
        return V(self, self.t[idx])

    def ap(self):
        return V(self, self.t[:])

    def part(self, sub):
        return Tl(self.t, self.space, key=(self.key, sub))


class V:
    def __init__(self, tile, ap):
        self.tile = tile
        self.ap = ap

    def __getitem__(self, idx):
        return V(self.tile, self.ap[idx])

    def rearrange(self, *a, **kw):
        return V(self.tile, self.ap.rearrange(*a, **kw))

    def bitcast(self, dt):
        return V(self.tile, self.ap.bitcast(dt))

    @property
    def shape(self):
        return self.ap.shape


class Op:
    __slots__ = ("eng", "fn", "waits", "inc", "kind")


class K:
    def __init__(self, nc, n_dma_slots=(("sp", 40), ("pool", 16), ("act", 12)), same_engine_raw=True):
        self.nc = nc
        self.stack = contextlib.ExitStack()
        self.ops = {e: [] for e in ENGS}
        self.count = {e: 0 for e in ENGS}
        self.sem = {}
        for e in ENGS:
            self.sem[e] = self.stack.enter_context(nc.semaphore("s_" + e))
        self.slots = {}
        for q, n in n_dma_slots:
            self.slots[q] = [[self.stack.enter_context(nc.semaphore(f"d_{q}{i}")), 0] for i in range(n)]
        self.slot_rr = {q: 0 for q, _ in n_dma_slots}
        self.waited = {e: {} for e in ENGS}
        self.last_w = {}
        self.readers = {}
        self.same_engine_raw = same_engine_raw
        self.n_ops = 0
        self.stacks = [self.stack]
        self.dram_w = {}
        self.pe_rg = {}
        self.uid = 0

    def push(self):
        self.stacks.append(contextlib.ExitStack())

    def pop(self):
        self.barrier()
        self.stacks.pop().close()

    def sb(self, name, shape, dtype=F32):
        self.uid += 1
        t = self.stacks[-1].enter_context(self.nc.sbuf_tensor(f"{name}_{self.uid}", list(shape), dtype))
        return Tl(t, "sb")

    def ps(self, name, shape, dtype=F32):
        self.uid += 1
        t = self.stacks[-1].enter_context(self.nc.psum_tensor(f"{name}_{self.uid}", list(shape), dtype))
        return Tl(t, "ps")

    def dram(self, name, shape, dtype=F32, kind="Internal"):
        t = self.nc.dram_tensor(name, list(shape), dtype, kind=kind).ap()
        return Tl(t, "dram")

    def _need(self, eng, deps, sem, val, src_eng):
        if sem is None:
            return
        if src_eng == eng and eng == "pe":
            return
        cur = deps.get(id(sem))
        if cur is None or cur[1] < val:
            deps[id(sem)] = (sem, val)

    def _record(self, eng, fn, reads, writes, dma_q=None, rg=None):
        deps = {}
        rkeys = []
        wkeys = []
        for v in reads:
            if v is None:
                continue
            tl = v.tile if isinstance(v, V) else v
            rkeys.append((tl.key, tl.space))
        for v in writes:
            tl = v.tile if isinstance(v, V) else v
            wkeys.append((tl.key, tl.space))
        is_dma = dma_q is not None
        for key, space in rkeys:
            if space == "dram":
                for (sem, val, seng, wdma) in self.dram_w.get(key, []):
                    self._need(eng, deps, sem, val, None)
                continue
            lw = self.last_w.get(key)
            if lw is not None:
                sem, val, seng, wdma = lw
                if seng == eng and not wdma and not is_dma and not self.same_engine_raw and eng != "pe":
                    pass
                else:
                    self._need(eng, deps, sem, val, seng if not (wdma or is_dma) else None)
            if space == "ps":
                for (sem, val, seng, rdma) in self.readers.get(key, []):
                    if seng != eng:
                        self._need(eng, deps, sem, val, seng)
        for key, space in wkeys:
            if rg is not None and space == "ps":
                prev = self.pe_rg.get(key)
                if prev is not None and prev[0] != rg:
                    self._need(eng, deps, prev[1][0], prev[1][1], None)
            lw = self.last_w.get(key) if space != "dram" else None
            if lw is not None:
                sem, val, seng, wdma = lw
                if seng != eng or wdma or is_dma:
                    self._need(eng, deps, sem, val, None if (wdma or is_dma) else seng)
            for (sem, val, seng, rdma) in self.readers.get(key, []):
                if seng != eng or rdma or is_dma:
                    self._need(eng, deps, sem, val, None if (rdma or is_dma) else seng)
        if is_dma:
            sl = self.slots[dma_q]
            i = self.slot_rr[dma_q]
            self.slot_rr[dma_q] = (i + 1) % len(sl)
            sem, uses = sl[i]
            if uses > 0:
                self._need(eng, deps, sem, 16 * uses, None)
            sl[i][1] = uses + 1
            tok = (sem, 16 * (uses + 1), eng, True)
            inc = (sem, 16)
        else:
            self.count[eng] += 1
            tok = (self.sem[eng], self.count[eng], eng, False)
            inc = (self.sem[eng], 1)
        waits = []
        wd = self.waited[eng]
        for sid, (sem, val) in deps.items():
            if wd.get(sid, 0) >= val:
                continue
            wd[sid] = val
            waits.append((sem, val))
        op = Op()
        op.eng = eng
        op.fn = fn
        op.waits = waits
        op.inc = inc
        self.ops[eng].append(op)
        self.n_ops += 1
        for key, space in rkeys:
            lst = self.readers.setdefault(key, [])
            for n_, t_ in enumerate(lst):
                if t_[0] is tok[0]:
                    lst[n_] = tok
                    break
            else:
                lst.append(tok)
        for key, space in wkeys:
            if space == "dram":
                self.dram_w.setdefault(key, []).append(tok)
                continue
            if rg is not None and space == "ps":
                self.pe_rg[key] = (rg, tok)
            self.last_w[key] = tok
            self.readers[key] = []
        return tok

    def barrier(self):
        for e in ENGS:
            waits = []
            wd = self.waited[e]
            for o in ENGS:
                if o == e or self.count[o] == 0:
                    continue
                sem = self.sem[o]
                if wd.get(id(sem), 0) < self.count[o]:
                    wd[id(sem)] = self.count[o]
                    waits.append((sem, self.count[o]))
            for q, sl in self.slots.items():
                for sem, uses in sl:
                    if uses > 0 and wd.get(id(sem), 0) < 16 * uses:
                        wd[id(sem)] = 16 * uses
                        waits.append((sem, 16 * uses))
            if waits:
                op = Op()
                op.eng = e
                op.fn = None
                op.waits = waits
                op.inc = None
                self.ops[e].append(op)
        self.last_w = {}
        self.readers = {}
        self.dram_w = {}

    @staticmethod
    def _a(x):
        return x.ap if isinstance(x, V) else x

    def mm(self, out, lhsT, rhs, start=True, stop=True, **kw):
        o, l, r = out.ap, lhsT.ap, rhs.ap
        rg = (int(l.start_partition()), int(l.shape[0]))
        return self._record("pe", lambda e: e.matmul(o, l, r, start=start, stop=stop, **kw), [lhsT, rhs], [out], rg=rg)

    def tr(self, out, in_, ident):
        o, i, d = out.ap, in_.ap, ident.ap
        rg = (int(i.start_partition()), int(i.shape[0]))
        return self._record("pe", lambda e: e.transpose(o, i, d), [in_, ident], [out], rg=rg)

    def act(self, out, in_, func, bias=None, scale=1.0, accum=None, eng="act"):
        o, i = out.ap, in_.ap
        kw = {}
        reads = [in_]
        if bias is not None:
            kw["bias"] = self._a(bias)
            if isinstance(bias, V):
                reads.append(bias)
        if isinstance(scale, V):
            reads.append(scale)
        kw["scale"] = self._a(scale)
        writes = [out]
        if accum is not None:
            kw["accum_out"] = accum.ap
            writes.append(accum)
        return self._record(eng, lambda e: e.activation(o, i, func, **kw), reads, writes)

    def tt(self, out, a, b, op, eng="dve"):
        o, x, y = out.ap, a.ap, b.ap
        return self._record(eng, lambda e: e.tensor_tensor(o, x, y, op), [a, b], [out])

    def ts(self, out, a, s1, s2=None, op0=ALU.mult, op1=None, eng="dve", accum=None):
        o, x = out.ap, a.ap
        reads = [a]
        for s in (s1, s2):
            if isinstance(s, V):
                reads.append(s)
        a1, a2 = self._a(s1), self._a(s2)
        kw = {}
        writes = [out]
        if accum is not None:
            kw["accum_out"] = accum.ap
            writes.append(accum)
        if op1 is None:
            return self._record(eng, lambda e: e.tensor_scalar(o, x, a1, None, op0, **kw), reads, writes)
        return self._record(eng, lambda e: e.tensor_scalar(o, x, a1, a2, op0, op1, **kw), reads, writes)

    def stt(self, out, a, scalar, b, op0, op1, eng="dve"):
        o, x, y = out.ap, a.ap, b.ap
        reads = [a, b]
        if isinstance(scalar, V):
            reads.append(scalar)
        s = self._a(scalar)
        return self._record(eng, lambda e: e.scalar_tensor_tensor(o, x, s, y, op0, op1), reads, [out])

    def copy(self, out, in_, eng="dve"):
        o, i = out.ap, in_.ap
        if eng == "act":
            return self._record(eng, lambda e: e.copy(o, i), [in_], [out])
        return self._record(eng, lambda e: e.tensor_copy(o, i), [in_], [out])

    def memset(self, out, val, eng="dve"):
        o = out.ap
        return self._record(eng, lambda e: e.memset(o, val), [], [out])

    def reduce(self, out, in_, op, axis=AX.X, eng="dve"):
        o, i = out.ap, in_.ap
        return self._record(eng, lambda e: e.tensor_reduce(o, i, axis, op), [in_], [out])

    def recip(self, out, in_):
        o, i = out.ap, in_.ap
        return self._record("dve", lambda e: e.reciprocal(o, i), [in_], [out])

    def generic(self, eng, fn, reads, writes):
        return self._record(eng, fn, reads, writes)

    def dma(self, out, in_, q="sp", **kw):
        o, i = out.ap, in_.ap
        return self._record(q, lambda e: e.dma_start(o, i, **kw), [in_], [out], dma_q=q)

    def check_deadlock(self):
        ptr = {e: 0 for e in ENGS}
        semv = {}
        progress = True
        while progress:
            progress = False
            for e in ENGS:
                lst = self.ops[e]
                while ptr[e] < len(lst):
                    op = lst[ptr[e]]
                    if all(semv.get(id(sem), 0) >= val for sem, val in op.waits):
                        if op.fn is not None:
                            semv[id(op.inc[0])] = semv.get(id(op.inc[0]), 0) + op.inc[1]
                        ptr[e] += 1
                        progress = True
                    else:
                        break
        stuck = {e: (ptr[e], len(self.ops[e])) for e in ENGS if ptr[e] < len(self.ops[e])}
        if stuck:
            raise RuntimeError(f"semaphore deadlock: {stuck}")

    def emit(self):
        nc = self.nc
        self.barrier()
        self.check_deadlock()
        ops = self.ops
        with nc.Block() as block:
            def run(eng_obj, lst):
                for op in lst:
                    for sem, val in op.waits:
                        eng_obj.wait_ge(sem, val)
                    if op.fn is not None:
                        ins = op.fn(eng_obj)
                        ins.then_inc(op.inc[0], op.inc[1])

            @block.tensor
            def _(e):
                run(e, ops["pe"])

            @block.scalar
            def _(e):
                run(e, ops["act"])

            @block.vector
            def _(e):
                run(e, ops["dve"])

            @block.gpsimd
            def _(e):
                run(e, ops["pool"])

            @block.sync
            def _(e):
                run(e, ops["sp"])
        self.stack.close()

T = 4096
D = 1024
NT = T // 128
EPS = 1e-6
EV_COLS = 3344
OD_COLS = 2608
FF = 2816
NKC = D // 128


class Ctx:
    pass


def _rows(ap2d):
    return ap2d.rearrange("(kc p) n -> p kc n", p=128)


def phase_consts(k, cx):
    cx.ident = k.sb("ident", [128, 128], BF16)
    cx.identf = k.sb("identf", [128, 128], F32)
    k.dma(cx.identf[:], cx.inp["c_ident"][:])
    k.copy(cx.ident[:], cx.identf[:])
    cx.ones_row = k.sb("ones_row", [1, 128], F32)
    k.memset(cx.ones_row[:], 1.0)


def phase_mod(k, cx, layer):
    inp = cx.inp
    bc = {}
    for nm in ("gsc1", "sh1", "g1", "gsc2", "sh2", "g2"):
        bc[nm] = k.sb(f"bc_{nm}", [128, D], F32)
    k.push()
    ccol = k.sb("ccol", [128, NKC], F32)
    k.dma(ccol[:], inp["c_col"][:])
    cond = k.sb("cond", [128, NKC], F32)
    k.act(cond[:], ccol[:], AF.Silu)
    modrow = k.sb("modrow", [1, 6 * D], F32)
    brow = k.sb("brow", [1, 6 * D], F32)
    k.dma(brow[:], inp["ada_b"][layer:layer + 1, :])
    g12 = k.sb("g12", [1, 2 * D], F32)
    k.dma(g12[:, 0:D], inp["norm1_g"][layer:layer + 1, :])
    k.dma(g12[:, D:2 * D], inp["norm2_g"][layer:layer + 1, :])
    wst = [k.sb(f"adast{i}", [128, NKC, 512], F32) for i in range(2)]
    pm = [k.ps(f"pmod{i}", [128, 512], F32) for i in range(2)]
    aw = _rows(inp["ada_w"][layer])
    for blk in range(12):
        w = wst[blk % 2]
        k.dma(w[:], aw[:, :, blk * 512:(blk + 1) * 512])
        p = pm[blk % 2]
        for kc in range(NKC):
            k.mm(p[0:1, :], cond[:, kc:kc + 1], w[:, kc, :], start=(kc == 0), stop=(kc == NKC - 1))
        k.tt(modrow[:, blk * 512:(blk + 1) * 512], p[0:1, :], brow[:, blk * 512:(blk + 1) * 512], ALU.add)
    for which, (sc_off, goff) in enumerate(((1, 0), (4, 1))):
        k.ts(modrow[:, sc_off * D:(sc_off + 1) * D], modrow[:, sc_off * D:(sc_off + 1) * D], 1.0, None, ALU.add)
        k.tt(modrow[:, sc_off * D:(sc_off + 1) * D], modrow[:, sc_off * D:(sc_off + 1) * D], g12[:, goff * D:(goff + 1) * D], ALU.mult)
    order = ("sh1", "gsc1", "g1", "sh2", "gsc2", "g2")
    i = 0
    for j, nm in enumerate(order):
        for half in range(2):
            p = pm[i % 2]
            i += 1
            k.mm(p[:], cx.ones_row[:], modrow[:, j * D + half * 512: j * D + (half + 1) * 512])
            k.copy(bc[nm][:, half * 512:(half + 1) * 512], p[:], eng=("act" if half else "dve"))
    k.pop()
    return bc


def load_weight_bf16(k, w_rows, ncols, name, col0=0):
    nkc = w_rows.shape[1]
    wb = k.sb(name, [128, nkc, ncols], BF16)
    k.push()
    st = [k.sb(f"{name}_st{i}", [128, ncols], F32) for i in range(2)]
    engs = ("dve", "pool", "act")
    for kc in range(nkc):
        s = st[kc % 2]
        k.dma(s[:], w_rows[:, kc, col0:col0 + ncols])
        k.copy(wb[:, kc, :], s[:], eng=engs[kc % 3])
    k.pop()
    return wb


class NormFront:
    def __init__(self, k, cx, gsc, sh):
        self.k, self.cx, self.gsc, self.sh = k, cx, gsc, sh
        self.xt = [k.sb(f"nf_x{i}", [128, D], F32) for i in range(2)]
        self.xm = [k.sb(f"nf_xm{i}", [128, D], F32) for i in range(2)]
        self.xb = [k.sb(f"nf_xb{i}", [128, D], BF16) for i in range(2)]
        self.junk = k.sb("nf_junk", [128, D], BF16)
        self.ss = [k.sb(f"nf_ss{i}", [128, 2], F32) for i in range(2)]
        self.pT = [k.ps(f"nf_pT{i}", [128, D], BF16) for i in range(2)]
        self.i = 0

    def tile(self, x_src, dst):
        k, cx = self.k, self.cx
        i = self.i % 2
        self.i += 1
        xt, xm, xb, ss, pT = self.xt[i], self.xm[i], self.xb[i], self.ss[i], self.pT[i]
        k.dma(xt[:], x_src)
        k.memset(ss[:], 0.0, eng="pool")
        k.act(self.junk[:], xt[:], AF.Square, accum=ss[:, 0:1])
        k.act(ss[:, 1:2], ss[:, 0:1], AF.Sqrt, bias=EPS, scale=1.0 / D)
        k.recip(ss[:, 1:2], ss[:, 1:2])
        k.stt(xm[:], xt[:], ss[:, 1:2], self.gsc[:], ALU.mult, ALU.mult)
        k.tt(xb[:], xm[:], self.sh[:], ALU.add, eng="pool")
        for kc in range(NKC):
            k.tr(pT[:, kc * 128:(kc + 1) * 128], xb[:, kc * 128:(kc + 1) * 128], cx.ident[:])
        k.copy(dst[:, 0:4, :], pT[:, 0:512].rearrange("p (k t) -> p k t", k=4), eng="act")
        k.copy(dst[:, 4:8, :], pT[:, 512:1024].rearrange("p (k t) -> p k t", k=4), eng="dve")
        return xt


class Evac:
    def __init__(self, k, name, shape, dtype, n=3):
        self.k = k
        self.bufs = [k.sb(f"{name}{i}", shape, dtype) for i in range(n)]
        self.i = 0

    def next(self):
        b = self.bufs[self.i % len(self.bufs)]
        self.i += 1
        return b


def phase_proj(k, cx, x_in, w_dram, ncols, bc_gsc, bc_sh, fm_specs, tm_specs, name):
    k.push()
    wb = load_weight_bf16(k, _rows(w_dram), ncols, name + "_wb")
    nf = NormFront(k, cx, bc_gsc, bc_sh)
    xnT = [k.sb(f"{name}_xnT{i}", [128, NKC, 512], BF16) for i in range(2)]
    pmm = [k.ps(f"{name}_pm{i}", [128, 512], F32) for i in range(6)]
    ev32 = Evac(k, name + "_ev32", [128, 512], F32, 3)
    ev16 = Evac(k, name + "_ev16", [128, 512], BF16, 3)
    pi = 0
    ei = 0
    engs = ("act", "dve")
    for tb in range(T // 512):
        xn = xnT[tb % 2]
        for j in range(4):
            t0 = tb * 512 + j * 128
            nf.tile(x_in[t0:t0 + 128, :], xn[:, :, j * 128:(j + 1) * 128])
        for (c0, n, dst, row0, dt, scale) in fm_specs:
            for cc in range(0, n, 128):
                m = min(128, n - cc)
                p = pmm[pi % 6]
                pi += 1
                for kc in range(NKC):
                    k.mm(p[0:m, :], wb[:, kc, c0 + cc:c0 + cc + m], xn[:, kc, :], start=(kc == 0), stop=(kc == NKC - 1))
                e = (ev32 if dt == F32 else ev16).next()
                eng = engs[ei % 2]
                ei += 1
                if eng == "act":
                    k.act(e[0:m, :], p[0:m, :], AF.Copy, scale=scale)
                else:
                    k.ts(e[0:m, :], p[0:m, :], scale, None, ALU.mult)
                k.dma(dst[row0 + cc:row0 + cc + m, tb * 512:(tb + 1) * 512], e[0:m, :], q="pool")
        for (c0, n, dst, col0, dt) in tm_specs:
            for j in range(4):
                t0 = tb * 512 + j * 128
                for cc in range(0, n, 512):
                    m = min(512, n - cc)
                    p = pmm[pi % 6]
                    pi += 1
                    for kc in range(NKC):
                        k.mm(p[:, 0:m], xn[:, kc, j * 128:(j + 1) * 128], wb[:, kc, c0 + cc:c0 + cc + m], start=(kc == 0), stop=(kc == NKC - 1))
                    e = (ev32 if dt == F32 else ev16).next()
                    k.copy(e[:, 0:m], p[:, 0:m], eng=engs[ei % 2])
                    ei += 1
                    k.dma(dst[t0:t0 + 128, col0 + cc:col0 + cc + m], e[:, 0:m], q="pool")
    k.pop()


def phase_gla(k, cx):
    inp = cx.inp
    k.push()
    a_up = k.sb("g_aup", [16, 256], F32)
    k.dma(a_up[:], inp["gla_a_up"][:])
    nab = k.sb("g_nab", [128, 2], F32)
    k.dma(nab[:], inp["gla_a_b_col"][:])
    k.ts(nab[:], nab[:], -1.0, None, ALU.mult)
    gbc = k.sb("g_gbc", [128, 128], F32)
    k.dma(gbc[:], inp["gla_norm_g_bc"][:])
    rmask = k.sb("g_rmask", [128, 512], F32)
    k.dma(rmask[:], inp["c_scan128"][:])
    tri = k.sb("g_tri", [128, 128], F32)
    k.dma(tri[:], inp["c_tri_incl"][:])
    S = [k.sb(f"g_S{i}", [128, 128], F32) for i in range(2)]
    Sbf = [k.sb(f"g_Sbf{i}", [128, 128], BF16) for i in range(2)]
    for i in range(2):
        k.memset(S[i][:], 0.0)
        k.memset(Sbf[i][:], 0.0)
    lrT = [k.sb(f"g_lrT{i}", [16, 512], F32) for i in range(2)]
    qT = [k.sb(f"g_qT{i}", [128, 512], F32) for i in range(2)]
    kT = [k.sb(f"g_kT{i}", [128, 512], F32) for i in range(2)]
    e1 = [k.sb(f"g_e1{i}", [128, 512], F32) for i in range(2)]
    sp = [k.sb(f"g_sp{i}", [128, 512], F32) for i in range(2)]
    bsp = [k.sb(f"g_bsp{i}", [128, 512], F32) for i in range(2)]
    eb = [k.sb(f"g_eb{i}", [128, 512], F32) for i in range(2)]
    ebi = [k.sb(f"g_ebi{i}", [128, 512], F32) for i in range(2)]
    qs = [k.sb(f"g_qs{i}", [128, 512], BF16) for i in range(2)]
    ks = [k.sb(f"g_ks{i}", [128, 512], BF16) for i in range(2)]
    kh = [k.sb(f"g_kh{i}", [128, 128], BF16) for i in range(2)]
    khT = [k.sb(f"g_khT{i}", [128, 128], BF16) for i in range(2)]
    vt = [k.sb(f"g_vt{i}", [128, 256], BF16) for i in range(3)]
    ogt = [k.sb(f"g_ogt{i}", [128, 256], F32) for i in range(3)]
    sil = [k.sb(f"g_sil{i}", [128, 256], F32) for i in range(2)]
    At = [k.sb(f"g_At{i}", [128, 128], BF16) for i in range(3)]
    t1 = [k.sb(f"g_t1{i}", [128, 128], F32) for i in range(2)]
    obuf = [k.sb(f"g_ob{i}", [128, 256], BF16) for i in range(2)]
    junk = k.sb("g_junk", [128, 128], BF16)
    ss = [k.sb(f"g_ss{i}", [128, 2], F32) for i in range(4)]
    pz = k.ps("g_pz", [128, 512], F32)
    pA = [k.ps(f"g_pA{i}", [128, 512], F32) for i in range(2)]
    po = [k.ps(f"g_po{i}", [128, 512], F32) for i in range(2)]
    pkv = k.ps("g_pkv", [128, 512], F32)
    pT = k.ps("g_pT", [128, 1024], BF16)
    ci = 0
    hi = 0
    for tb in range(T // 512):
        tsl = slice(tb * 512, (tb + 1) * 512)
        lr = lrT[tb % 2]
        k.dma(lr[:], cx.G_lr[0:16, tsl])
        for hp in range(2):
            b = (tb * 2 + hp) % 2
            k.dma(qT[b][:], cx.G_qk[hp * 128:(hp + 1) * 128, tsl])
            k.dma(kT[b][:], cx.G_qk[256 + hp * 128:256 + (hp + 1) * 128, tsl])
            k.mm(pz[:], a_up[:, hp * 128:(hp + 1) * 128], lr[:])
            k.act(e1[b][:], pz[:], AF.Exp, bias=nab[:, hp:hp + 1], scale=-1.0)
            k.act(sp[b][:], e1[b][:], AF.Ln, bias=1.0)
            o_, m_, s_ = bsp[b].t[:], rmask.t[:], sp[b].t[:]
            k.generic("dve", lambda e, o_=o_, m_=m_, s_=s_: e.tensor_tensor_scan(o_, m_, s_, 0.0, ALU.mult, ALU.add), [rmask, sp[b]], [bsp[b]])
            k.act(eb[b][:], bsp[b][:], AF.Exp, scale=-1.0 / 16)
            k.act(ebi[b][:], bsp[b][:], AF.Exp, scale=1.0 / 16)
            k.stt(qs[b][:], qT[b][:], 0.125, eb[b][:], ALU.mult, ALU.mult)
            k.tt(ks[b][:], kT[b][:], ebi[b][:], ALU.mult, eng="pool")
            for c in range(4):
                t0 = tb * 512 + c * 128
                cs = slice(c * 128, (c + 1) * 128)
                v = vt[ci % 3]
                og = ogt[ci % 3]
                k.dma(v[:], cx.G_v[t0:t0 + 128, hp * 256:(hp + 1) * 256])
                k.dma(og[:], cx.G_og[t0:t0 + 128, hp * 256:(hp + 1) * 256])
                sl_ = sil[ci % 2]
                k.act(sl_[:], og[:], AF.Silu)
                khc = kh[ci % 2]
                k.ts(khc[:], ks[b][:, cs], eb[b][:, c * 128 + 127:c * 128 + 128], None, ALU.mult)
                k.tr(pT[:, 0:128], khc[:], cx.ident[:])
                kht = khT[ci % 2]
                k.copy(kht[:], pT[:, 0:128], eng="act")
                ob = obuf[ci % 2]
                for hh in range(2):
                    hb = hh * 64
                    a_ps = pA[hi % 2]
                    o_ps = po[hi % 2]
                    at = At[hi % 3]
                    s2 = ss[hi % 4]
                    tt1 = t1[hi % 2]
                    hi += 1
                    k.mm(a_ps[:, 0:128], ks[b][hb:hb + 64, cs], qs[b][hb:hb + 64, cs])
                    k.tt(at[:], a_ps[:, 0:128], tri[:], ALU.mult)
                    k.mm(o_ps[:, 0:128], at[:], v[:, hh * 128:(hh + 1) * 128], start=True, stop=False)
                    k.mm(o_ps[:, 0:128], qs[b][hb:hb + 64, cs], Sbf[hp][hb:hb + 64, :], start=False, stop=True)
                    k.memset(s2[:], 0.0, eng="pool")
                    k.act(junk[:], o_ps[:, 0:128], AF.Square, accum=s2[:, 0:1])
                    k.act(s2[:, 1:2], s2[:, 0:1], AF.Sqrt, bias=EPS, scale=1.0 / 128)
                    k.recip(s2[:, 1:2], s2[:, 1:2])
                    k.stt(tt1[:], o_ps[:, 0:128], s2[:, 1:2], gbc[:], ALU.mult, ALU.mult)
                    k.tt(ob[:, hh * 128:(hh + 1) * 128], tt1[:], sl_[:, hh * 128:(hh + 1) * 128], ALU.mult, eng="pool")
                k.dma(cx.O_mix[t0:t0 + 128, hp * 256:(hp + 1) * 256], ob[:], q="pool")
                k.mm(pkv[:, 0:256], kht[:], v[:])
                ebc = eb[b][:, c * 128 + 127:c * 128 + 128]
                k.stt(S[hp][0:64, :], S[hp][0:64, :], ebc[0:64, :], pkv[0:64, 0:128], ALU.mult, ALU.add)
                k.stt(S[hp][64:128, :], S[hp][64:128, :], ebc[64:128, :], pkv[64:128, 128:256], ALU.mult, ALU.add)
                k.copy(Sbf[hp][:], S[hp][:], eng="act")
                ci += 1
    k.pop()


def phase_wout(k, cx, x_in, x_out, w_dram, g_bc, src, src_mode, name):
    k.push()
    wb = load_weight_bf16(k, _rows(w_dram), D, name + "_wb")
    oT = [k.sb(f"{name}_oT{i}", [128, NKC, 512], BF16) for i in range(2)]
    ot = [k.sb(f"{name}_ot{i}", [128, D], BF16) for i in range(2)]
    xt = [k.sb(f"{name}_x{i}", [128, D], F32) for i in range(2)]
    tmp = [k.sb(f"{name}_tmp{i}", [128, D], F32) for i in range(2)]
    xo = [k.sb(f"{name}_xo{i}", [128, D], F32) for i in range(2)]
    pT = [k.ps(f"{name}_pT{i}", [128, D], BF16) for i in range(2)]
    pm = [k.ps(f"{name}_pm{i}", [128, 512], F32) for i in range(6)]
    ti = 0
    pi = 0
    for tb in range(T // 512):
        o = oT[tb % 2]
        if src_mode == "fm":
            k.dma(o[:], src[:, tb * 512:(tb + 1) * 512].rearrange("(kc p) t -> p kc t", p=128))
        else:
            for j in range(4):
                t0 = tb * 512 + j * 128
                a = ot[(tb * 4 + j) % 2]
                p = pT[(tb * 4 + j) % 2]
                k.dma(a[:], src[t0:t0 + 128, :])
                for kc in range(NKC):
                    k.tr(p[:, kc * 128:(kc + 1) * 128], a[:, kc * 128:(kc + 1) * 128], cx.ident[:])
                k.copy(o[:, 0:4, j * 128:(j + 1) * 128], p[:, 0:512].rearrange("p (k t) -> p k t", k=4), eng="act")
                k.copy(o[:, 4:8, j * 128:(j + 1) * 128], p[:, 512:1024].rearrange("p (k t) -> p k t", k=4), eng="dve")
        for j in range(4):
            t0 = tb * 512 + j * 128
            x = xt[ti % 2]
            tm_ = tmp[ti % 2]
            xo_ = xo[ti % 2]
            ti += 1
            k.dma(x[:], x_in[t0:t0 + 128, :])
            for half in range(2):
                p = pm[pi % 6]
                pi += 1
                for kc in range(NKC):
                    k.mm(p[:], o[:, kc, j * 128:(j + 1) * 128], wb[:, kc, half * 512:(half + 1) * 512], start=(kc == 0), stop=(kc == NKC - 1))
                k.tt(tm_[:, half * 512:(half + 1) * 512], p[:], g_bc[:, half * 512:(half + 1) * 512], ALU.mult)
            k.tt(xo_[:], tm_[:], x[:], ALU.add, eng="pool")
            k.dma(x_out[t0:t0 + 128, :], xo_[:], q="pool")
    k.pop()


def phase_ffn_up(k, cx, layer, x_in, bc):
    inp = cx.inp
    TB = 512
    NF = FF // 128
    k.push()
    wup = load_weight_bf16(k, _rows(inp["ffn_w_up"][layer]), 2 * FF, "f_wup")
    cw = k.sb("f_cw", [128, NF, 4], F32)
    k.dma(cw[:], inp["ffn_conv_col"][layer])
    nf = NormFront(k, cx, bc["gsc2"], bc["sh2"])
    xnT = [k.sb(f"f_xnT{i}", [128, NKC, TB], BF16) for i in range(2)]
    halo = k.sb("f_halo", [128, NF, 2], F32)
    k.memset(halo[:], 0.0)
    ub = [k.sb(f"f_ub{i}", [128, TB + 2], F32) for i in range(3)]
    cv = [k.sb(f"f_cv{i}", [128, TB], F32) for i in range(3)]
    ge = [k.sb(f"f_ge{i}", [128, TB], F32) for i in range(3)]
    hb = [k.sb(f"f_hb{i}", [128, TB], BF16) for i in range(3)]
    pu = [k.ps(f"f_pu{i}", [128, 512], F32) for i in range(3)]
    pv = [k.ps(f"f_pv{i}", [128, 512], F32) for i in range(3)]
    ui = 0
    for tb in range(T // TB):
        xn = xnT[tb % 2]
        for j in range(TB // 128):
            t0 = tb * TB + j * 128
            nf.tile(x_in[t0:t0 + 128, :], xn[:, :, j * 128:(j + 1) * 128])
        pend = None

        def tail(st):
            fc, u, c_, g_, h_, p_v = st
            k.ts(c_[:], u[:, 2:TB + 2], cw[:, fc, 2:3], cw[:, fc, 3:4], ALU.mult, ALU.add, eng="pool")
            k.stt(c_[:], u[:, 1:TB + 1], cw[:, fc, 1:2], c_[:], ALU.mult, ALU.add)
            k.stt(c_[:], u[:, 0:TB], cw[:, fc, 0:1], c_[:], ALU.mult, ALU.add)
            k.act(g_[:], c_[:], AF.Gelu)
            k.tt(h_[:], g_[:], p_v[:, 0:TB], ALU.mult)
            k.dma(cx.H_fm[fc * 128:(fc + 1) * 128, tb * TB:(tb + 1) * TB], h_[:], q="pool")

        for fc in range(NF):
            p_u = pu[ui % 3]
            p_v = pv[ui % 3]
            u = ub[ui % 3]
            c_ = cv[ui % 3]
            g_ = ge[ui % 3]
            h_ = hb[ui % 3]
            ui += 1
            for kc in range(NKC):
                k.mm(p_u[:, 0:TB], wup[:, kc, fc * 128:(fc + 1) * 128], xn[:, kc, :], start=(kc == 0), stop=(kc == NKC - 1))
            for kc in range(NKC):
                k.mm(p_v[:, 0:TB], wup[:, kc, FF + fc * 128:FF + (fc + 1) * 128], xn[:, kc, :], start=(kc == 0), stop=(kc == NKC - 1))
            k.copy(u[:, 0:2], halo[:, fc, :], eng="pool")
            k.copy(u[:, 2:TB + 2], p_u[:, 0:TB], eng="act")
            k.copy(halo[:, fc, :], u[:, TB:TB + 2], eng="pool")
            if pend is not None:
                tail(pend)
            pend = (fc, u, c_, g_, h_, p_v)
        tail(pend)
    k.pop()


def phase_ffn_down(k, cx, layer, x_in, x_out, bc, final=False):
    inp = cx.inp
    TB = 512
    NF = FF // 128
    k.push()
    wdn = load_weight_bf16(k, _rows(inp["ffn_w_down"][layer]), D, "f_wdn")
    hT = [k.sb(f"f_hT{i}", [128, NF, TB], BF16) for i in range(2)]
    pd = [k.ps(f"f_pd{i}", [128, 512], F32) for i in range(8)]
    xt = [k.sb(f"f_x{i}", [128, D], F32) for i in range(2)]
    tmp = [k.sb(f"f_tmp{i}", [128, D], F32) for i in range(2)]
    if final:
        fg = k.sb("f_fg", [128, D], F32)
        k.dma(fg[:], inp["final_norm_g_bc"][:])
        fss = [k.sb(f"f_fss{i}", [128, 2], F32) for i in range(2)]
        fjunk = k.sb("f_fjunk", [128, D], BF16)
    ti = 0
    di = 0
    for tb in range(T // TB):
        h = hT[tb % 2]
        k.dma(h[:], cx.H_fm[:, tb * TB:(tb + 1) * TB].rearrange("(fc p) t -> p fc t", p=128))
        for j in range(TB // 128):
            t0 = tb * TB + j * 128
            x = xt[ti % 2]
            tm_ = tmp[ti % 2]
            k.dma(x[:], x_in[t0:t0 + 128, :])
            for half in range(2):
                p = pd[di % 8]
                di += 1
                for fc in range(NF):
                    k.mm(p[:], h[:, fc, j * 128:(j + 1) * 128], wdn[:, fc, half * 512:(half + 1) * 512], start=(fc == 0), stop=(fc == NF - 1))
                k.tt(tm_[:, half * 512:(half + 1) * 512], p[:], bc["g2"][:, half * 512:(half + 1) * 512], ALU.mult)
            k.tt(x[:], tm_[:], x[:], ALU.add, eng="pool")
            if not final:
                k.dma(x_out[t0:t0 + 128, :], x[:], q="pool")
            else:
                s2 = fss[ti % 2]
                k.memset(s2[:], 0.0, eng="pool")
                k.act(fjunk[:], x[:], AF.Square, accum=s2[:, 0:1])
                k.act(s2[:, 1:2], s2[:, 0:1], AF.Sqrt, bias=EPS, scale=1.0 / D)
                k.recip(s2[:, 1:2], s2[:, 1:2])
                k.stt(tm_[:], x[:], s2[:, 1:2], fg[:], ALU.mult, ALU.mult)
                k.dma(x_out[t0:t0 + 128, :], tm_[:], q="pool")
            ti += 1
    k.pop()


def phase_rwkv(k, cx):
    inp = cx.inp
    LG = 0.6065306597126334
    GN_EPS = 64e-5
    k.push()

    def const(name, shape, dtype=F32, src=None):
        t = k.sb("rc_" + name, shape, F32)
        k.dma(t[:], inp[src or ("rw_" + name)][:])
        if dtype == F32:
            return t
        tb_ = k.sb("rcb_" + name, shape, dtype)
        k.copy(tb_[:], t[:])
        return tb_

    mu = const("mu_col", [128, 10])
    omu = k.sb("rc_omu", [128, 10], F32)
    k.ts(omu[:], mu[:], -1.0, 1.0, ALU.mult, ALU.add)
    mu_k = const("mu_k_col", [128, 4])
    omu_k = k.sb("rc_omu_k", [128, 4], F32)
    k.ts(omu_k[:], mu_k[:], -1.0, 1.0, ALU.mult, ALU.add)
    mu_al = const("mu_al_col", [64, 1])
    omu_al = k.sb("rc_omu_al", [64, 1], F32)
    k.ts(omu_al[:], mu_al[:], -1.0, 1.0, ALU.mult, ALU.add)
    muv = const("muv_bc", [128, 512])
    omuv = k.sb("rc_omuv", [128, 512], F32)
    k.ts(omuv[:], muv[:], -1.0, 1.0, ALU.mult, ALU.add)
    w0 = const("w0_col", [128, 4])
    a0 = const("a0_col", [128, 4])
    kkc = const("k_k_col", [128, 4])
    kac = const("k_a_col", [128, 4])
    okac = k.sb("rc_okac", [128, 4], F32)
    k.ts(okac[:], kac[:], -1.0, 1.0, ALU.mult, ALU.add)
    w2 = const("w2", [64, 512])
    a2 = const("a2", [64, 512])
    g2b = const("g2", [128, 512], BF16)
    rkcol = const("rkcol", [128, 4, 2])
    gnw = const("gn_w_bc", [128, 512])
    gnb = const("gn_b_bc", [128, 512])
    bones = const("blockones", [128, 128], src="c_blockones")
    negblk = const("negblock", [128, 128], src="c_negblock")
    scan64 = const("scan64", [128, 512], src="c_scan64")
    MK1 = const("mk1", [128, 256], src="c_mk1")
    MK2 = const("mk2", [128, 256], src="c_mk2")
    NMT = const("nmt", [128, 128], src="c_nmt")
    identf = cx.identf
    ident = cx.ident

    def f32t(name, shape=(128, 512)):
        return k.sb("r_" + name, list(shape), F32)

    rraw, kraw = f32t("rraw", (128, 513)), f32t("kraw", (128, 513))
    wlraw, alraw, glraw = f32t("wlraw", (64, 513)), f32t("alraw", (64, 513)), f32t("glraw", (128, 513))
    wls, als, gls, twl = f32t("wls", (64, 512)), f32t("als", (64, 512)), f32t("gls", (128, 512)), f32t("twl", (64, 512))
    sgls = [k.sb(f"r_sgl{i}", [128, 512], BF16) for i in range(2)]
    rs, ks, sg, csg, dd = f32t("rs"), f32t("ks"), f32t("sg"), f32t("csg"), f32t("dd")
    E1s = [f32t("E1a"), f32t("E1b")]
    E2, E3, aa = f32t("E2"), f32t("E3"), f32t("aa")
    kk, sq, rn, kkn, t1, kmod, beta, rk = f32t("kk"), f32t("sq"), f32t("rn"), f32t("kkn"), f32t("t1"), f32t("kmod"), f32t("beta"), f32t("rk")
    ARs = [k.sb(f"r_AR{i}", [128, 4, 2, 128], BF16) for i in range(2)]
    kbTs = [k.sb(f"r_kbT{i}", [128, 512], BF16) for i in range(2)]
    bbTs = [k.sb(f"r_bbT{i}", [128, 512], BF16) for i in range(2)]
    KGs = [k.sb(f"r_KG{i}", [128, 512], BF16) for i in range(2)]
    BGs = [k.sb(f"r_BG{i}", [128, 512], BF16) for i in range(2)]
    vraw = [f32t(f"vraw{i}") for i in range(2)]
    vprev = [f32t(f"vprev{i}") for i in range(2)]
    vs = [k.sb(f"r_vs{i}", [128, 4, 512], F32) for i in range(2)]
    vb = [k.sb(f"r_vb{i}", [128, 4, 512], BF16) for i in range(2)]
    RZ = [k.sb(f"r_RZ{i}", [128, 4, 2, 128], BF16) for i in range(2)]
    for t_ in RZ:
        k.memset(t_[:], 0.0)
    Y0s = [k.sb(f"r_Y0s{i}", [128, 4, 2, 64], F32) for i in range(2)]
    Gs = [k.sb(f"r_Gs{i}", [128, 8, 64], F32) for i in range(2)]
    MT = [k.sb(f"r_MT{i}", [128, 8, 128], BF16) for i in range(2)]
    Hst = [k.sb(f"r_Hst{i}", [128, 8, 64], BF16) for i in range(2)]
    Hcur = [k.sb(f"r_Hcur{i}", [128, 64], BF16) for i in range(4)]
    for t_ in Hcur:
        k.memset(t_[:], 0.0)
    bsc = [k.sb(f"r_bsc{i}", [128, 4, 2], F32) for i in range(2)]
    tokT = [k.sb(f"r_tokT{i}", [128, 2, 128], BF16) for i in range(2)]
    AZ = [[k.sb(f"r_AZ{i}{h}", [128, 128], BF16) for h in range(2)] for i in range(2)]
    C1 = [k.sb(f"r_C1{h}", [128, 256], BF16) for h in range(2)]
    C2 = [k.sb(f"r_C2{h}", [128, 256], BF16) for h in range(2)]
    Ln = [[k.sb(f"r_Ln{h}{i}", [128, 128], BF16) for i in range(2)] for h in range(2)]
    LTb = [[k.sb(f"r_LT{h}{i}", [128, 128], BF16) for i in range(2)] for h in range(2)]
    QT = [[k.sb(f"r_QT{h}{i}", [128, 128], BF16) for i in range(2)] for h in range(2)]
    WUp = [k.sb(f"r_WU{i}", [128, 2, 2, 64], BF16) for i in range(2)]
    tmpM = k.sb("r_tmpM", [128, 128], F32)
    yt = [k.sb(f"r_yt{i}", [128, 2, 64], F32) for i in range(2)]
    ysq = k.sb("r_ysq", [128, 2, 64], F32)
    st = [k.sb(f"r_st{i}", [128, 8], F32) for i in range(2)]
    yn = [k.sb(f"r_yn{i}", [128, 2, 64], F32) for i in range(2)]
    ob = [k.sb(f"r_ob{i}", [128, 128], BF16) for i in range(2)]
    B = [k.ps(f"r_B{i}", [128, 512], F32) for i in range(7)]
    BT = k.ps("r_BT", [128, 1024], BF16)
    oi = 0
    lim_tb, lim_hp, lim_stage = getattr(cx, "rw_limit", (T // 512, 4, 99))

    def shared_gen(tb):
        sgl = sgls[tb % 2]
        t00 = tb * 512
        def load_halo(dst, row0, nrows):
            if tb == 0:
                k.memset(dst[0:nrows, 0:1], 0.0)
                k.dma(dst[0:nrows, 1:513], cx.R_fm[row0:row0 + nrows, 0:512])
            else:
                k.dma(dst[0:nrows, :], cx.R_fm[row0:row0 + nrows, t00 - 1:t00 + 512])

        def shift(dst, raw, n, mcol):
            k.ts(dst[0:n, :], raw[0:n, 1:513], omu[0:n, mcol:mcol + 1], None, ALU.mult, eng="pool")
            k.stt(dst[0:n, :], raw[0:n, 0:512], mu[0:n, mcol:mcol + 1], dst[0:n, :], ALU.mult, ALU.add)

        load_halo(wlraw, 512, 64)
        load_halo(alraw, 1088, 64)
        load_halo(glraw, 1152, 128)
        k.ts(wls[:], wlraw[:, 1:513], omu[0:64, 4:5], None, ALU.mult, eng="pool")
        k.stt(wls[:], wlraw[:, 0:512], mu[0:64, 4:5], wls[:], ALU.mult, ALU.add)
        k.ts(als[:], alraw[:, 1:513], omu_al[:, 0:1], None, ALU.mult, eng="pool")
        k.stt(als[:], alraw[:, 0:512], mu_al[:, 0:1], als[:], ALU.mult, ALU.add)
        shift(gls, glraw, 128, 9)
        k.act(twl[:], wls[:], AF.Tanh)
        yield
        k.act(sgl[:], gls[:], AF.Sigmoid)
        yield
        vsb, vbb = vs[tb % 2], vb[tb % 2]
        for j in range(4):
            t0 = t00 + j * 128
            vr, vp = vraw[j % 2], vprev[j % 2]
            k.dma(vr[:], cx.R_v[t0:t0 + 128, :])
            if t0 == 0:
                k.memset(vp[0:1, :], 0.0)
                k.dma(vp[1:128, :], cx.R_v[0:127, :])
            else:
                k.dma(vp[:], cx.R_v[t0 - 1:t0 + 127, :])
            k.tt(vsb[:, j, :], vr[:], omuv[:], ALU.mult, eng="pool")
            k.tt(vp[:], vp[:], muv[:], ALU.mult, eng="pool")
            k.tt(vsb[:, j, :], vsb[:, j, :], vp[:], ALU.add, eng="pool")
            k.copy(vbb[:, j, :], vsb[:, j, :], eng="act")
            yield

    def pre_gen(tb, hp):
        t00 = tb * 512

        def load_halo(dst, row0, nrows):
            if tb == 0:
                k.memset(dst[0:nrows, 0:1], 0.0)
                k.dma(dst[0:nrows, 1:513], cx.R_fm[row0:row0 + nrows, 0:512])
            else:
                k.dma(dst[0:nrows, :], cx.R_fm[row0:row0 + nrows, t00 - 1:t00 + 512])
        w = (tb * 4 + hp) % 2
        rz, y0s, gs, mt, hst, bs = RZ[w], Y0s[w], Gs[w], MT[w], Hst[w], bsc[w]
        AR, kbT, bbT, KG, BG, E1 = ARs[w], kbTs[w], bbTs[w], KGs[w], BGs[w], E1s[w]
        sgl = sgls[tb % 2]
        vsb, vbb = vs[tb % 2], vb[tb % 2]
        load_halo(rraw, hp * 128, 128)
        load_halo(kraw, 576 + hp * 128, 128)
        k.ts(rs[:], rraw[:, 1:513], omu[:, hp:hp + 1], None, ALU.mult)
        k.stt(rs[:], rraw[:, 0:512], mu[:, hp:hp + 1], rs[:], ALU.mult, ALU.add)
        k.ts(ks[:], kraw[:, 1:513], omu_k[:, hp:hp + 1], None, ALU.mult)
        k.stt(ks[:], kraw[:, 0:512], mu_k[:, hp:hp + 1], ks[:], ALU.mult, ALU.add)
        yield
        k.mm(B[0][:], w2[:, hp * 128:(hp + 1) * 128], twl[:])
        k.act(sg[:], B[0][:], AF.Sigmoid, bias=w0[:, hp:hp + 1])
        o_, m_, s_ = csg.t[:], scan64.t[:], sg.t[:]
        k.generic("dve", lambda e, o_=o_, m_=m_, s_=s_: e.tensor_tensor_scan(o_, m_, s_, 0.0, ALU.mult, ALU.add), [scan64, sg], [csg])
        k.act(E1[:], csg[:], AF.Exp, scale=-LG)
        k.act(E2[:], csg[:], AF.Exp, scale=LG)
        yield
        k.tt(dd[:], csg[:], sg[:], ALU.subtract, eng="pool")
        k.act(E3[:], dd[:], AF.Exp, scale=-LG)
        k.mm(B[0][:], a2[:, hp * 128:(hp + 1) * 128], als[:])
        k.act(aa[:], B[0][:], AF.Sigmoid, bias=a0[:, hp:hp + 1])
        yield
        k.ts(kk[:], ks[:], kkc[:, hp:hp + 1], None, ALU.mult)
        k.tt(sq[:], kk[:], kk[:], ALU.mult, eng="pool")
        k.mm(B[0][:], bones[:], sq[:])
        k.ts(rn[:], B[0][:], 1e-24, None, ALU.max)
        yield
        k.act(rn[:], rn[:], AF.Ln)
        k.act(rn[:], rn[:], AF.Exp, scale=-0.5)
        k.tt(kkn[:], kk[:], rn[:], ALU.mult)
        k.ts(t1[:], aa[:], kac[:, hp:hp + 1], okac[:, hp:hp + 1], ALU.mult, ALU.add, eng="pool")
        yield
        k.tt(kmod[:], ks[:], t1[:], ALU.mult, eng="pool")
        k.tt(beta[:], aa[:], kkn[:], ALU.mult, eng="pool")
        k.tt(AR[:, :, 0, :], kkn[:].rearrange("p (j t) -> p j t", j=4), E3[:].rearrange("p (j t) -> p j t", j=4), ALU.mult)
        k.tt(AR[:, :, 1, :], rs[:].rearrange("p (j t) -> p j t", j=4), E1[:].rearrange("p (j t) -> p j t", j=4), ALU.mult)
        yield
        k.tt(kbT[:], kmod[:], E2[:], ALU.mult)
        k.tt(bbT[:], beta[:], E2[:], ALU.mult, eng="pool")
        for c in range(8):
            csl = slice(c * 64, (c + 1) * 64)
            gcol = E1[:, c * 64 + 63:c * 64 + 64]
            k.ts(KG[:, csl], kbT[:, csl], gcol, None, ALU.mult, eng="pool")
            k.ts(BG[:, csl], bbT[:, csl], gcol, None, ALU.mult, eng="pool")
        k.tt(rk[:], rs[:], kmod[:], ALU.mult, eng="pool")
        for j in range(4):
            jsl = slice(j * 128, (j + 1) * 128)
            k.mm(B[0][:, j * 2:j * 2 + 2], rk[:, jsl], rkcol[:, hp, :])
        k.copy(bs[:], B[0][:, 0:8].rearrange("p (j h) -> p j h", j=4), eng="act")
        yield
        yield

    def main_gen(tb, hp):
        nonlocal oi
        t00 = tb * 512
        w = (tb * 4 + hp) % 2
        rz, y0s, gs, mt, hst, bs = RZ[w], Y0s[w], Gs[w], MT[w], Hst[w], bsc[w]
        AR, kbT, bbT, KG, BG, E1 = ARs[w], kbTs[w], bbTs[w], KGs[w], BGs[w], E1s[w]
        sgl = sgls[tb % 2]
        vsb, vbb = vs[tb % 2], vb[tb % 2]
        for j in range(4):
            jsl = slice(j * 128, (j + 1) * 128)
            tk = tokT[j % 2]
            az = AZ[j % 2]
            wu = WUp[j % 2]
            k.tr(BT[:, 0:128], AR[:, j, 0, :], ident[:])
            k.tr(BT[:, 128:256], KG[:, jsl], ident[:])
            k.tr(BT[:, 256:384], BG[:, jsl], ident[:])
            k.copy(az[0][:, 0:64], BT[:, 0:64], eng="act")
            k.copy(az[1][:, 0:64], BT[:, 64:128], eng="act")
            k.copy(tk[:], BT[:, 128:384].rearrange("p (a b) -> p a b", a=2), eng="act")
            def head_stream(h):
                hb = h * 64
                bk = B[1 + h]
                bi = B[3 + h]
                vh = vbb[:, j, hp * 128 + h * 64:hp * 128 + (h + 1) * 64]
                k.mm(bk[:, 0:256], kbT[hb:hb + 64, jsl], AR[hb:hb + 64, j, :, :].rearrange("p a t -> p (a t)"))
                k.tt(C1[h][:], bk[:, 0:256], MK1[:], ALU.mult)
                k.mm(bk[:, 256:512], bbT[hb:hb + 64, jsl], AR[hb:hb + 64, j, :, :].rearrange("p a t -> p (a t)"))
                k.tt(C2[h][:], bk[:, 256:512], MK2[:], ALU.mult)
                k.mm(bi[:, 0:128], AR[hb:hb + 64, j, 0, :], bbT[hb:hb + 64, jsl])
                k.tt(Ln[h][0][:], bi[:, 0:128], NMT[:], ALU.mult)
                yield
                if lim_stage < 3:
                    return
                lt = C2[h][:, 0:128]
                qt = QT[h][0]
                k.tt(qt[:], lt, ident[:], ALU.add, eng="pool")
                lp = Ln[h][0][:]
                lpt = lt
                for i in range(5):
                    lp2 = Ln[h][(i + 1) % 2]
                    k.mm(bi[:, 128:256], lpt, lp)
                    if i < 4:
                        lpt2 = LTb[h][i % 2]
                        k.mm(bi[:, 256:384], lp, lpt)
                    k.copy(lp2[:], bi[:, 128:256], eng="act")
                    if i < 4:
                        k.copy(lpt2[:], bi[:, 256:384], eng="act")
                    yield
                    qn = QT[h][(i + 1) % 2]
                    k.mm(bi[:, 384:512], lp2[:], qt[:])
                    k.tt(qn[:], bi[:, 384:512], qt[:], ALU.add)
                    yield
                    qt = qn
                    lp = lp2[:]
                    if i < 4:
                        lpt = lpt2[:]
                if lim_stage < 4:
                    return
                k.mm(bk[:, 0:64], C1[h][:, 0:128], vh)
                k.copy(az[h][:, 64:128], bk[:, 0:64], eng="act")
                yield
                k.mm(bk[:, 64:192], qt[:], az[h][:])
                k.copy(wu[:, 0, h, :], bk[:, 64:128], eng="act")
                k.ts(wu[:, 1, h, :], bk[:, 128:192], -1.0, None, ALU.mult)
                yield
                k.mm(bk[:, 192:256], C1[h][:, 128:256], vh, start=True, stop=False)
                k.mm(bk[:, 192:256], C2[h][:, 128:256], wu[:, 1, h, :], start=False, stop=True)
                k.copy(y0s[:, j, h, :], bk[:, 192:256], eng="act")

            gens = [head_stream(0), head_stream(1)]
            while gens:
                for g_ in list(gens):
                    try:
                        next(g_)
                    except StopIteration:
                        gens.remove(g_)
                yield
            if lim_stage < 5:
                continue
            bp = B[5]
            wa = wu[:, 0, :, :].rearrange("p h k -> p (h k)")
            k.mm(bp[:, 0:128], wa, C2[0][:, 128:256])
            k.mm(bp[:, 128:256], wa, C2[1][:, 128:256])
            for h in range(2):
                hb = h * 64
                for c in range(2):
                    cs = slice(c * 64, (c + 1) * 64)
                    k.tt(rz[hb:hb + 64, j, c, cs], AR[hb:hb + 64, j, 1, cs], bp[hb:hb + 64, h * 128 + c * 64:h * 128 + (c + 1) * 64], ALU.subtract)
            for c in range(2):
                cb = c * 64
                cc = j * 2 + c
                k.mm(bp[:, 256:384], tk[cb:cb + 64, 0, :], vbb[cb:cb + 64, j, hp * 128:(hp + 1) * 128], start=True, stop=False)
                k.mm(bp[:, 256:384], tk[cb:cb + 64, 1, :], wu[cb:cb + 64, 1, :, :].rearrange("p h k -> p (h k)"), start=False, stop=True)
                k.copy(gs[0:64, cc, :], bp[0:64, 256:320], eng="act")
                k.copy(gs[64:128, cc, :], bp[64:128, 320:384], eng="act")
                k.mm(bp[:, 384:512], wu[cb:cb + 64, 0, :, :].rearrange("p h k -> p (h k)"), tk[cb:cb + 64, 1, :])
                k.tt(tmpM[:], bp[:, 384:512], negblk[:], ALU.mult)
                k.stt(mt[:, cc, :], identf[:], E1[:, cc * 64 + 63:cc * 64 + 64], tmpM[:], ALU.mult, ALU.add)
        if lim_stage < 6:
            return
        bh = B[6]
        k.copy(hst[:, 0, :], Hcur[hp][:], eng="pool")
        for c in range(8):
            k.mm(bh[:, 0:64], mt[:, c, :], hst[:, c, :])
            if c < 7:
                k.tt(hst[:, c + 1, :], bh[:, 0:64], gs[:, c, :], ALU.add)
            else:
                k.tt(Hcur[hp][:], bh[:, 0:64], gs[:, c, :], ALU.add)
        for j in range(4 if lim_stage >= 7 else 0):
            t0 = t00 + j * 128
            jsl = slice(j * 128, (j + 1) * 128)
            y = yt[oi % 2]
            s_ = st[oi % 2]
            yn_ = yn[oi % 2]
            o_b = ob[oi % 2]
            oi += 1
            for h in range(2):
                hb = h * 64
                k.mm(bh[:, 64 + h * 64:128 + h * 64], rz[hb:hb + 64, j, 0, :], hst[hb:hb + 64, 2 * j, :], start=True, stop=False)
                k.mm(bh[:, 64 + h * 64:128 + h * 64], rz[hb:hb + 64, j, 1, :], hst[hb:hb + 64, 2 * j + 1, :], start=False, stop=True)
            k.tt(y[:], bh[:, 64:192].rearrange("p (h v) -> p h v", h=2), y0s[:, j, :, :], ALU.add)
            if lim_stage < 8:
                continue
            k.reduce(s_[:, 0:2], y[:], ALU.add)
            k.tt(ysq[:], y[:], y[:], ALU.mult, eng="pool")
            k.reduce(s_[:, 2:4], ysq[:], ALU.add)
            k.ts(s_[:, 0:2], s_[:, 0:2], 1.0 / 64, None, ALU.mult)
            k.tt(s_[:, 4:6], s_[:, 0:2], s_[:, 0:2], ALU.mult)
            k.stt(s_[:, 2:4], s_[:, 2:4], 1.0 / 64, s_[:, 4:6], ALU.mult, ALU.subtract)
            k.act(s_[:, 2:4], s_[:, 2:4], AF.Sqrt, bias=GN_EPS)
            k.recip(s_[:, 2:4], s_[:, 2:4])
            if lim_stage < 9:
                continue
            for h in range(2):
                k.ts(yn_[:, h, :], y[:, h, :], s_[:, h:h + 1], s_[:, 2 + h:3 + h], ALU.subtract, ALU.mult)
            ynf = yn_[:].rearrange("p h v -> p (h v)")
            k.tt(ynf, ynf, gnw[:, hp * 128:(hp + 1) * 128], ALU.mult, eng="pool")
            k.tt(ynf, ynf, gnb[:, hp * 128:(hp + 1) * 128], ALU.add, eng="pool")
            for h in range(2):
                k.stt(yn_[:, h, :], vsb[:, j, hp * 128 + h * 64:hp * 128 + (h + 1) * 64], bs[:, j, h:h + 1], yn_[:, h, :], ALU.mult, ALU.add)
            if lim_stage < 10:
                continue
            k.mm(bh[:, 256:384], sgl[:, jsl], g2b[:, hp * 128:(hp + 1) * 128])
            k.tt(o_b[:], ynf, bh[:, 256:384], ALU.mult)
            k.dma(cx.O_mix[t0:t0 + 128, 512 + hp * 128:512 + (hp + 1) * 128], o_b[:], q="pool")


    def run_rr(gens):
        gens = list(gens)
        while gens:
            for g_ in list(gens):
                try:
                    next(g_)
                except StopIteration:
                    gens.remove(g_)

    def chain_gen(*gs_):
        for g_ in gs_:
            yield from g_

    n_tb = min(T // 512, lim_tb)
    n_hp = min(4, lim_hp)
    units = [(tb, hp) for tb in range(n_tb) for hp in range(n_hp)]
    run_rr([chain_gen(shared_gen(0), pre_gen(0, 0))])
    for ui_, (tb, hp) in enumerate(units):
        gens = [main_gen(tb, hp)] if lim_stage >= 2 else []
        if ui_ + 1 < len(units):
            ntb, nhp = units[ui_ + 1]
            if ntb != tb:
                gens.append(chain_gen(shared_gen(ntb), pre_gen(ntb, nhp)))
            else:
                gens.append(pre_gen(ntb, nhp))
        run_rr(gens)
    k.pop()


NSA_SLOPES = [2.0 ** (-8.0 * (i + 1) / 16) for i in range(16)]


def phase_nsa(k, cx):
    inp = cx.inp
    k.push()

    def cload(name, shape, dtype=F32, parts=None):
        t = k.sb("nc_" + name, shape, F32)
        if parts is None:
            k.dma(t[:], inp[name][:])
        else:
            k.dma(t[parts[0]:parts[1]], inp[name][:])
        if dtype == F32:
            return t
        tb_ = k.sb("ncb_" + name, shape, dtype)
        if parts is None:
            k.copy(tb_[:], t[:])
        else:
            k.copy(tb_[parts[0]:parts[1]], t[parts[0]:parts[1]])
        return tb_

    ident = cx.ident
    diagD = cload("c_diagD", [128, 4, 512])
    winD = cload("c_winD", [128, 4, 512])
    kbias = cload("c_kbias", [128, 16 * 28])
    ov = cload("c_ov", [128, 2, 65], BF16)
    selg = cload("c_selg", [48, 48, 64], BF16)
    w2k = cload("nsa_w2k", [64, 64], BF16)
    w2v = cload("nsa_w2v", [64, 64], BF16)
    peT = cload("nsa_peT", [64, 2, 32], BF16)
    w1 = []
    for i, nm in enumerate(("nsa_w1k", "nsa_w1v")):
        wt = k.sb(f"n_w1_{i}", [64, 32, 64], BF16)
        k.push()
        st = k.sb("n_w1st", [64, 32, 64], F32)
        k.dma(st[:], inp[nm][:].rearrange("(l d) h -> d l h", d=64))
        k.copy(wt[:], st[:])
        k.pop()
        w1.append(wt)
    KA_sel = k.sb("n_KAs", [128, 32, 128], BF16)
    KA_win = k.sb("n_KAw", [128, 32, 128], BF16)
    k.push()
    st = k.sb("n_kaugst", [128, 32, 128], F32)
    k.dma(st[64:128], inp["c_kaug_sel"][:])
    k.copy(KA_sel[64:128], st[64:128])
    k.dma(st[64:128], inp["c_kaug_win"][:])
    k.copy(KA_win[64:128], st[64:128])
    k.pop()
    QA = [[k.sb(f"n_QA{i}{r}", [128, 512], BF16) for r in range(4)] for i in range(2)]
    VA_sel = k.sb("n_VAs", [128, 32, 128], BF16)
    VA_win = k.sb("n_VAw", [128, 32, 128], BF16)
    k.memset(VA_sel[:, :, 64:128], 1.0)
    k.memset(VA_win[:, :, 64:128], 1.0, eng="pool")
    VCA = k.sb("n_VCA", [128, 2, 128], BF16)
    k.memset(VCA[:], 0.0)
    k.memset(VCA[:, :, 64:128], 1.0)
    KC = k.sb("n_KC", [64, 256], BF16)
    XC = [k.sb(f"n_XC{i}", [64, T], BF16) for i in range(2)]
    h1 = [k.sb(f"n_h1{i}", [64, 256], BF16) for i in range(2)]
    cpe = k.sb("n_cpe", [64, 2], F32)
    Ec = k.sb("n_Ec", [128, 4, 2, 512], BF16)
    cD = [k.sb(f"n_cD{i}", [128, 512], F32) for i in range(4)]
    sc = [k.sb(f"n_sc{i}", [128, 512], F32) for i in range(3)]
    Pb = [k.sb(f"n_P{i}", [128, 512], BF16) for i in range(6)]
    gts = k.sb("n_gts", [48, 512], BF16)
    gtr = k.sb("n_gtr", [48, 512], F32)
    rden = [k.sb(f"n_rden{i}", [64, 512], F32) for i in range(2)]
    ff_ = [k.sb(f"n_f{i}", [64, 512], F32) for i in range(2)]
    acc = [k.sb(f"n_acc{i}", [64, 512], F32) for i in range(4)]
    tmpa = [k.sb(f"n_tmpa{i}", [64, 512], F32) for i in range(2)]
    ob = [k.sb(f"n_ob{i}", [64, 512], BF16) for i in range(2)]
    sval = [k.sb(f"n_sval{i}", [128, 64], F32) for i in range(2)]
    sadd = [k.sb(f"n_sadd{i}", [128, 64], F32) for i in range(2)]
    imp = [k.sb(f"n_imp{i}", [128, 64], F32) for i in range(2)]
    rec = [k.sb(f"n_rec{i}", [128, 1], F32) for i in range(4)]
    top8 = [k.sb(f"n_top8{i}", [128, 8], F32) for i in range(2)]
    msk = [k.sb(f"n_msk{i}", [128, 64], F32) for i in range(2)]
    MTk = [k.sb(f"n_MTk{i}", [128, 128], BF16) for i in range(2)]
    for t_ in MTk:
        k.memset(t_[:], 0.0)
    S = [k.ps(f"n_S{i}", [128, 512], F32) for i in range(4)]
    O = [k.ps(f"n_O{i}", [128, 512], F32) for i in range(2)]
    PG = k.ps("n_PG", [128, 512], F32)
    PI = PG
    PT = k.ps("n_PT", [128, 1024], BF16)

    qaug_st = k.sb("n_qaugst", [128, 512], F32)

    for i in range(2):
        for l in range(32):
            k.mm(PG[0:64, i:i + 1], w1[i][:, l, :], peT[:, i, l:l + 1], start=(l == 0), stop=(l == 31))
    k.copy(cpe[:], PG[0:64, 0:2])

    si = 0
    pi_ = 0
    oi = 0
    fi = 0
    for g in range(4):
        k.dma(KA_sel[0:64, :, :], cx.N_ks[g * 64:(g + 1) * 64, :].rearrange("d (kt s) -> d kt s", s=128))
        k.dma(KA_win[0:64, :, :], cx.N_kw[g * 64:(g + 1) * 64, :].rearrange("d (kt s) -> d kt s", s=128))
        k.dma(VA_sel[:, :, 0:64], cx.N_vs[:, g * 64:(g + 1) * 64].rearrange("(kt p) c -> p kt c", p=128))
        k.dma(VA_win[:, :, 0:64], cx.N_vw[:, g * 64:(g + 1) * 64].rearrange("(kt p) c -> p kt c", p=128))
        for r in range(4):
            h = g * 4 + r
            k.dma(qaug_st[64:128, :], inp["c_qaug"][h])
            for i in range(2):
                k.copy(QA[i][r][64:128, :], qaug_st[64:128, :])
        for i in range(2):
            k.dma(XC[i][:], cx.N_c[i * 256 + g * 64:i * 256 + (g + 1) * 64, :])
            for l in range(32):
                k.mm(PG[0:64, 0:255], w1[i][:, l, :], XC[i][:, l:l + 4065:16], start=(l == 0), stop=(l == 31))
            k.act(h1[i][:, 0:255], PG[0:64, 0:255], AF.Gelu, bias=cpe[:, i:i + 1])
        k.mm(PG[0:64, 256:511], w2k[:], h1[0][:, 0:255])
        k.copy(KC[:, 0:255], PG[0:64, 256:511])
        for nt, nn in ((0, 128), (1, 127)):
            k.mm(PI[0:nn, 0:64], h1[1][:, nt * 128:nt * 128 + nn], w2v[:])
            k.copy(VCA[0:nn, nt, 0:64], PI[0:nn, 0:64])
        for qb in range(T // 512):
            qsl = slice(qb * 512, (qb + 1) * 512)
            qa = QA[qb % 2]
            for r in range(4):
                h = g * 4 + r
                k.dma(qa[r][0:64, :], cx.N_q[h * 64:(h + 1) * 64, qsl])
            k.dma(gtr[:], cx.N_gt[:, qsl])
            k.act(gts[:], gtr[:], AF.Sigmoid)
            nts = ((0, 128),) if qb < 4 else ((0, 128), (1, 127))
            cds = {}
            for nt, nn in nts:
                cd = cD[(qb * 2 + nt) % 4]
                k.dma(cd[:], inp["c_cmpD"][(qb if nt == 0 else 8 + qb - 4)])
                cds[nt] = cd

            def finalize(o_ps, h, br, first):
                nonlocal fi
                rd, f_, tm_ = rden[fi % 2], ff_[fi % 2], tmpa[fi % 2]
                fi += 1
                if br == 0:
                    k.ts(rd[:], o_ps[64:128, :], 1e-30, None, ALU.max)
                    k.act(rd[:], rd[:], AF.Ln)
                else:
                    k.act(rd[:], o_ps[64:128, :], AF.Ln)
                k.act(rd[:], rd[:], AF.Exp, scale=-1.0)
                k.mm(PG[0:64, :], selg[:, h * 3 + br, :], gts[:])
                k.tt(f_[:], PG[0:64, :], rd[:], ALU.mult)
                a = acc[h % 4]
                if first:
                    k.tt(a[:], o_ps[0:64, :], f_[:], ALU.mult)
                else:
                    k.tt(tm_[:], o_ps[0:64, :], f_[:], ALU.mult)
                    k.tt(a[:], a[:], tm_[:], ALU.add, eng="pool")

            for r in range(4):
                h = g * 4 + r
                slope = NSA_SLOPES[h]
                o_ps = O[oi % 2]
                oi += 1
                for idx, (nt, nn) in enumerate(nts):
                    s_ps = S[si % 3]
                    s_sb = sc[si % 3]
                    si += 1
                    k.mm(s_ps[0:nn, :], KC[:, nt * 128:nt * 128 + nn], qa[r][0:64, :])
                    k.stt(s_sb[0:nn, :], cds[nt][0:nn, :], slope, s_ps[0:nn, :], ALU.mult, ALU.add)
                    k.act(Ec[0:nn, r, nt, :], s_sb[0:nn, :], AF.Exp)
                for idx, (nt, nn) in enumerate(nts):
                    k.mm(o_ps[:, :], VCA[0:nn, nt, :], Ec[0:nn, r, nt, :], start=(idx == 0), stop=(idx == len(nts) - 1))
                finalize(o_ps, h, 0, True)
            for qt in range(4):
                t0 = qb * 512 + qt * 128
                sv, sa = sval[qt % 2], sadd[qt % 2]
                im, t8, mk, mtk = imp[qt % 2], top8[qt % 2], msk[qt % 2], MTk[qt % 2]
                k.dma(sv[:], inp["c_selvalid"][t0:t0 + 128, :])
                k.dma(sa[:], inp["c_seladd"][t0:t0 + 128, :])
                for r in range(4):
                    rc = rec[r]
                    for idx, (nt, nn) in enumerate(nts):
                        k.mm(PI[:, r * 65:(r + 1) * 65], Ec[0:nn, r, nt, qt * 128:(qt + 1) * 128], ov[0:nn, nt, :], start=(idx == 0), stop=(idx == len(nts) - 1))
                    k.ts(rc[:], PI[:, r * 65 + 64:r * 65 + 65], 1e-30, None, ALU.max)
                    k.recip(rc[:], rc[:])
                    if r == 0:
                        k.ts(im[:], PI[:, 0:64], rc[:, 0:1], None, ALU.mult)
                    else:
                        k.stt(im[:], PI[:, r * 65:r * 65 + 64], rc[:, 0:1], im[:], ALU.mult, ALU.add)
                k.tt(im[:], im[:], sv[:], ALU.mult)
                k.tt(im[:], im[:], sa[:], ALU.add)
                i_, o_ = im.t[:], t8.t[:]
                k.generic("dve", lambda e, i_=i_, o_=o_: e.max(o_, i_), [im], [t8])
                k.ts(mk[:], im[:], t8[:, 7:8], None, ALU.is_ge)
                k.ts(mtk[:, 64:126], mk[:, 1:63], 30000.0, -30000.0, ALU.mult, ALU.add)
                k.tr(PT[:, 0:128], mtk[:], ident[:])
                for r in range(4):
                    k.copy(qa[r][64:126, qt * 128:(qt + 1) * 128], PT[64:126, 0:128], eng=("act" if r % 2 else "dve"))
            def stream(br, r, o_ps):
                nonlocal si, pi_
                KA, VA = (KA_sel, VA_sel) if br == 1 else (KA_win, VA_win)
                h = g * 4 + r
                slope = NSA_SLOPES[h]
                tiles = []
                if br == 1:
                    tiles += [("fast", kt, kt - 4 * qb) for kt in range(4 * qb)]
                elif qb > 0:
                    tiles += [("far", 4 * qb - 4 + j, j) for j in range(4)]
                tiles += [("diag", 4 * qb + j, j) for j in range(4)]
                pend = None
                for idx, (kind, kt, j) in enumerate(tiles):
                    s_ps = S[si % 4]
                    s_sb = sc[si % 3]
                    si += 1
                    p_ = Pb[pi_ % 6]
                    pi_ += 1
                    if kind == "diag":
                        c0, c1 = 128 * j, 512
                    elif kind == "far":
                        c0, c1 = 0, 128 * (j + 1)
                    else:
                        c0, c1 = 0, 512
                    k.mm(s_ps[:, c0:c1], KA[:, kt, :], qa[r][:, c0:c1])
                    if kind == "fast":
                        col = h * 28 + (j + 28)
                        k.act(p_[:], s_ps[:], AF.Exp, bias=kbias[:, col:col + 1])
                    else:
                        dt_ = diagD if kind == "diag" else winD
                        k.stt(s_sb[:, c0:c1], dt_[:, j, c0:c1], slope, s_ps[:, c0:c1], ALU.mult, ALU.add)
                        k.act(p_[:, c0:c1], s_sb[:, c0:c1], AF.Exp)
                    yield
                    if pend is not None:
                        pp, pkt, pidx, pc0, pc1 = pend
                        k.mm(o_ps[:, pc0:pc1], VA[:, pkt, :], pp[:, pc0:pc1], start=(pidx == 0), stop=False)
                    pend = (p_, kt, idx, c0, c1)
                pp, pkt, pidx, pc0, pc1 = pend
                k.mm(o_ps[:, pc0:pc1], VA[:, pkt, :], pp[:, pc0:pc1], start=(pidx == 0), stop=True)
                yield
                finalize(o_ps, h, br, False)

            for r in range(4):
                h = g * 4 + r
                gens = [stream(1, r, O[0]), stream(2, r, O[1])]
                while gens:
                    for g_ in list(gens):
                        try:
                            next(g_)
                        except StopIteration:
                            gens.remove(g_)
                o_b = ob[h % 2]
                k.copy(o_b[:], acc[h % 4][:], eng="act")
                k.dma(cx.O_fm[h * 64:(h + 1) * 64, qsl], o_b[:], q="pool")
    k.pop()


INPUT_SHAPES = {
    "x": [T, D], "c_col": [128, NKC], "ada_w": [2, D, 6 * D], "ada_b": [2, 6 * D],
    "norm1_g": [2, D], "norm2_g": [2, D], "final_norm_g_bc": [128, D],
    "ffn_w_up": [2, D, 2 * FF], "ffn_conv_col": [2, 128, FF // 128, 4], "ffn_w_down": [2, FF, D],
    "ev_w_in": [1, D, EV_COLS], "ev_w_out": [1, D, D], "od_w_in": [1, D, OD_COLS], "od_w_out": [1, D, D],
    "gla_a_up": [16, 256], "gla_a_b_col": [128, 2], "gla_norm_g_bc": [128, 128],
    "c_ident": [128, 128], "c_scan128": [128, 512], "c_tri_incl": [128, 128],
    "rw_mu_col": [128, 10], "rw_mu_k_col": [128, 4], "rw_mu_al_col": [64, 1], "rw_muv_bc": [128, 512],
    "rw_w0_col": [128, 4], "rw_a0_col": [128, 4], "rw_k_k_col": [128, 4], "rw_k_a_col": [128, 4],
    "rw_w2": [64, 512], "rw_a2": [64, 512], "rw_g2": [128, 512], "rw_rkcol": [128, 4, 2],
    "rw_gn_w_bc": [128, 512], "rw_gn_b_bc": [128, 512],
    "c_blockones": [128, 128], "c_negblock": [128, 128], "c_scan64": [128, 512],
    "c_mk1": [128, 256], "c_mk2": [128, 256], "c_nmt": [128, 128],
    "c_diagD": [128, 4, 512], "c_winD": [128, 4, 512], "c_kbias": [128, 16 * 28], "c_ov": [128, 2, 65],
    "c_selg": [48, 48, 64], "nsa_w2k": [64, 64], "nsa_w2v": [64, 64], "nsa_peT": [64, 2, 32],
    "nsa_w1k": [2048, 64], "nsa_w1v": [2048, 64], "c_kaug_sel": [64, 32, 128], "c_kaug_win": [64, 32, 128],
    "c_qaug": [16, 64, 512], "c_cmpD": [12, 128, 512], "c_selvalid": [T, 64], "c_seladd": [T, 64],
}


def build(stop=None, dbg=(), rw_limit=None, skip=()):
    nc = bass.Bass("TRN2", target_bir_lowering=False)
    k = K(nc)
    cx = Ctx()
    if rw_limit is not None:
        cx.rw_limit = rw_limit
    cx.inp = {}
    for name, shape in INPUT_SHAPES.items():
        cx.inp[name] = k.dram(name, shape, F32, kind="ExternalInput")

    def scratch(name, shape, dtype=F32):
        return k.dram(name, shape, dtype, kind=("ExternalOutput" if name in dbg else "Internal"))

    out = k.dram("out", [T, D], F32, kind="ExternalOutput")
    cx.G_qk = scratch("G_qk", [512, T])
    cx.G_lr = scratch("G_lr", [16, T])
    cx.G_v = scratch("G_v", [T, 512], BF16)
    cx.G_og = scratch("G_og", [T, 512])
    cx.R_fm = scratch("R_fm", [1280, T])
    cx.R_v = scratch("R_v", [T, 512])
    cx.O_mix = scratch("O_mix", [T, D], BF16)
    cx.O_fm = scratch("O_fm", [D, T], BF16)
    cx.H_fm = scratch("H_fm", [FF, T], BF16)
    cx.XA = scratch("XA", [T, D])
    cx.XB = scratch("XB", [T, D])
    cx.XC = scratch("XC", [T, D])
    cx.N_q = scratch("N_q", [D, T], BF16)
    cx.N_c = scratch("N_c", [512, T], BF16)
    cx.N_ks = scratch("N_ks", [256, T], BF16)
    cx.N_kw = scratch("N_kw", [256, T], BF16)
    cx.N_gt = scratch("N_gt", [48, T])
    cx.N_vs = scratch("N_vs", [T, 256], BF16)
    cx.N_vw = scratch("N_vw", [T, 256], BF16)
    x = cx.inp["x"]

    def done(stage):
        return stop is not None and stage == stop

    phase_consts(k, cx)
    k.push()
    bc = phase_mod(k, cx, 0)
    if not done("mod0") and "l0" not in skip:
        fm = [(0, 512, cx.G_qk, 0, F32, 1.0), (1536, 16, cx.G_lr, 0, F32, 1.0),
              (1552, 1088, cx.R_fm, 0, F32, 1.0), (1552 + 1600, 192, cx.R_fm, 1088, F32, 1.0)]
        tm = [(512, 512, cx.G_v, 0, BF16), (1024, 512, cx.G_og, 0, F32), (1552 + 1088, 512, cx.R_v, 0, F32)]
        phase_proj(k, cx, x, cx.inp["ev_w_in"][0], EV_COLS, bc["gsc1"], bc["sh1"], fm, tm, "pj0")
    stages = ["mod0", "proj0", "gla", "rwkv", "wout0", "ffn0u", "ffn0d", "proj1", "nsa", "wout1", "ffn1u", "ffn1d"]
    def upto(stage):
        return stop is None or stop not in stages or stages.index(stop) >= stages.index(stage)
    if "l0" in skip:
        stages_l0_off = True
    if upto("gla") and "gla" not in skip and "l0" not in skip:
        phase_gla(k, cx)
    if upto("rwkv") and "l0" not in skip:
        phase_rwkv(k, cx)
    if upto("wout0") and "l0" not in skip:
        phase_wout(k, cx, x, cx.XA, cx.inp["ev_w_out"][0], bc["g1"], cx.O_mix, "tm", "wo0")
    if upto("ffn0u") and "l0" not in skip:
        phase_ffn_up(k, cx, 0, cx.XA, bc)
    if upto("ffn0d") and "l0" not in skip:
        phase_ffn_down(k, cx, 0, cx.XA, cx.XB, bc)
    k.pop()
    if upto("proj1"):
        xin1 = cx.XB if "l0" not in skip else x
        k.push()
        bc = phase_mod(k, cx, 1)
        fm = [(0, 1024, cx.N_q, 0, BF16, 0.125), (1024, 512, cx.N_c, 0, BF16, 1.0), (1536, 256, cx.N_ks, 0, BF16, 1.0),
              (2048, 256, cx.N_kw, 0, BF16, 1.0), (2560, 48, cx.N_gt, 0, F32, 1.0)]
        tm = [(1792, 256, cx.N_vs, 0, BF16), (2304, 256, cx.N_vw, 0, BF16)]
        phase_proj(k, cx, xin1, cx.inp["od_w_in"][0], OD_COLS, bc["gsc1"], bc["sh1"], fm, tm, "pj1")
        if upto("nsa"):
            phase_nsa(k, cx)
        if upto("wout1"):
            phase_wout(k, cx, xin1, cx.XC, cx.inp["od_w_out"][0], bc["g1"], cx.O_fm, "fm", "wo1")
        if upto("ffn1u"):
            phase_ffn_up(k, cx, 1, cx.XC, bc)
        if upto("ffn1d"):
            phase_ffn_down(k, cx, 1, cx.XC, out, bc, final=True)
        k.pop()
    build.stats = {e: len(k.ops[e]) for e in ENGS}
    k.emit()
    return nc


def host_inputs(inputs, b):
    f = np.float32
    m = {}
    m["x"] = np.ascontiguousarray(inputs["x"][b], dtype=f)
    m["c_col"] = np.ascontiguousarray(inputs["c"][b].reshape(NKC, 128).T, dtype=f)
    for nm in ("ada_w", "ada_b", "norm1_g", "norm2_g", "ffn_w_up", "ffn_w_down", "ev_w_in", "ev_w_out", "od_w_in", "od_w_out"):
        m[nm] = np.ascontiguousarray(inputs[nm], dtype=f)
    m["final_norm_g_bc"] = np.ascontiguousarray(np.broadcast_to(inputs["final_norm_g"][None, :], (128, D)), dtype=f)
    cw = np.concatenate([inputs["ffn_conv_w"], inputs["ffn_conv_b"][:, None, :]], axis=1)
    m["ffn_conv_col"] = np.ascontiguousarray(cw.reshape(2, 4, FF // 128, 128).transpose(0, 3, 2, 1), dtype=f)
    m["gla_a_up"] = np.ascontiguousarray(inputs["gla_a_up"][0], dtype=f)
    m["gla_a_b_col"] = np.ascontiguousarray(inputs["gla_a_b"][0].reshape(2, 128).T, dtype=f)
    m["gla_norm_g_bc"] = np.ascontiguousarray(np.broadcast_to(inputs["gla_norm_g"][0][None, :], (128, 128)), dtype=f)
    m["c_ident"] = np.eye(128, dtype=f)
    sc = np.ones((128, 512), f)
    sc[:, 0::128] = 0.0
    m["c_scan128"] = sc
    i = np.arange(128)
    m["c_tri_incl"] = (i[:, None] <= i[None, :]).astype(f)
    smu = inputs["ev_shift_mu"][0]
    mu_fm = np.concatenate([smu[0:1088], smu[1600:1792]])
    m["rw_mu_col"] = np.ascontiguousarray(mu_fm.reshape(10, 128).T, dtype=f)
    m["rw_mu_k_col"] = np.ascontiguousarray(smu[576:1088].reshape(4, 128).T, dtype=f)
    m["rw_mu_al_col"] = np.ascontiguousarray(smu[1600:1664].reshape(64, 1), dtype=f)
    m["rw_muv_bc"] = np.ascontiguousarray(np.broadcast_to(smu[1088:1600][None, :], (128, 512)), dtype=f)
    for nm in ("w0", "a0", "k_k", "k_a"):
        m[f"rw_{nm}_col"] = np.ascontiguousarray(inputs[f"rw_{nm}"][0].reshape(4, 128).T, dtype=f)
    m["rw_w2"] = np.ascontiguousarray(inputs["rw_w2"][0], dtype=f)
    m["rw_a2"] = np.ascontiguousarray(inputs["rw_a2"][0], dtype=f)
    m["rw_g2"] = np.ascontiguousarray(inputs["rw_g2"][0], dtype=f)
    rk = inputs["rw_r_k"][0]
    rkcol = np.zeros((128, 4, 2), f)
    for hp in range(4):
        rkcol[0:64, hp, 0] = rk[2 * hp]
        rkcol[64:128, hp, 1] = rk[2 * hp + 1]
    m["rw_rkcol"] = rkcol
    m["rw_gn_w_bc"] = np.ascontiguousarray(np.broadcast_to(inputs["rw_gn_w"][0][None, :], (128, 512)), dtype=f)
    m["rw_gn_b_bc"] = np.ascontiguousarray(np.broadcast_to(inputs["rw_gn_b"][0][None, :], (128, 512)), dtype=f)
    same = (i[:, None] // 64) == (i[None, :] // 64)
    m["c_blockones"] = same.astype(f)
    m["c_negblock"] = -same.astype(f)
    s64 = np.ones((128, 512), f)
    s64[:, 0::64] = 0.0
    m["c_scan64"] = s64
    mstrict = (same & (i[:, None] < i[None, :])).astype(f)
    mincl = (same & (i[:, None] <= i[None, :])).astype(f)
    m["c_mk1"] = np.concatenate([mstrict, mincl], axis=1)
    m["c_mk2"] = np.concatenate([-mstrict, mincl], axis=1)
    m["c_nmt"] = np.ascontiguousarray(-mstrict.T)
    s_ = np.arange(128)[:, None].astype(np.float64)
    q_ = np.arange(512)[None, :].astype(np.float64)
    NEG = -1.0e6
    dd = np.zeros((128, 4, 512), f)
    wd = np.zeros((128, 4, 512), f)
    for j in range(4):
        dd[:, j, :] = np.where(128 * j + s_ <= q_, 128 * j + s_ - 511.0, NEG)
        wd[:, j, :] = np.where(128 * j + s_ > q_, 128 * j - 512.0 + s_ - 511.0, NEG)
    m["c_diagD"] = dd
    m["c_winD"] = wd
    slopes = np.array(NSA_SLOPES, np.float64)
    kb = np.zeros((128, 16 * 28), f)
    for h in range(16):
        for jj in range(28):
            kb[:, h * 28 + jj] = (slopes[h] * (np.arange(128) + 128.0 * (jj - 28) - 511.0)).astype(f)
    m["c_kbias"] = kb
    n_ = np.arange(256)
    cstart = n_ * 16
    cend = cstart + 31
    sstart = np.arange(64) * 64
    ovl = ((cstart[:, None] <= sstart[None, :] + 63) & (cend[:, None] >= sstart[None, :])).astype(f)
    ovl[255] = 0.0
    ov = np.zeros((128, 2, 65), f)
    ov[:, 0, :64] = ovl[:128]
    ov[:, 1, :64] = ovl[128:]
    ov[:, :, 64] = 1.0
    ov[127, 1, :] = 0.0
    m["c_ov"] = ov
    sg = np.zeros((48, 48, 64), f)
    for i_ in range(48):
        sg[i_, i_, :] = 1.0
    m["c_selg"] = sg
    m["nsa_w2k"] = np.ascontiguousarray(inputs["cmp_w2_k"][0], dtype=f)
    m["nsa_w2v"] = np.ascontiguousarray(inputs["cmp_w2_v"][0], dtype=f)
    m["nsa_peT"] = np.ascontiguousarray(np.stack([inputs["cmp_pe_k"][0].T, inputs["cmp_pe_v"][0].T], axis=1), dtype=f)
    m["nsa_w1k"] = np.ascontiguousarray(inputs["cmp_w1_k"][0], dtype=f)
    m["nsa_w1v"] = np.ascontiguousarray(inputs["cmp_w1_v"][0], dtype=f)
    ka_s = np.zeros((64, 32, 128), f)
    ka_w = np.zeros((64, 32, 128), f)
    for kt in range(32):
        for half in range(2):
            b_ = 2 * kt + half
            if 1 <= b_ <= 62:
                ka_s[b_ - 1, kt, half * 64:(half + 1) * 64] = 1.0
    ka_s[62:64] = 1.0
    ka_w[62:64] = 1.0
    m["c_kaug_sel"] = ka_s
    m["c_kaug_win"] = ka_w
    import ml_dtypes
    qa = np.zeros((16, 64, 512), f)
    for h in range(16):
        rv = slopes[h] * (511.0 - np.arange(512))
        hi = rv.astype(f).astype(ml_dtypes.bfloat16).astype(np.float64)
        lo = (rv - hi).astype(f).astype(ml_dtypes.bfloat16).astype(np.float64)
        qa[h, 62] = hi
        qa[h, 63] = lo
    m["c_qaug"] = qa
    cD = np.zeros((12, 128, 512), f)
    for qb in range(8):
        for nt in range(2):
            if nt == 1 and qb < 4:
                continue
            e_n = 16.0 * (128 * nt + np.arange(128)[:, None]) + 31.0
            t_q = 512.0 * qb + np.arange(512)[None, :]
            cD[qb if nt == 0 else 8 + qb - 4] = np.where(t_q >= e_n, -(t_q - e_n), NEG)
    m["c_cmpD"] = cD
    t_ = np.arange(T)
    ahead = (t_ // 64)[:, None] - np.arange(64)[None, :]
    valid = ahead >= 0
    forced = (np.arange(64)[None, :] == 0) | (valid & (ahead < 2))
    m["c_selvalid"] = valid.astype(f)
    m["c_seladd"] = np.where(valid, np.where(forced, 100.0, 0.0), -100.0).astype(f)
    return m


_CACHE = {}


def kernel(**inputs):
    inputs = {k_: np.asarray(v) for k_, v in inputs.items()}
    if "nc" not in _CACHE:
        _CACHE["nc"] = build()
    nc = _CACHE["nc"]
    B = inputs["x"].shape[0]
    maps = [host_inputs(inputs, b) for b in range(B)]
    zero = dict(maps[0])
    zero["x"] = np.zeros_like(maps[0]["x"])
    slots = [0, 1, 4, 5][:B]
    in_maps = [zero] * 8
    for b, c_ in enumerate(slots):
        in_maps[c_] = maps[b]
    res = run_bass_kernel_spmd(nc, in_maps, core_ids=list(range(8)))
    out = np.stack([np.asarray(res.results[c_]["out"], dtype=np.float32) for c_ in slots], axis=0)
    return out
```

```python
import contextlib
import numpy as np
import concourse.bass as bass
import concourse.mybir as mybir
from concourse.bass_utils import run_bass_kernel_spmd

F32 = mybir.dt.float32
BF16 = mybir.dt.bfloat16
AF = mybir.ActivationFunctionType
ALU = mybir.AluOpType
AX = mybir.AxisListType

ENGS = ("pe", "act", "dve", "pool", "sp")


class Tl:
    _n = 0

    def __init__(self, t, space, key=None):
        self.t = t
        self.space = space
        Tl._n += 1
        self.key = key if key is not None else ("t", Tl._n)

    def __getitem__(self, idx):
        return V(self, self.t[idx])

    def ap(self):
        return V(self, self.t[:])

    def part(self, sub):
        return Tl(self.t, self.space, key=(self.key, sub))


class V:
    def __init__(self, tile, ap):
        self.tile = tile
        self.ap = ap

    def __getitem__(self, idx):
        return V(self.tile, self.ap[idx])

    def rearrange(self, *a, **kw):
        return V(self.tile, self.ap.rearrange(*a, **kw))

    def bitcast(self, dt):
        return V(self.tile, self.ap.bitcast(dt))

    @property
    def shape(self):
        return self.ap.shape


class Op:
    __slots__ = ("eng", "fn", "waits", "inc", "kind")


class K:
    def __init__(self, nc, n_dma_slots=(("sp", 40), ("pool", 16), ("act", 12)), same_engine_raw=True):
        self.nc = nc
        self.stack = contextlib.ExitStack()
        self.ops = {e: [] for e in ENGS}
        self.count = {e: 0 for e in ENGS}
        self.sem = {}
        for e in ENGS:
            self.sem[e] = self.stack.enter_context(nc.semaphore("s_" + e))
        self.slots = {}
        for q, n in n_dma_slots:
            self.slots[q] = [[self.stack.enter_context(nc.semaphore(f"d_{q}{i}")), 0] for i in range(n)]
        self.slot_rr = {q: 0 for q, _ in n_dma_slots}
        self.waited = {e: {} for e in ENGS}
        self.last_w = {}
        self.readers = {}
        self.same_engine_raw = same_engine_raw
        self.n_ops = 0
        self.stacks = [self.stack]
        self.dram_w = {}
        self.pe_rg = {}
        self.uid = 0

    def push(self):
        self.stacks.append(contextlib.ExitStack())

    def pop(self):
        self.barrier()
        self.stacks.pop().close()

    def sb(self, name, shape, dtype=F32):
        self.uid += 1
        t = self.stacks[-1].enter_context(self.nc.sbuf_tensor(f"{name}_{self.uid}", list(shape), dtype))
        return Tl(t, "sb")

    def ps(self, name, shape, dtype=F32):
        self.uid += 1
        t = self.stacks[-1].enter_context(self.nc.psum_tensor(f"{name}_{self.uid}", list(shape), dtype))
        return Tl(t, "ps")

    def dram(self, name, shape, dtype=F32, kind="Internal"):
        t = self.nc.dram_tensor(name, list(shape), dtype, kind=kind).ap()
        return Tl(t, "dram")

    def _need(self, eng, deps, sem, val, src_eng):
        if sem is None:
            return
        if src_eng == eng and eng == "pe":
            return
        cur = deps.get(id(sem))
        if cur is None or cur[1] < val:
            deps[id(sem)] = (sem, val)

    def _record(self, eng, fn, reads, writes, dma_q=None, rg=None):
        deps = {}
        rkeys = []
        wkeys = []
        for v in reads:
            if v is None:
                continue
            tl = v.tile if isinstance(v, V) else v
            rkeys.append((tl.key, tl.space))
        for v in writes:
            tl = v.tile if isinstance(v, V) else v
            wkeys.append((tl.key, tl.space))
        is_dma = dma_q is not None
        for key, space in rkeys:
            if space == "dram":
                for (sem, val, seng, wdma) in self.dram_w.get(key, []):
                    self._need(eng, deps, sem, val, None)
                continue
            lw = self.last_w.get(key)
            if lw is not None:
                sem, val, seng, wdma = lw
                if seng == eng and not wdma and not is_dma and not self.same_engine_raw and eng != "pe":
                    pass
                else:
                    self._need(eng, deps, sem, val, seng if not (wdma or is_dma) else None)
            if space == "ps":
                for (sem, val, seng, rdma) in self.readers.get(key, []):
                    if seng != eng:
                        self._need(eng, deps, sem, val, seng)
        for key, space in wkeys:
            if rg is not None and space == "ps":
                prev = self.pe_rg.get(key)
                if prev is not None and prev[0] != rg:
                    self._need(eng, deps, prev[1][0], prev[1][1], None)
            lw = self.last_w.get(key) if space != "dram" else None
            if lw is not None:
                sem, val, seng, wdma = lw
                if seng != eng or wdma or is_dma:
                    self._need(eng, deps, sem, val, None if (wdma or is_dma) else seng)
            for (sem, val, seng, rdma) in self.readers.get(key, []):
                if seng != eng or rdma or is_dma:
                    self._need(eng, deps, sem, val, None if (rdma or is_dma) else seng)
        if is_dma:
            sl = self.slots[dma_q]
            i = self.slot_rr[dma_q]
            self.slot_rr[dma_q] = (i + 1) % len(sl)
            sem, uses = sl[i]
            if uses > 0:
                self._need(eng, deps, sem, 16 * uses, None)
            sl[i][1] = uses + 1
            tok = (sem, 16 * (uses + 1), eng, True)
            inc = (sem, 16)
        else:
            self.count[eng] += 1
            tok = (self.sem[eng], self.count[eng], eng, False)
            inc = (self.sem[eng], 1)
        waits = []
        wd = self.waited[eng]
        for sid, (sem, val) in deps.items():
            if wd.get(sid, 0) >= val:
                continue
            wd[sid] = val
            waits.append((sem, val))
        op = Op()
        op.eng = eng
        op.fn = fn
        op.waits = waits
        op.inc = inc
        self.ops[eng].append(op)
        self.n_ops += 1
        for key, space in rkeys:
            lst = self.readers.setdefault(key, [])
            for n_, t_ in enumerate(lst):
                if t_[0] is tok[0]:
                    lst[n_] = tok
                    break
            else:
                lst.append(tok)
        for key, space in wkeys:
            if space == "dram":
                self.dram_w.setdefault(key, []).append(tok)
                continue
            if rg is not None and space == "ps":
                self.pe_rg[key] = (rg, tok)
            self.last_w[key] = tok
            self.readers[key] = []
        return tok

    def barrier(self):
        for e in ENGS:
            waits = []
            wd = self.waited[e]
            for o in ENGS:
                if o == e or self.count[o] == 0:
                    continue
                sem = self.sem[o]
                if wd.get(id(sem), 0) < self.count[o]:
                    wd[id(sem)] = self.count[o]
                    waits.append((sem, self.count[o]))
            for q, sl in self.slots.items():
                for sem, uses in sl:
                    if uses > 0 and wd.get(id(sem), 0) < 16 * uses:
                        wd[id(sem)] = 16 * uses
                        waits.append((sem, 16 * uses))
            if waits:
                op = Op()
                op.eng = e
                op.fn = None
                op.waits = waits
                op.inc = None
                self.ops[e].append(op)
        self.last_w = {}
        self.readers = {}
        self.dram_w = {}

    @staticmethod
    def _a(x):
        return x.ap if isinstance(x, V) else x

    def mm(self, out, lhsT, rhs, start=True, stop=True, **kw):
        o, l, r = out.ap, lhsT.ap, rhs.ap
        rg = (int(l.start_partition()), int(l.shape[0]))
        return self._record("pe", lambda e: e.matmul(o, l, r, start=start, stop=stop, **kw), [lhsT, rhs], [out], rg=rg)

    def tr(self, out, in_, ident):
        o, i, d = out.ap, in_.ap, ident.ap
        rg = (int(i.start_partition()), int(i.shape[0]))
        return self._record("pe", lambda e: e.transpose(o, i, d), [in_, ident], [out], rg=rg)

    def act(self, out, in_, func, bias=None, scale=1.0, accum=None, eng="act"):
        o, i = out.ap, in_.ap
        kw = {}
        reads = [in_]
        if bias is not None:
            kw["bias"] = self._a(bias)
            if isinstance(bias, V):
                reads.append(bias)
        if isinstance(scale, V):
            reads.append(scale)
        kw["scale"] = self._a(scale)
        writes = [out]
        if accum is not None:
            kw["accum_out"] = accum.ap
            writes.append(accum)
        return self._record(eng, lambda e: e.activation(o, i, func, **kw), reads, writes)

    def tt(self, out, a, b, op, eng="dve"):
        o, x, y = out.ap, a.ap, b.ap
        return self._record(eng, lambda e: e.tensor_tensor(o, x, y, op), [a, b], [out])

    def ts(self, out, a, s1, s2=None, op0=ALU.mult, op1=None, eng="dve", accum=None):
        o, x = out.ap, a.ap
        reads = [a]
        for s in (s1, s2):
            if isinstance(s, V):
                reads.append(s)
        a1, a2 = self._a(s1), self._a(s2)
        kw = {}
        writes = [out]
        if accum is not None:
            kw["accum_out"] = accum.ap
            writes.append(accum)
        if op1 is None:
            return self._record(eng, lambda e: e.tensor_scalar(o, x, a1, None, op0, **kw), reads, writes)
        return self._record(eng, lambda e: e.tensor_scalar(o, x, a1, a2, op0, op1, **kw), reads, writes)

    def stt(self, out, a, scalar, b, op0, op1, eng="dve"):
        o, x, y = out.ap, a.ap, b.ap
        reads = [a, b]
        if isinstance(scalar, V):
            reads.append(scalar)
        s = self._a(scalar)
        return self._record(eng, lambda e: e.scalar_tensor_tensor(o, x, s, y, op0, op1), reads, [out])

    def copy(self, out, in_, eng="dve"):
        o, i = out.ap, in_.ap
        if eng == "act":
            return self._record(eng, lambda e: e.copy(o, i), [in_], [out])
        return self._record(eng, lambda e: e.tensor_copy(o, i), [in_], [out])

    def memset(self, out, val, eng="dve"):
        o = out.ap
        return self._record(eng, lambda e: e.memset(o, val), [], [out])

    def reduce(self, out, in_, op, axis=AX.X, eng="dve"):
        o, i = out.ap, in_.ap
        return self._record(eng, lambda e: e.tensor_reduce(o, i, axis, op), [in_], [out])

    def recip(self, out, in_):
        o, i = out.ap, in_.ap
        return self._record("dve", lambda e: e.reciprocal(o, i), [in_], [out])

    def generic(self, eng, fn, reads, writes):
        return self._record(eng, fn, reads, writes)

    def dma(self, out, in_, q="sp", **kw):
        o, i = out.ap, in_.ap
        return self._record(q, lambda e: e.dma_start(o, i, **kw), [in_], [out], dma_q=q)

    def check_deadlock(self):
        ptr = {e: 0 for e in ENGS}
        semv = {}
        progress = True
        while progress:
            progress = False
            for e in ENGS:
                lst = self.ops[e]
                while ptr[e] < len(lst):
                    op = lst[ptr[e]]
                    if all(semv.get(id(sem), 0) >= val for sem, val in op.waits):
                        if op.fn is not None:
                            semv[id(op.inc[0])] = semv.get(id(op.inc[0]), 0) + op.inc[1]
                        ptr[e] += 1
                        progress = True
                    else:
                        break
        stuck = {e: (ptr[e], len(self.ops[e])) for e in ENGS if ptr[e] < len(self.ops[e])}
        if stuck:
            raise RuntimeError(f"semaphore deadlock: {stuck}")

    def emit(self):
        nc = self.nc
        self.barrier()
        self.check_deadlock()
        ops = self.ops
        with nc.Block() as block:
            def run(eng_obj, lst):
                for op in lst:
                    for sem, val in op.waits:
                        eng_obj.wait_ge(sem, val)
                    if op.fn is not None:
                        ins = op.fn(eng_obj)
                        ins.then_inc(op.inc[0], op.inc[1])

            @block.tensor
            def _(e):
                run(e, ops["pe"])

            @block.scalar
            def _(e):
                run(e, ops["act"])

            @block.vector
            def _(e):
                run(e, ops["dve"])

            @block.gpsimd
            def _(e):
                run(e, ops["pool"])

            @block.sync
            def _(e):
                run(e, ops["sp"])
        self.stack.close()

T = 4096
D = 1024
NT = T // 128
EPS = 1e-6
EV_COLS = 3344
OD_COLS = 2608
FF = 2816
NKC = D // 128


class Ctx:
    pass


def _rows(ap2d):
    return ap2d.rearrange("(kc p) n -> p kc n", p=128)


def phase_consts(k, cx):
    cx.ident = k.sb("ident", [128, 128], BF16)
    cx.identf = k.sb("identf", [128, 128], F32)
    k.dma(cx.identf[:], cx.inp["c_ident"][:])
    k.copy(cx.ident[:], cx.identf[:])
    cx.ones_row = k.sb("ones_row", [1, 128], F32)
    k.memset(cx.ones_row[:], 1.0)


def phase_mod(k, cx, layer):
    inp = cx.inp
    bc = {}
    for nm in ("gsc1", "sh1", "g1", "gsc2", "sh2", "g2"):
        bc[nm] = k.sb(f"bc_{nm}", [128, D], F32)
    k.push()
    ccol = k.sb("ccol", [128, NKC], F32)
    k.dma(ccol[:], inp["c_col"][:])
    cond = k.sb("cond", [128, NKC], F32)
    k.act(cond[:], ccol[:], AF.Silu)
    modrow = k.sb("modrow", [1, 6 * D], F32)
    brow = k.sb("brow", [1, 6 * D], F32)
    k.dma(brow[:], inp["ada_b"][layer:layer + 1, :])
    g12 = k.sb("g12", [1, 2 * D], F32)
    k.dma(g12[:, 0:D], inp["norm1_g"][layer:layer + 1, :])
    k.dma(g12[:, D:2 * D], inp["norm2_g"][layer:layer + 1, :])
    wst = [k.sb(f"adast{i}", [128, NKC, 512], F32) for i in range(2)]
    pm = [k.ps(f"pmod{i}", [128, 512], F32) for i in range(2)]
    aw = _rows(inp["ada_w"][layer])
    for blk in range(12):
        w = wst[blk % 2]
        k.dma(w[:], aw[:, :, blk * 512:(blk + 1) * 512])
        p = pm[blk % 2]
        for kc in range(NKC):
            k.mm(p[0:1, :], cond[:, kc:kc + 1], w[:, kc, :], start=(kc == 0), stop=(kc == NKC - 1))
        k.tt(modrow[:, blk * 512:(blk + 1) * 512], p[0:1, :], brow[:, blk * 512:(blk + 1) * 512], ALU.add)
    for which, (sc_off, goff) in enumerate(((1, 0), (4, 1))):
        k.ts(modrow[:, sc_off * D:(sc_off + 1) * D], modrow[:, sc_off * D:(sc_off + 1) * D], 1.0, None, ALU.add)
        k.tt(modrow[:, sc_off * D:(sc_off + 1) * D], modrow[:, sc_off * D:(sc_off + 1) * D], g12[:, goff * D:(goff + 1) * D], ALU.mult)
    order = ("sh1", "gsc1", "g1", "sh2", "gsc2", "g2")
    i = 0
    for j, nm in enumerate(order):
        for half in range(2):
            p = pm[i % 2]
            i += 1
            k.mm(p[:], cx.ones_row[:], modrow[:, j * D + half * 512: j * D + (half + 1) * 512])
            k.copy(bc[nm][:, half * 512:(half + 1) * 512], p[:], eng=("act" if half else "dve"))
    k.pop()
    return bc


def load_weight_bf16(k, w_rows, ncols, name, col0=0):
    nkc = w_rows.shape[1]
    wb = k.sb(name, [128, nkc, ncols], BF16)
    k.push()
    st = [k.sb(f"{name}_st{i}", [128, ncols], F32) for i in range(2)]
    engs = ("dve", "pool", "act")
    for kc in range(nkc):
        s = st[kc % 2]
        k.dma(s[:], w_rows[:, kc, col0:col0 + ncols])
        k.copy(wb[:, kc, :], s[:], eng=engs[kc % 3])
    k.pop()
    return wb


class NormFront:
    def __init__(self, k, cx, gsc, sh):
        self.k, self.cx, self.gsc, self.sh = k, cx, gsc, sh
        self.xt = [k.sb(f"nf_x{i}", [128, D], F32) for i in range(2)]
        self.xm = [k.sb(f"nf_xm{i}", [128, D], F32) for i in range(2)]
        self.xb = [k.sb(f"nf_xb{i}", [128, D], BF16) for i in range(2)]
        self.junk = k.sb("nf_junk", [128, D], BF16)
        self.ss = [k.sb(f"nf_ss{i}", [128, 2], F32) for i in range(2)]
        self.pT = [k.ps(f"nf_pT{i}", [128, D], BF16) for i in range(2)]
        self.i = 0

    def tile(self, x_src, dst):
        k, cx = self.k, self.cx
        i = self.i % 2
        self.i += 1
        xt, xm, xb, ss, pT = self.xt[i], self.xm[i], self.xb[i], self.ss[i], self.pT[i]
        k.dma(xt[:], x_src)
        k.memset(ss[:], 0.0, eng="pool")
        k.act(self.junk[:], xt[:], AF.Square, accum=ss[:, 0:1])
        k.act(ss[:, 1:2], ss[:, 0:1], AF.Sqrt, bias=EPS, scale=1.0 / D)
        k.recip(ss[:, 1:2], ss[:, 1:2])
        k.stt(xm[:], xt[:], ss[:, 1:2], self.gsc[:], ALU.mult, ALU.mult)
        k.tt(xb[:], xm[:], self.sh[:], ALU.add, eng="pool")
        for kc in range(NKC):
            k.tr(pT[:, kc * 128:(kc + 1) * 128], xb[:, kc * 128:(kc + 1) * 128], cx.ident[:])
        k.copy(dst[:, 0:4, :], pT[:, 0:512].rearrange("p (k t) -> p k t", k=4), eng="act")
        k.copy(dst[:, 4:8, :], pT[:, 512:1024].rearrange("p (k t) -> p k t", k=4), eng="dve")
        return xt


class Evac:
    def __init__(self, k, name, shape, dtype, n=3):
        self.k = k
        self.bufs = [k.sb(f"{name}{i}", shape, dtype) for i in range(n)]
        self.i = 0

    def next(self):
        b = self.bufs[self.i % len(self.bufs)]
        self.i += 1
        return b


def phase_proj(k, cx, x_in, w_dram, ncols, bc_gsc, bc_sh, fm_specs, tm_specs, name):
    k.push()
    wb = load_weight_bf16(k, _rows(w_dram), ncols, name + "_wb")
    nf = NormFront(k, cx, bc_gsc, bc_sh)
    xnT = [k.sb(f"{name}_xnT{i}", [128, NKC, 512], BF16) for i in range(2)]
    pmm = [k.ps(f"{name}_pm{i}", [128, 512], F32) for i in range(6)]
    ev32 = Evac(k, name + "_ev32", [128, 512], F32, 3)
    ev16 = Evac(k, name + "_ev16", [128, 512], BF16, 3)
    pi = 0
    ei = 0
    engs = ("act", "dve")
    for tb in range(T // 512):
        xn = xnT[tb % 2]
        for j in range(4):
            t0 = tb * 512 + j * 128
            nf.tile(x_in[t0:t0 + 128, :], xn[:, :, j * 128:(j + 1) * 128])
        for (c0, n, dst, row0, dt, scale) in fm_specs:
            for cc in range(0, n, 128):
                m = min(128, n - cc)
                p = pmm[pi % 6]
                pi += 1
                for kc in range(NKC):
                    k.mm(p[0:m, :], wb[:, kc, c0 + cc:c0 + cc + m], xn[:, kc, :], start=(kc == 0), stop=(kc == NKC - 1))
                e = (ev32 if dt == F32 else ev16).next()
                eng = engs[ei % 2]
                ei += 1
                if eng == "act":
                    k.act(e[0:m, :], p[0:m, :], AF.Copy, scale=scale)
                else:
                    k.ts(e[0:m, :], p[0:m, :], scale, None, ALU.mult)
                k.dma(dst[row0 + cc:row0 + cc + m, tb * 512:(tb + 1) * 512], e[0:m, :], q="pool")
        for (c0, n, dst, col0, dt) in tm_specs:
            for j in range(4):
                t0 = tb * 512 + j * 128
                for cc in range(0, n, 512):
                    m = min(512, n - cc)
                    p = pmm[pi % 6]
                    pi += 1
                    for kc in range(NKC):
                        k.mm(p[:, 0:m], xn[:, kc, j * 128:(j + 1) * 128], wb[:, kc, c0 + cc:c0 + cc + m], start=(kc == 0), stop=(kc == NKC - 1))
                    e = (ev32 if dt == F32 else ev16).next()
                    k.copy(e[:, 0:m], p[:, 0:m], eng=engs[ei % 2])
                    ei += 1
                    k.dma(dst[t0:t0 + 128, col0 + cc:col0 + cc + m], e[:, 0:m], q="pool")
    k.pop()


def phase_gla(k, cx):
    inp = cx.inp
    k.push()
    a_up = k.sb("g_aup", [16, 256], F32)
    k.dma(a_up[:], inp["gla_a_up"][:])
    nab = k.sb("g_nab", [128, 2], F32)
    k.dma(nab[:], inp["gla_a_b_col"][:])
    k.ts(nab[:], nab[:], -1.0, None, ALU.mult)
    gbc = k.sb("g_gbc", [128, 128], F32)
    k.dma(gbc[:], inp["gla_norm_g_bc"][:])
    rmask = k.sb("g_rmask", [128, 512], F32)
    k.dma(rmask[:], inp["c_scan128"][:])
    tri = k.sb("g_tri", [128, 128], F32)
    k.dma(tri[:], inp["c_tri_incl"][:])
    S = [k.sb(f"g_S{i}", [128, 128], F32) for i in range(2)]
    Sbf = [k.sb(f"g_Sbf{i}", [128, 128], BF16) for i in range(2)]
    for i in range(2):
        k.memset(S[i][:], 0.0)
        k.memset(Sbf[i][:], 0.0)
    lrT = [k.sb(f"g_lrT{i}", [16, 512], F32) for i in range(2)]
    qT = [k.sb(f"g_qT{i}", [128, 512], F32) for i in range(2)]
    kT = [k.sb(f"g_kT{i}", [128, 512], F32) for i in range(2)]
    e1 = [k.sb(f"g_e1{i}", [128, 512], F32) for i in range(2)]
    sp = [k.sb(f"g_sp{i}", [128, 512], F32) for i in range(2)]
    bsp = [k.sb(f"g_bsp{i}", [128, 512], F32) for i in range(2)]
    eb = [k.sb(f"g_eb{i}", [128, 512], F32) for i in range(2)]
    ebi = [k.sb(f"g_ebi{i}", [128, 512], F32) for i in range(2)]
    qs = [k.sb(f"g_qs{i}", [128, 512], BF16) for i in range(2)]
    ks = [k.sb(f"g_ks{i}", [128, 512], BF16) for i in range(2)]
    kh = [k.sb(f"g_kh{i}", [128, 128], BF16) for i in range(2)]
    khT = [k.sb(f"g_khT{i}", [128, 128], BF16) for i in range(2)]
    vt = [k.sb(f"g_vt{i}", [128, 256], BF16) for i in range(3)]
    ogt = [k.sb(f"g_ogt{i}", [128, 256], F32) for i in range(3)]
    sil = [k.sb(f"g_sil{i}", [128, 256], F32) for i in range(2)]
    At = [k.sb(f"g_At{i}", [128, 128], BF16) for i in range(3)]
    t1 = [k.sb(f"g_t1{i}", [128, 128], F32) for i in range(2)]
    obuf = [k.sb(f"g_ob{i}", [128, 256], BF16) for i in range(2)]
    junk = k.sb("g_junk", [128, 128], BF16)
    ss = [k.sb(f"g_ss{i}", [128, 2], F32) for i in range(4)]
    pz = k.ps("g_pz", [128, 512], F32)
    pA = [k.ps(f"g_pA{i}", [128, 512], F32) for i in range(2)]
    po = [k.ps(f"g_po{i}", [128, 512], F32) for i in range(2)]
    pkv = k.ps("g_pkv", [128, 512], F32)
    pT = k.ps("g_pT", [128, 1024], BF16)
    ci = 0
    hi = 0
    for tb in range(T // 512):
        tsl = slice(tb * 512, (tb + 1) * 512)
        lr = lrT[tb % 2]
        k.dma(lr[:], cx.G_lr[0:16, tsl])
        for hp in range(2):
            b = (tb * 2 + hp) % 2
            k.dma(qT[b][:], cx.G_qk[hp * 128:(hp + 1) * 128, tsl])
            k.dma(kT[b][:], cx.G_qk[256 + hp * 128:256 + (hp + 1) * 128, tsl])
            k.mm(pz[:], a_up[:, hp * 128:(hp + 1) * 128], lr[:])
            k.act(e1[b][:], pz[:], AF.Exp, bias=nab[:, hp:hp + 1], scale=-1.0)
            k.act(sp[b][:], e1[b][:], AF.Ln, bias=1.0)
            o_, m_, s_ = bsp[b].t[:], rmask.t[:], sp[b].t[:]
            k.generic("dve", lambda e, o_=o_, m_=m_, s_=s_: e.tensor_tensor_scan(o_, m_, s_, 0.0, ALU.mult, ALU.add), [rmask, sp[b]], [bsp[b]])
            k.act(eb[b][:], bsp[b][:], AF.Exp, scale=-1.0 / 16)
            k.act(ebi[b][:], bsp[b][:], AF.Exp, scale=1.0 / 16)
            k.stt(qs[b][:], qT[b][:], 0.125, eb[b][:], ALU.mult, ALU.mult)
            k.tt(ks[b][:], kT[b][:], ebi[b][:], ALU.mult, eng="pool")
            for c in range(4):
                t0 = tb * 512 + c * 128
                cs = slice(c * 128, (c + 1) * 128)
                v = vt[ci % 3]
                og = ogt[ci % 3]
                k.dma(v[:], cx.G_v[t0:t0 + 128, hp * 256:(hp + 1) * 256])
                k.dma(og[:], cx.G_og[t0:t0 + 128, hp * 256:(hp + 1) * 256])
                sl_ = sil[ci % 2]
                k.act(sl_[:], og[:], AF.Silu)
                khc = kh[ci % 2]
                k.ts(khc[:], ks[b][:, cs], eb[b][:, c * 128 + 127:c * 128 + 128], None, ALU.mult)
                k.tr(pT[:, 0:128], khc[:], cx.ident[:])
                kht = khT[ci % 2]
                k.copy(kht[:], pT[:, 0:128], eng="act")
                ob = obuf[ci % 2]
                def gla_head(hh, hi_):
                    hb = hh * 64
                    a_ps = pA[hi_ % 2]
                    o_ps = po[hi_ % 2]
                    at = At[hi_ % 3]
                    s2 = ss[hi_ % 4]
                    tt1 = t1[hi_ % 2]
                    k.mm(a_ps[:, 0:128], ks[b][hb:hb + 64, cs], qs[b][hb:hb + 64, cs])
                    k.tt(at[:], a_ps[:, 0:128], tri[:], ALU.mult)
                    yield
                    k.mm(o_ps[:, 0:128], at[:], v[:, hh * 128:(hh + 1) * 128], start=True, stop=False)
                    k.mm(o_ps[:, 0:128], qs[b][hb:hb + 64, cs], Sbf[hp][hb:hb + 64, :], start=False, stop=True)
                    k.memset(s2[:], 0.0, eng="pool")
                    k.act(junk[:], o_ps[:, 0:128], AF.Square, accum=s2[:, 0:1])
                    yield
                    k.act(s2[:, 1:2], s2[:, 0:1], AF.Sqrt, bias=EPS, scale=1.0 / 128)
                    k.recip(s2[:, 1:2], s2[:, 1:2])
                    yield
                    k.stt(tt1[:], o_ps[:, 0:128], s2[:, 1:2], gbc[:], ALU.mult, ALU.mult)
                    k.tt(ob[:, hh * 128:(hh + 1) * 128], tt1[:], sl_[:, hh * 128:(hh + 1) * 128], ALU.mult, eng="pool")

                gens = [gla_head(0, hi), gla_head(1, hi + 1)]
                hi += 2
                while gens:
                    for g_ in list(gens):
                        try:
                            next(g_)
                        except StopIteration:
                            gens.remove(g_)
                k.dma(cx.O_mix[t0:t0 + 128, hp * 256:(hp + 1) * 256], ob[:], q="pool")
                k.mm(pkv[:, 0:256], kht[:], v[:])
                ebc = eb[b][:, c * 128 + 127:c * 128 + 128]
                k.stt(S[hp][0:64, :], S[hp][0:64, :], ebc[0:64, :], pkv[0:64, 0:128], ALU.mult, ALU.add)
                k.stt(S[hp][64:128, :], S[hp][64:128, :], ebc[64:128, :], pkv[64:128, 128:256], ALU.mult, ALU.add)
                k.copy(Sbf[hp][:], S[hp][:], eng="act")
                ci += 1
    k.pop()


def phase_wout(k, cx, x_in, x_out, w_dram, g_bc, src, src_mode, name):
    k.push()
    wb = load_weight_bf16(k, _rows(w_dram), D, name + "_wb")
    oT = [k.sb(f"{name}_oT{i}", [128, NKC, 512], BF16) for i in range(2)]
    ot = [k.sb(f"{name}_ot{i}", [128, D], BF16) for i in range(2)]
    xt = [k.sb(f"{name}_x{i}", [128, D], F32) for i in range(2)]
    tmp = [k.sb(f"{name}_tmp{i}", [128, D], F32) for i in range(2)]
    xo = [k.sb(f"{name}_xo{i}", [128, D], F32) for i in range(2)]
    pT = [k.ps(f"{name}_pT{i}", [128, D], BF16) for i in range(2)]
    pm = [k.ps(f"{name}_pm{i}", [128, 512], F32) for i in range(6)]
    ti = 0
    pi = 0
    for tb in range(T // 512):
        o = oT[tb % 2]
        if src_mode == "fm":
            k.dma(o[:], src[:, tb * 512:(tb + 1) * 512].rearrange("(kc p) t -> p kc t", p=128))
        else:
            for j in range(4):
                t0 = tb * 512 + j * 128
                a = ot[(tb * 4 + j) % 2]
                p = pT[(tb * 4 + j) % 2]
                k.dma(a[:], src[t0:t0 + 128, :])
                for kc in range(NKC):
                    k.tr(p[:, kc * 128:(kc + 1) * 128], a[:, kc * 128:(kc + 1) * 128], cx.ident[:])
                k.copy(o[:, 0:4, j * 128:(j + 1) * 128], p[:, 0:512].rearrange("p (k t) -> p k t", k=4), eng="act")
                k.copy(o[:, 4:8, j * 128:(j + 1) * 128], p[:, 512:1024].rearrange("p (k t) -> p k t", k=4), eng="dve")
        for j in range(4):
            t0 = tb * 512 + j * 128
            x = xt[ti % 2]
            tm_ = tmp[ti % 2]
            xo_ = xo[ti % 2]
            ti += 1
            k.dma(x[:], x_in[t0:t0 + 128, :])
            for half in range(2):
                p = pm[pi % 6]
                pi += 1
                for kc in range(NKC):
                    k.mm(p[:], o[:, kc, j * 128:(j + 1) * 128], wb[:, kc, half * 512:(half + 1) * 512], start=(kc == 0), stop=(kc == NKC - 1))
                k.tt(tm_[:, half * 512:(half + 1) * 512], p[:], g_bc[:, half * 512:(half + 1) * 512], ALU.mult)
            k.tt(xo_[:], tm_[:], x[:], ALU.add, eng="pool")
            k.dma(x_out[t0:t0 + 128, :], xo_[:], q="pool")
    k.pop()


def phase_ffn_up(k, cx, layer, x_in, bc):
    inp = cx.inp
    TB = 512
    NF = FF // 128
    k.push()
    wup = load_weight_bf16(k, _rows(inp["ffn_w_up"][layer]), 2 * FF, "f_wup")
    cw = k.sb("f_cw", [128, NF, 4], F32)
    k.dma(cw[:], inp["ffn_conv_col"][layer])
    nf = NormFront(k, cx, bc["gsc2"], bc["sh2"])
    xnT = [k.sb(f"f_xnT{i}", [128, NKC, TB], BF16) for i in range(2)]
    halo = k.sb("f_halo", [128, NF, 2], F32)
    k.memset(halo[:], 0.0)
    ub = [k.sb(f"f_ub{i}", [128, TB + 2], F32) for i in range(3)]
    cv = [k.sb(f"f_cv{i}", [128, TB], F32) for i in range(3)]
    ge = [k.sb(f"f_ge{i}", [128, TB], F32) for i in range(3)]
    hb = [k.sb(f"f_hb{i}", [128, TB], BF16) for i in range(3)]
    pu = [k.ps(f"f_pu{i}", [128, 512], F32) for i in range(3)]
    pv = [k.ps(f"f_pv{i}", [128, 512], F32) for i in range(3)]
    ui = 0
    for tb in range(T // TB):
        xn = xnT[tb % 2]
        for j in range(TB // 128):
            t0 = tb * TB + j * 128
            nf.tile(x_in[t0:t0 + 128, :], xn[:, :, j * 128:(j + 1) * 128])
        pend = None

        def tail(st):
            fc, u, c_, g_, h_, p_v = st
            k.ts(c_[:], u[:, 2:TB + 2], cw[:, fc, 2:3], cw[:, fc, 3:4], ALU.mult, ALU.add, eng="pool")
            k.stt(c_[:], u[:, 1:TB + 1], cw[:, fc, 1:2], c_[:], ALU.mult, ALU.add)
            k.stt(c_[:], u[:, 0:TB], cw[:, fc, 0:1], c_[:], ALU.mult, ALU.add)
            k.act(g_[:], c_[:], AF.Gelu)
            k.tt(h_[:], g_[:], p_v[:, 0:TB], ALU.mult)
            k.dma(cx.H_fm[fc * 128:(fc + 1) * 128, tb * TB:(tb + 1) * TB], h_[:], q="pool")

        for fc in range(NF):
            p_u = pu[ui % 3]
            p_v = pv[ui % 3]
            u = ub[ui % 3]
            c_ = cv[ui % 3]
            g_ = ge[ui % 3]
            h_ = hb[ui % 3]
            ui += 1
            for kc in range(NKC):
                k.mm(p_u[:, 0:TB], wup[:, kc, fc * 128:(fc + 1) * 128], xn[:, kc, :], start=(kc == 0), stop=(kc == NKC - 1))
            for kc in range(NKC):
                k.mm(p_v[:, 0:TB], wup[:, kc, FF + fc * 128:FF + (fc + 1) * 128], xn[:, kc, :], start=(kc == 0), stop=(kc == NKC - 1))
            k.copy(u[:, 0:2], halo[:, fc, :], eng="pool")
            k.copy(u[:, 2:TB + 2], p_u[:, 0:TB], eng="act")
            k.copy(halo[:, fc, :], u[:, TB:TB + 2], eng="pool")
            if pend is not None:
                tail(pend)
            pend = (fc, u, c_, g_, h_, p_v)
        tail(pend)
    k.pop()


def phase_ffn_down(k, cx, layer, x_in, x_out, bc, final=False):
    inp = cx.inp
    TB = 512
    NF = FF // 128
    k.push()
    wdn = load_weight_bf16(k, _rows(inp["ffn_w_down"][layer]), D, "f_wdn")
    hT = [k.sb(f"f_hT{i}", [128, NF, TB], BF16) for i in range(2)]
    pd = [k.ps(f"f_pd{i}", [128, 512], F32) for i in range(8)]
    xt = [k.sb(f"f_x{i}", [128, D], F32) for i in range(2)]
    tmp = [k.sb(f"f_tmp{i}", [128, D], F32) for i in range(2)]
    if final:
        fg = k.sb("f_fg", [128, D], F32)
        k.dma(fg[:], inp["final_norm_g_bc"][:])
        fss = [k.sb(f"f_fss{i}", [128, 2], F32) for i in range(2)]
        fjunk = k.sb("f_fjunk", [128, D], BF16)
    ti = 0
    di = 0
    for tb in range(T // TB):
        h = hT[tb % 2]
        k.dma(h[:], cx.H_fm[:, tb * TB:(tb + 1) * TB].rearrange("(fc p) t -> p fc t", p=128))
        for j in range(TB // 128):
            t0 = tb * TB + j * 128
            x = xt[ti % 2]
            tm_ = tmp[ti % 2]
            k.dma(x[:], x_in[t0:t0 + 128, :])
            for half in range(2):
                p = pd[di % 8]
                di += 1
                for fc in range(NF):
                    k.mm(p[:], h[:, fc, j * 128:(j + 1) * 128], wdn[:, fc, half * 512:(half + 1) * 512], start=(fc == 0), stop=(fc == NF - 1))
                k.tt(tm_[:, half * 512:(half + 1) * 512], p[:], bc["g2"][:, half * 512:(half + 1) * 512], ALU.mult)
            k.tt(x[:], tm_[:], x[:], ALU.add, eng="pool")
            if not final:
                k.dma(x_out[t0:t0 + 128, :], x[:], q="pool")
            else:
                s2 = fss[ti % 2]
                k.memset(s2[:], 0.0, eng="pool")
                k.act(fjunk[:], x[:], AF.Square, accum=s2[:, 0:1])
                k.act(s2[:, 1:2], s2[:, 0:1], AF.Sqrt, bias=EPS, scale=1.0 / D)
                k.recip(s2[:, 1:2], s2[:, 1:2])
                k.stt(tm_[:], x[:], s2[:, 1:2], fg[:], ALU.mult, ALU.mult)
                k.dma(x_out[t0:t0 + 128, :], tm_[:], q="pool")
            ti += 1
    k.pop()


def phase_rwkv(k, cx):
    inp = cx.inp
    LG = 0.6065306597126334
    GN_EPS = 64e-5
    k.push()

    def const(name, shape, dtype=F32, src=None):
        t = k.sb("rc_" + name, shape, F32)
        k.dma(t[:], inp[src or ("rw_" + name)][:])
        if dtype == F32:
            return t
        tb_ = k.sb("rcb_" + name, shape, dtype)
        k.copy(tb_[:], t[:])
        return tb_

    mu = const("mu_col", [128, 10])
    omu = k.sb("rc_omu", [128, 10], F32)
    k.ts(omu[:], mu[:], -1.0, 1.0, ALU.mult, ALU.add)
    mu_k = const("mu_k_col", [128, 4])
    omu_k = k.sb("rc_omu_k", [128, 4], F32)
    k.ts(omu_k[:], mu_k[:], -1.0, 1.0, ALU.mult, ALU.add)
    mu_al = const("mu_al_col", [64, 1])
    omu_al = k.sb("rc_omu_al", [64, 1], F32)
    k.ts(omu_al[:], mu_al[:], -1.0, 1.0, ALU.mult, ALU.add)
    muv = const("muv_bc", [128, 512])
    omuv = k.sb("rc_omuv", [128, 512], F32)
    k.ts(omuv[:], muv[:], -1.0, 1.0, ALU.mult, ALU.add)
    w0 = const("w0_col", [128, 4])
    a0 = const("a0_col", [128, 4])
    kkc = const("k_k_col", [128, 4])
    kac = const("k_a_col", [128, 4])
    okac = k.sb("rc_okac", [128, 4], F32)
    k.ts(okac[:], kac[:], -1.0, 1.0, ALU.mult, ALU.add)
    w2 = const("w2", [64, 512])
    a2 = const("a2", [64, 512])
    g2b = const("g2", [128, 512], BF16)
    rkcol = const("rkcol", [128, 4, 2])
    gnw = const("gn_w_bc", [128, 512])
    gnb = const("gn_b_bc", [128, 512])
    bones = const("blockones", [128, 128], src="c_blockones")
    negblk = const("negblock", [128, 128], src="c_negblock")
    scan64 = const("scan64", [128, 512], src="c_scan64")
    MK1 = const("mk1", [128, 256], src="c_mk1")
    MK2 = const("mk2", [128, 256], src="c_mk2")
    NMT = const("nmt", [128, 128], src="c_nmt")
    identf = cx.identf
    ident = cx.ident

    def f32t(name, shape=(128, 512)):
        return k.sb("r_" + name, list(shape), F32)

    rraw, kraw = f32t("rraw", (128, 513)), f32t("kraw", (128, 513))
    wlraw, alraw, glraw = f32t("wlraw", (64, 513)), f32t("alraw", (64, 513)), f32t("glraw", (128, 513))
    wls, als, gls, twl = f32t("wls", (64, 512)), f32t("als", (64, 512)), f32t("gls", (128, 512)), f32t("twl", (64, 512))
    sgls = [k.sb(f"r_sgl{i}", [128, 512], BF16) for i in range(2)]
    rs, ks, sg, csg, dd = f32t("rs"), f32t("ks"), f32t("sg"), f32t("csg"), f32t("dd")
    E1s = [f32t("E1a"), f32t("E1b")]
    E2, E3, aa = f32t("E2"), f32t("E3"), f32t("aa")
    kk, sq, rn, kkn, t1, kmod, beta, rk = f32t("kk"), f32t("sq"), f32t("rn"), f32t("kkn"), f32t("t1"), f32t("kmod"), f32t("beta"), f32t("rk")
    ARs = [k.sb(f"r_AR{i}", [128, 4, 2, 128], BF16) for i in range(2)]
    kbTs = [k.sb(f"r_kbT{i}", [128, 512], BF16) for i in range(2)]
    bbTs = [k.sb(f"r_bbT{i}", [128, 512], BF16) for i in range(2)]
    KGs = [k.sb(f"r_KG{i}", [128, 512], BF16) for i in range(2)]
    BGs = [k.sb(f"r_BG{i}", [128, 512], BF16) for i in range(2)]
    vraw = [f32t(f"vraw{i}") for i in range(2)]
    vprev = [f32t(f"vprev{i}") for i in range(2)]
    vs = [k.sb(f"r_vs{i}", [128, 4, 512], F32) for i in range(2)]
    vb = [k.sb(f"r_vb{i}", [128, 4, 512], BF16) for i in range(2)]
    RZ = [k.sb(f"r_RZ{i}", [128, 4, 2, 128], BF16) for i in range(2)]
    for t_ in RZ:
        k.memset(t_[:], 0.0)
    Y0s = [k.sb(f"r_Y0s{i}", [128, 4, 2, 64], F32) for i in range(2)]
    Gs = [k.sb(f"r_Gs{i}", [128, 8, 64], F32) for i in range(2)]
    MT = [k.sb(f"r_MT{i}", [128, 8, 128], BF16) for i in range(2)]
    Hst = [k.sb(f"r_Hst{i}", [128, 8, 64], BF16) for i in range(2)]
    Hcur = [k.sb(f"r_Hcur{i}", [128, 64], BF16) for i in range(4)]
    for t_ in Hcur:
        k.memset(t_[:], 0.0)
    bsc = [k.sb(f"r_bsc{i}", [128, 4, 2], F32) for i in range(2)]
    tokT = [k.sb(f"r_tokT{i}", [128, 2, 128], BF16) for i in range(2)]
    AZ = [[k.sb(f"r_AZ{i}{h}", [128, 128], BF16) for h in range(2)] for i in range(2)]
    C1 = [k.sb(f"r_C1{h}", [128, 256], BF16) for h in range(2)]
    C2 = [k.sb(f"r_C2{h}", [128, 256], BF16) for h in range(2)]
    Ln = [[k.sb(f"r_Ln{h}{i}", [128, 128], BF16) for i in range(2)] for h in range(2)]
    LTb = [[k.sb(f"r_LT{h}{i}", [128, 128], BF16) for i in range(2)] for h in range(2)]
    QT = [[k.sb(f"r_QT{h}{i}", [128, 128], BF16) for i in range(2)] for h in range(2)]
    WUp = [k.sb(f"r_WU{i}", [128, 2, 2, 64], BF16) for i in range(2)]
    tmpM = k.sb("r_tmpM", [128, 128], F32)
    yt = [k.sb(f"r_yt{i}", [128, 2, 64], F32) for i in range(2)]
    ysq = k.sb("r_ysq", [128, 2, 64], F32)
    st = [k.sb(f"r_st{i}", [128, 8], F32) for i in range(2)]
    yn = [k.sb(f"r_yn{i}", [128, 2, 64], F32) for i in range(2)]
    ob = [k.sb(f"r_ob{i}", [128, 128], BF16) for i in range(2)]
    B = [k.ps(f"r_B{i}", [128, 512], F32) for i in range(7)]
    BT = k.ps("r_BT", [128, 1024], BF16)
    oi = 0
    lim_tb, lim_hp, lim_stage = getattr(cx, "rw_limit", (T // 512, 4, 99))

    def shared_gen(tb):
        sgl = sgls[tb % 2]
        t00 = tb * 512
        def load_halo(dst, row0, nrows):
            if tb == 0:
                k.memset(dst[0:nrows, 0:1], 0.0)
                k.dma(dst[0:nrows, 1:513], cx.R_fm[row0:row0 + nrows, 0:512])
            else:
                k.dma(dst[0:nrows, :], cx.R_fm[row0:row0 + nrows, t00 - 1:t00 + 512])

        def shift(dst, raw, n, mcol):
            k.ts(dst[0:n, :], raw[0:n, 1:513], omu[0:n, mcol:mcol + 1], None, ALU.mult, eng="pool")
            k.stt(dst[0:n, :], raw[0:n, 0:512], mu[0:n, mcol:mcol + 1], dst[0:n, :], ALU.mult, ALU.add)

        load_halo(wlraw, 512, 64)
        load_halo(alraw, 1088, 64)
        load_halo(glraw, 1152, 128)
        k.ts(wls[:], wlraw[:, 1:513], omu[0:64, 4:5], None, ALU.mult, eng="pool")
        k.stt(wls[:], wlraw[:, 0:512], mu[0:64, 4:5], wls[:], ALU.mult, ALU.add)
        k.ts(als[:], alraw[:, 1:513], omu_al[:, 0:1], None, ALU.mult, eng="pool")
        k.stt(als[:], alraw[:, 0:512], mu_al[:, 0:1], als[:], ALU.mult, ALU.add)
        shift(gls, glraw, 128, 9)
        k.act(twl[:], wls[:], AF.Tanh)
        yield
        k.act(sgl[:], gls[:], AF.Sigmoid)
        yield
        vsb, vbb = vs[tb % 2], vb[tb % 2]
        for j in range(4):
            t0 = t00 + j * 128
            vr, vp = vraw[j % 2], vprev[j % 2]
            k.dma(vr[:], cx.R_v[t0:t0 + 128, :])
            if t0 == 0:
                k.memset(vp[0:1, :], 0.0)
                k.dma(vp[1:128, :], cx.R_v[0:127, :])
            else:
                k.dma(vp[:], cx.R_v[t0 - 1:t0 + 127, :])
            k.tt(vsb[:, j, :], vr[:], omuv[:], ALU.mult, eng="pool")
            k.tt(vp[:], vp[:], muv[:], ALU.mult, eng="pool")
            k.tt(vsb[:, j, :], vsb[:, j, :], vp[:], ALU.add, eng="pool")
            k.copy(vbb[:, j, :], vsb[:, j, :], eng="act")
            yield

    def pre_gen(tb, hp):
        t00 = tb * 512

        def load_halo(dst, row0, nrows):
            if tb == 0:
                k.memset(dst[0:nrows, 0:1], 0.0)
                k.dma(dst[0:nrows, 1:513], cx.R_fm[row0:row0 + nrows, 0:512])
            else:
                k.dma(dst[0:nrows, :], cx.R_fm[row0:row0 + nrows, t00 - 1:t00 + 512])
        w = (tb * 4 + hp) % 2
        rz, y0s, gs, mt, hst, bs = RZ[w], Y0s[w], Gs[w], MT[w], Hst[w], bsc[w]
        AR, kbT, bbT, KG, BG, E1 = ARs[w], kbTs[w], bbTs[w], KGs[w], BGs[w], E1s[w]
        sgl = sgls[tb % 2]
        vsb, vbb = vs[tb % 2], vb[tb % 2]
        load_halo(rraw, hp * 128, 128)
        load_halo(kraw, 576 + hp * 128, 128)
        k.ts(rs[:], rraw[:, 1:513], omu[:, hp:hp + 1], None, ALU.mult)
        k.stt(rs[:], rraw[:, 0:512], mu[:, hp:hp + 1], rs[:], ALU.mult, ALU.add)
        k.ts(ks[:], kraw[:, 1:513], omu_k[:, hp:hp + 1], None, ALU.mult)
        k.stt(ks[:], kraw[:, 0:512], mu_k[:, hp:hp + 1], ks[:], ALU.mult, ALU.add)
        yield
        k.mm(B[0][:], w2[:, hp * 128:(hp + 1) * 128], twl[:])
        k.act(sg[:], B[0][:], AF.Sigmoid, bias=w0[:, hp:hp + 1])
        o_, m_, s_ = csg.t[:], scan64.t[:], sg.t[:]
        k.generic("dve", lambda e, o_=o_, m_=m_, s_=s_: e.tensor_tensor_scan(o_, m_, s_, 0.0, ALU.mult, ALU.add), [scan64, sg], [csg])
        k.act(E1[:], csg[:], AF.Exp, scale=-LG)
        k.act(E2[:], csg[:], AF.Exp, scale=LG)
        yield
        k.tt(dd[:], csg[:], sg[:], ALU.subtract, eng="pool")
        k.act(E3[:], dd[:], AF.Exp, scale=-LG)
        k.mm(B[0][:], a2[:, hp * 128:(hp + 1) * 128], als[:])
        k.act(aa[:], B[0][:], AF.Sigmoid, bias=a0[:, hp:hp + 1])
        yield
        k.ts(kk[:], ks[:], kkc[:, hp:hp + 1], None, ALU.mult)
        k.tt(sq[:], kk[:], kk[:], ALU.mult, eng="pool")
        k.mm(B[0][:], bones[:], sq[:])
        k.ts(rn[:], B[0][:], 1e-24, None, ALU.max)
        yield
        k.act(rn[:], rn[:], AF.Ln)
        k.act(rn[:], rn[:], AF.Exp, scale=-0.5)
        k.tt(kkn[:], kk[:], rn[:], ALU.mult)
        k.ts(t1[:], aa[:], kac[:, hp:hp + 1], okac[:, hp:hp + 1], ALU.mult, ALU.add, eng="pool")
        yield
        k.tt(kmod[:], ks[:], t1[:], ALU.mult, eng="pool")
        k.tt(beta[:], aa[:], kkn[:], ALU.mult, eng="pool")
        k.tt(AR[:, :, 0, :], kkn[:].rearrange("p (j t) -> p j t", j=4), E3[:].rearrange("p (j t) -> p j t", j=4), ALU.mult)
        k.tt(AR[:, :, 1, :], rs[:].rearrange("p (j t) -> p j t", j=4), E1[:].rearrange("p (j t) -> p j t", j=4), ALU.mult)
        yield
        k.tt(kbT[:], kmod[:], E2[:], ALU.mult)
        k.tt(bbT[:], beta[:], E2[:], ALU.mult, eng="pool")
        for c in range(8):
            csl = slice(c * 64, (c + 1) * 64)
            gcol = E1[:, c * 64 + 63:c * 64 + 64]
            k.ts(KG[:, csl], kbT[:, csl], gcol, None, ALU.mult, eng="pool")
            k.ts(BG[:, csl], bbT[:, csl], gcol, None, ALU.mult, eng="pool")
        k.tt(rk[:], rs[:], kmod[:], ALU.mult, eng="pool")
        for j in range(4):
            jsl = slice(j * 128, (j + 1) * 128)
            k.mm(B[0][:, j * 2:j * 2 + 2], rk[:, jsl], rkcol[:, hp, :])
        k.copy(bs[:], B[0][:, 0:8].rearrange("p (j h) -> p j h", j=4), eng="act")
        yield
        yield

    def main_gen(tb, hp):
        nonlocal oi
        t00 = tb * 512
        w = (tb * 4 + hp) % 2
        rz, y0s, gs, mt, hst, bs = RZ[w], Y0s[w], Gs[w], MT[w], Hst[w], bsc[w]
        AR, kbT, bbT, KG, BG, E1 = ARs[w], kbTs[w], bbTs[w], KGs[w], BGs[w], E1s[w]
        sgl = sgls[tb % 2]
        vsb, vbb = vs[tb % 2], vb[tb % 2]
        for j in range(4):
            jsl = slice(j * 128, (j + 1) * 128)
            tk = tokT[j % 2]
            az = AZ[j % 2]
            wu = WUp[j % 2]
            k.tr(BT[:, 0:128], AR[:, j, 0, :], ident[:])
            k.tr(BT[:, 128:256], KG[:, jsl], ident[:])
            k.tr(BT[:, 256:384], BG[:, jsl], ident[:])
            k.copy(az[0][:, 0:64], BT[:, 0:64], eng="act")
            k.copy(az[1][:, 0:64], BT[:, 64:128], eng="act")
            k.copy(tk[:], BT[:, 128:384].rearrange("p (a b) -> p a b", a=2), eng="act")
            def head_stream(h):
                hb = h * 64
                bk = B[1 + h]
                bi = B[3 + h]
                vh = vbb[:, j, hp * 128 + h * 64:hp * 128 + (h + 1) * 64]
                k.mm(bk[:, 0:256], kbT[hb:hb + 64, jsl], AR[hb:hb + 64, j, :, :].rearrange("p a t -> p (a t)"))
                k.tt(C1[h][:], bk[:, 0:256], MK1[:], ALU.mult)
                k.mm(bk[:, 256:512], bbT[hb:hb + 64, jsl], AR[hb:hb + 64, j, :, :].rearrange("p a t -> p (a t)"))
                k.tt(C2[h][:], bk[:, 256:512], MK2[:], ALU.mult)
                k.mm(bi[:, 0:128], AR[hb:hb + 64, j, 0, :], bbT[hb:hb + 64, jsl])
                k.tt(Ln[h][0][:], bi[:, 0:128], NMT[:], ALU.mult)
                yield
                if lim_stage < 3:
                    return
                lt = C2[h][:, 0:128]
                qt = QT[h][0]
                k.tt(qt[:], lt, ident[:], ALU.add, eng="pool")
                lp = Ln[h][0][:]
                lpt = lt
                for i in range(5):
                    lp2 = Ln[h][(i + 1) % 2]
                    k.mm(bi[:, 128:256], lpt, lp)
                    if i < 4:
                        lpt2 = LTb[h][i % 2]
                        k.mm(bi[:, 256:384], lp, lpt)
                    k.copy(lp2[:], bi[:, 128:256], eng="act")
                    if i < 4:
                        k.copy(lpt2[:], bi[:, 256:384], eng="act")
                    yield
                    qn = QT[h][(i + 1) % 2]
                    k.mm(bi[:, 384:512], lp2[:], qt[:])
                    k.tt(qn[:], bi[:, 384:512], qt[:], ALU.add)
                    yield
                    qt = qn
                    lp = lp2[:]
                    if i < 4:
                        lpt = lpt2[:]
                if lim_stage < 4:
                    return
                k.mm(bk[:, 0:64], C1[h][:, 0:128], vh)
                k.copy(az[h][:, 64:128], bk[:, 0:64], eng="act")
                yield
                k.mm(bk[:, 64:192], qt[:], az[h][:])
                k.copy(wu[:, 0, h, :], bk[:, 64:128], eng="act")
                k.ts(wu[:, 1, h, :], bk[:, 128:192], -1.0, None, ALU.mult)
                yield
                k.mm(bk[:, 192:256], C1[h][:, 128:256], vh, start=True, stop=False)
                k.mm(bk[:, 192:256], C2[h][:, 128:256], wu[:, 1, h, :], start=False, stop=True)
                k.copy(y0s[:, j, h, :], bk[:, 192:256], eng="act")

            gens = [head_stream(0), head_stream(1)]
            while gens:
                for g_ in list(gens):
                    try:
                        next(g_)
                    except StopIteration:
                        gens.remove(g_)
                yield
            if lim_stage < 5:
                continue
            bp = B[5]
            wa = wu[:, 0, :, :].rearrange("p h k -> p (h k)")
            k.mm(bp[:, 0:128], wa, C2[0][:, 128:256])
            k.mm(bp[:, 128:256], wa, C2[1][:, 128:256])
            for h in range(2):
                hb = h * 64
                for c in range(2):
                    cs = slice(c * 64, (c + 1) * 64)
                    k.tt(rz[hb:hb + 64, j, c, cs], AR[hb:hb + 64, j, 1, cs], bp[hb:hb + 64, h * 128 + c * 64:h * 128 + (c + 1) * 64], ALU.subtract)
            for c in range(2):
                cb = c * 64
                cc = j * 2 + c
                k.mm(bp[:, 256:384], tk[cb:cb + 64, 0, :], vbb[cb:cb + 64, j, hp * 128:(hp + 1) * 128], start=True, stop=False)
                k.mm(bp[:, 256:384], tk[cb:cb + 64, 1, :], wu[cb:cb + 64, 1, :, :].rearrange("p h k -> p (h k)"), start=False, stop=True)
                k.copy(gs[0:64, cc, :], bp[0:64, 256:320], eng="act")
                k.copy(gs[64:128, cc, :], bp[64:128, 320:384], eng="act")
                k.mm(bp[:, 384:512], wu[cb:cb + 64, 0, :, :].rearrange("p h k -> p (h k)"), tk[cb:cb + 64, 1, :])
                k.tt(tmpM[:], bp[:, 384:512], negblk[:], ALU.mult)
                k.stt(mt[:, cc, :], identf[:], E1[:, cc * 64 + 63:cc * 64 + 64], tmpM[:], ALU.mult, ALU.add)
        if lim_stage < 6:
            return
        bh = B[6]
        k.copy(hst[:, 0, :], Hcur[hp][:], eng="pool")
        for c in range(8):
            k.mm(bh[:, 0:64], mt[:, c, :], hst[:, c, :])
            if c < 7:
                k.tt(hst[:, c + 1, :], bh[:, 0:64], gs[:, c, :], ALU.add)
            else:
                k.tt(Hcur[hp][:], bh[:, 0:64], gs[:, c, :], ALU.add)
        for j in range(4 if lim_stage >= 7 else 0):
            t0 = t00 + j * 128
            jsl = slice(j * 128, (j + 1) * 128)
            y = yt[oi % 2]
            s_ = st[oi % 2]
            yn_ = yn[oi % 2]
            o_b = ob[oi % 2]
            oi += 1
            for h in range(2):
                hb = h * 64
                k.mm(bh[:, 64 + h * 64:128 + h * 64], rz[hb:hb + 64, j, 0, :], hst[hb:hb + 64, 2 * j, :], start=True, stop=False)
                k.mm(bh[:, 64 + h * 64:128 + h * 64], rz[hb:hb + 64, j, 1, :], hst[hb:hb + 64, 2 * j + 1, :], start=False, stop=True)
            k.tt(y[:], bh[:, 64:192].rearrange("p (h v) -> p h v", h=2), y0s[:, j, :, :], ALU.add)
            if lim_stage < 8:
                continue
            k.reduce(s_[:, 0:2], y[:], ALU.add)
            k.tt(ysq[:], y[:], y[:], ALU.mult, eng="pool")
            k.reduce(s_[:, 2:4], ysq[:], ALU.add)
            k.ts(s_[:, 0:2], s_[:, 0:2], 1.0 / 64, None, ALU.mult)
            k.tt(s_[:, 4:6], s_[:, 0:2], s_[:, 0:2], ALU.mult)
            k.stt(s_[:, 2:4], s_[:, 2:4], 1.0 / 64, s_[:, 4:6], ALU.mult, ALU.subtract)
            k.act(s_[:, 2:4], s_[:, 2:4], AF.Sqrt, bias=GN_EPS)
            k.recip(s_[:, 2:4], s_[:, 2:4])
            if lim_stage < 9:
                continue
            for h in range(2):
                k.ts(yn_[:, h, :], y[:, h, :], s_[:, h:h + 1], s_[:, 2 + h:3 + h], ALU.subtract, ALU.mult)
            ynf = yn_[:].rearrange("p h v -> p (h v)")
            k.tt(ynf, ynf, gnw[:, hp * 128:(hp + 1) * 128], ALU.mult, eng="pool")
            k.tt(ynf, ynf, gnb[:, hp * 128:(hp + 1) * 128], ALU.add, eng="pool")
            for h in range(2):
                k.stt(yn_[:, h, :], vsb[:, j, hp * 128 + h * 64:hp * 128 + (h + 1) * 64], bs[:, j, h:h + 1], yn_[:, h, :], ALU.mult, ALU.add)
            if lim_stage < 10:
                continue
            k.mm(bh[:, 256:384], sgl[:, jsl], g2b[:, hp * 128:(hp + 1) * 128])
            k.tt(o_b[:], ynf, bh[:, 256:384], ALU.mult)
            k.dma(cx.O_mix[t0:t0 + 128, 512 + hp * 128:512 + (hp + 1) * 128], o_b[:], q="pool")


    def run_rr(gens):
        gens = list(gens)
        while gens:
            for g_ in list(gens):
                try:
                    next(g_)
                except StopIteration:
                    gens.remove(g_)

    def chain_gen(*gs_):
        for g_ in gs_:
            yield from g_

    n_tb = min(T // 512, lim_tb)
    n_hp = min(4, lim_hp)
    units = [(tb, hp) for tb in range(n_tb) for hp in range(n_hp)]
    run_rr([chain_gen(shared_gen(0), pre_gen(0, 0))])
    for ui_, (tb, hp) in enumerate(units):
        gens = [main_gen(tb, hp)] if lim_stage >= 2 else []
        if ui_ + 1 < len(units):
            ntb, nhp = units[ui_ + 1]
            if ntb != tb:
                gens.append(chain_gen(shared_gen(ntb), pre_gen(ntb, nhp)))
            else:
                gens.append(pre_gen(ntb, nhp))
        run_rr(gens)
    k.pop()


NSA_SLOPES = [2.0 ** (-8.0 * (i + 1) / 16) for i in range(16)]


def phase_nsa(k, cx):
    inp = cx.inp
    k.push()

    def cload(name, shape, dtype=F32, parts=None):
        t = k.sb("nc_" + name, shape, F32)
        if parts is None:
            k.dma(t[:], inp[name][:])
        else:
            k.dma(t[parts[0]:parts[1]], inp[name][:])
        if dtype == F32:
            return t
        tb_ = k.sb("ncb_" + name, shape, dtype)
        if parts is None:
            k.copy(tb_[:], t[:])
        else:
            k.copy(tb_[parts[0]:parts[1]], t[parts[0]:parts[1]])
        return tb_

    ident = cx.ident
    diagD = cload("c_diagD", [128, 4, 512])
    winD = cload("c_winD", [128, 4, 512])
    kbias = cload("c_kbias", [128, 16 * 28])
    ov = cload("c_ov", [128, 2, 65], BF16)
    selg = cload("c_selg", [48, 48, 64], BF16)
    w2k = cload("nsa_w2k", [64, 64], BF16)
    w2v = cload("nsa_w2v", [64, 64], BF16)
    peT = cload("nsa_peT", [64, 2, 32], BF16)
    w1 = []
    for i, nm in enumerate(("nsa_w1k", "nsa_w1v")):
        wt = k.sb(f"n_w1_{i}", [64, 32, 64], BF16)
        k.push()
        st = k.sb("n_w1st", [64, 32, 64], F32)
        k.dma(st[:], inp[nm][:].rearrange("(l d) h -> d l h", d=64))
        k.copy(wt[:], st[:])
        k.pop()
        w1.append(wt)
    KA_sel = k.sb("n_KAs", [128, 32, 128], BF16)
    KA_win = k.sb("n_KAw", [128, 32, 128], BF16)
    k.push()
    st = k.sb("n_kaugst", [128, 32, 128], F32)
    k.dma(st[64:128], inp["c_kaug_sel"][:])
    k.copy(KA_sel[64:128], st[64:128])
    k.dma(st[64:128], inp["c_kaug_win"][:])
    k.copy(KA_win[64:128], st[64:128])
    k.pop()
    QA = [[k.sb(f"n_QA{i}{r}", [128, 512], BF16) for r in range(4)] for i in range(2)]
    VA_sel = k.sb("n_VAs", [128, 32, 128], BF16)
    VA_win = k.sb("n_VAw", [128, 32, 128], BF16)
    k.memset(VA_sel[:, :, 64:128], 1.0)
    k.memset(VA_win[:, :, 64:128], 1.0, eng="pool")
    VCA = k.sb("n_VCA", [128, 2, 128], BF16)
    k.memset(VCA[:], 0.0)
    k.memset(VCA[:, :, 64:128], 1.0)
    KC = k.sb("n_KC", [64, 256], BF16)
    XC = [k.sb(f"n_XC{i}", [64, T], BF16) for i in range(2)]
    h1 = [k.sb(f"n_h1{i}", [64, 256], BF16) for i in range(2)]
    cpe = k.sb("n_cpe", [64, 2], F32)
    Ec = k.sb("n_Ec", [128, 4, 2, 512], BF16)
    cD = [k.sb(f"n_cD{i}", [128, 512], F32) for i in range(4)]
    sc = [k.sb(f"n_sc{i}", [128, 512], F32) for i in range(3)]
    Pb = [k.sb(f"n_P{i}", [128, 512], BF16) for i in range(6)]
    gts = k.sb("n_gts", [48, 512], BF16)
    gtr = k.sb("n_gtr", [48, 512], F32)
    rden = [k.sb(f"n_rden{i}", [64, 512], F32) for i in range(2)]
    ff_ = [k.sb(f"n_f{i}", [64, 512], F32) for i in range(2)]
    acc = [k.sb(f"n_acc{i}", [64, 512], F32) for i in range(4)]
    tmpa = [k.sb(f"n_tmpa{i}", [64, 512], F32) for i in range(2)]
    ob = [k.sb(f"n_ob{i}", [64, 512], BF16) for i in range(2)]
    sval = [k.sb(f"n_sval{i}", [128, 64], F32) for i in range(2)]
    sadd = [k.sb(f"n_sadd{i}", [128, 64], F32) for i in range(2)]
    imp = [k.sb(f"n_imp{i}", [128, 64], F32) for i in range(2)]
    rec = [k.sb(f"n_rec{i}", [128, 1], F32) for i in range(4)]
    top8 = [k.sb(f"n_top8{i}", [128, 8], F32) for i in range(2)]
    msk = [k.sb(f"n_msk{i}", [128, 64], F32) for i in range(2)]
    MTk = [k.sb(f"n_MTk{i}", [128, 128], BF16) for i in range(2)]
    for t_ in MTk:
        k.memset(t_[:], 0.0)
    S = [k.ps(f"n_S{i}", [128, 512], F32) for i in range(4)]
    O = [k.ps(f"n_O{i}", [128, 512], F32) for i in range(2)]
    PG = k.ps("n_PG", [128, 512], F32)
    PI = PG
    PT = k.ps("n_PT", [128, 1024], BF16)

    qaug_st = k.sb("n_qaugst", [128, 512], F32)

    for i in range(2):
        for l in range(32):
            k.mm(PG[0:64, i:i + 1], w1[i][:, l, :], peT[:, i, l:l + 1], start=(l == 0), stop=(l == 31))
    k.copy(cpe[:], PG[0:64, 0:2])

    si = 0
    pi_ = 0
    oi = 0
    fi = 0
    for g in range(4):
        k.dma(KA_sel[0:64, :, :], cx.N_ks[g * 64:(g + 1) * 64, :].rearrange("d (kt s) -> d kt s", s=128))
        k.dma(KA_win[0:64, :, :], cx.N_kw[g * 64:(g + 1) * 64, :].rearrange("d (kt s) -> d kt s", s=128))
        k.dma(VA_sel[:, :, 0:64], cx.N_vs[:, g * 64:(g + 1) * 64].rearrange("(kt p) c -> p kt c", p=128))
        k.dma(VA_win[:, :, 0:64], cx.N_vw[:, g * 64:(g + 1) * 64].rearrange("(kt p) c -> p kt c", p=128))
        for r in range(4):
            h = g * 4 + r
            k.dma(qaug_st[64:128, :], inp["c_qaug"][h])
            for i in range(2):
                k.copy(QA[i][r][64:128, :], qaug_st[64:128, :])
        for i in range(2):
            k.dma(XC[i][:], cx.N_c[i * 256 + g * 64:i * 256 + (g + 1) * 64, :])
            for l in range(32):
                k.mm(PG[0:64, 0:255], w1[i][:, l, :], XC[i][:, l:l + 4065:16], start=(l == 0), stop=(l == 31))
            k.act(h1[i][:, 0:255], PG[0:64, 0:255], AF.Gelu, bias=cpe[:, i:i + 1])
        k.mm(PG[0:64, 256:511], w2k[:], h1[0][:, 0:255])
        k.copy(KC[:, 0:255], PG[0:64, 256:511])
        for nt, nn in ((0, 128), (1, 127)):
            k.mm(PI[0:nn, 0:64], h1[1][:, nt * 128:nt * 128 + nn], w2v[:])
            k.copy(VCA[0:nn, nt, 0:64], PI[0:nn, 0:64])
        for qb in range(T // 512):
            qsl = slice(qb * 512, (qb + 1) * 512)
            qa = QA[qb % 2]
            for r in range(4):
                h = g * 4 + r
                k.dma(qa[r][0:64, :], cx.N_q[h * 64:(h + 1) * 64, qsl])
            k.dma(gtr[:], cx.N_gt[:, qsl])
            k.act(gts[:], gtr[:], AF.Sigmoid)
            nts = ((0, 128),) if qb < 4 else ((0, 128), (1, 127))
            cds = {}
            for nt, nn in nts:
                cd = cD[(qb * 2 + nt) % 4]
                k.dma(cd[:], inp["c_cmpD"][(qb if nt == 0 else 8 + qb - 4)])
                cds[nt] = cd

            def finalize(o_ps, h, br, first):
                nonlocal fi
                rd, f_, tm_ = rden[fi % 2], ff_[fi % 2], tmpa[fi % 2]
                fi += 1
                if br == 0:
                    k.ts(rd[:], o_ps[64:128, :], 1e-30, None, ALU.max)
                    k.act(rd[:], rd[:], AF.Ln)
                else:
                    k.act(rd[:], o_ps[64:128, :], AF.Ln)
                k.act(rd[:], rd[:], AF.Exp, scale=-1.0)
                k.mm(PG[0:64, :], selg[:, h * 3 + br, :], gts[:])
                k.tt(f_[:], PG[0:64, :], rd[:], ALU.mult)
                a = acc[h % 4]
                if first:
                    k.tt(a[:], o_ps[0:64, :], f_[:], ALU.mult)
                else:
                    k.tt(tm_[:], o_ps[0:64, :], f_[:], ALU.mult)
                    k.tt(a[:], a[:], tm_[:], ALU.add, eng="pool")

            for r in range(4):
                h = g * 4 + r
                slope = NSA_SLOPES[h]
                o_ps = O[oi % 2]
                oi += 1
                for idx, (nt, nn) in enumerate(nts):
                    s_ps = S[si % 3]
                    s_sb = sc[si % 3]
                    si += 1
                    k.mm(s_ps[0:nn, :], KC[:, nt * 128:nt * 128 + nn], qa[r][0:64, :])
                    k.stt(s_sb[0:nn, :], cds[nt][0:nn, :], slope, s_ps[0:nn, :], ALU.mult, ALU.add)
                    k.act(Ec[0:nn, r, nt, :], s_sb[0:nn, :], AF.Exp)
                for idx, (nt, nn) in enumerate(nts):
                    k.mm(o_ps[:, :], VCA[0:nn, nt, :], Ec[0:nn, r, nt, :], start=(idx == 0), stop=(idx == len(nts) - 1))
                finalize(o_ps, h, 0, True)
            for qt in range(4):
                t0 = qb * 512 + qt * 128
                sv, sa = sval[qt % 2], sadd[qt % 2]
                im, t8, mk, mtk = imp[qt % 2], top8[qt % 2], msk[qt % 2], MTk[qt % 2]
                k.dma(sv[:], inp["c_selvalid"][t0:t0 + 128, :])
                k.dma(sa[:], inp["c_seladd"][t0:t0 + 128, :])
                for r in range(4):
                    rc = rec[r]
                    for idx, (nt, nn) in enumerate(nts):
                        k.mm(PI[:, r * 65:(r + 1) * 65], Ec[0:nn, r, nt, qt * 128:(qt + 1) * 128], ov[0:nn, nt, :], start=(idx == 0), stop=(idx == len(nts) - 1))
                    k.ts(rc[:], PI[:, r * 65 + 64:r * 65 + 65], 1e-30, None, ALU.max)
                    k.recip(rc[:], rc[:])
                    if r == 0:
                        k.ts(im[:], PI[:, 0:64], rc[:, 0:1], None, ALU.mult)
                    else:
                        k.stt(im[:], PI[:, r * 65:r * 65 + 64], rc[:, 0:1], im[:], ALU.mult, ALU.add)
                k.tt(im[:], im[:], sv[:], ALU.mult)
                k.tt(im[:], im[:], sa[:], ALU.add)
                i_, o_ = im.t[:], t8.t[:]
                k.generic("dve", lambda e, i_=i_, o_=o_: e.max(o_, i_), [im], [t8])
                k.ts(mk[:], im[:], t8[:, 7:8], None, ALU.is_ge)
                k.ts(mtk[:, 64:126], mk[:, 1:63], 30000.0, -30000.0, ALU.mult, ALU.add)
                k.tr(PT[:, 0:128], mtk[:], ident[:])
                for r in range(4):
                    k.copy(qa[r][64:126, qt * 128:(qt + 1) * 128], PT[64:126, 0:128], eng=("act" if r % 2 else "dve"))
            def stream(br, r, o_ps):
                nonlocal si, pi_
                KA, VA = (KA_sel, VA_sel) if br == 1 else (KA_win, VA_win)
                h = g * 4 + r
                slope = NSA_SLOPES[h]
                tiles = []
                if br == 1:
                    tiles += [("fast", kt, kt - 4 * qb) for kt in range(4 * qb)]
                elif qb > 0:
                    tiles += [("far", 4 * qb - 4 + j, j) for j in range(4)]
                tiles += [("diag", 4 * qb + j, j) for j in range(4)]
                pend = None
                for idx, (kind, kt, j) in enumerate(tiles):
                    s_ps = S[si % 4]
                    s_sb = sc[si % 3]
                    si += 1
                    p_ = Pb[pi_ % 6]
                    pi_ += 1
                    if kind == "diag":
                        c0, c1 = 128 * j, 512
                    elif kind == "far":
                        c0, c1 = 0, 128 * (j + 1)
                    else:
                        c0, c1 = 0, 512
                    k.mm(s_ps[:, c0:c1], KA[:, kt, :], qa[r][:, c0:c1])
                    if kind == "fast":
                        col = h * 28 + (j + 28)
                        k.act(p_[:], s_ps[:], AF.Exp, bias=kbias[:, col:col + 1])
                    else:
                        dt_ = diagD if kind == "diag" else winD
                        k.stt(s_sb[:, c0:c1], dt_[:, j, c0:c1], slope, s_ps[:, c0:c1], ALU.mult, ALU.add)
                        k.act(p_[:, c0:c1], s_sb[:, c0:c1], AF.Exp)
                    yield
                    if pend is not None:
                        pp, pkt, pidx, pc0, pc1 = pend
                        k.mm(o_ps[:, pc0:pc1], VA[:, pkt, :], pp[:, pc0:pc1], start=(pidx == 0), stop=False)
                    pend = (p_, kt, idx, c0, c1)
                pp, pkt, pidx, pc0, pc1 = pend
                k.mm(o_ps[:, pc0:pc1], VA[:, pkt, :], pp[:, pc0:pc1], start=(pidx == 0), stop=True)
                yield
                finalize(o_ps, h, br, False)

            for r in range(4):
                h = g * 4 + r
                gens = [stream(1, r, O[0]), stream(2, r, O[1])]
                while gens:
                    for g_ in list(gens):
                        try:
                            next(g_)
                        except StopIteration:
                            gens.remove(g_)
                o_b = ob[h % 2]
                k.copy(o_b[:], acc[h % 4][:], eng="act")
                k.dma(cx.O_fm[h * 64:(h + 1) * 64, qsl], o_b[:], q="pool")
    k.pop()


INPUT_SHAPES = {
    "x": [T, D], "c_col": [128, NKC], "ada_w": [2, D, 6 * D], "ada_b": [2, 6 * D],
    "norm1_g": [2, D], "norm2_g": [2, D], "final_norm_g_bc": [128, D],
    "ffn_w_up": [2, D, 2 * FF], "ffn_conv_col": [2, 128, FF // 128, 4], "ffn_w_down": [2, FF, D],
    "ev_w_in": [1, D, EV_COLS], "ev_w_out": [1, D, D], "od_w_in": [1, D, OD_COLS], "od_w_out": [1, D, D],
    "gla_a_up": [16, 256], "gla_a_b_col": [128, 2], "gla_norm_g_bc": [128, 128],
    "c_ident": [128, 128], "c_scan128": [128, 512], "c_tri_incl": [128, 128],
    "rw_mu_col": [128, 10], "rw_mu_k_col": [128, 4], "rw_mu_al_col": [64, 1], "rw_muv_bc": [128, 512],
    "rw_w0_col": [128, 4], "rw_a0_col": [128, 4], "rw_k_k_col": [128, 4], "rw_k_a_col": [128, 4],
    "rw_w2": [64, 512], "rw_a2": [64, 512], "rw_g2": [128, 512], "rw_rkcol": [128, 4, 2],
    "rw_gn_w_bc": [128, 512], "rw_gn_b_bc": [128, 512],
    "c_blockones": [128, 128], "c_negblock": [128, 128], "c_scan64": [128, 512],
    "c_mk1": [128, 256], "c_mk2": [128, 256], "c_nmt": [128, 128],
    "c_diagD": [128, 4, 512], "c_winD": [128, 4, 512], "c_kbias": [128, 16 * 28], "c_ov": [128, 2, 65],
    "c_selg": [48, 48, 64], "nsa_w2k": [64, 64], "nsa_w2v": [64, 64], "nsa_peT": [64, 2, 32],
    "nsa_w1k": [2048, 64], "nsa_w1v": [2048, 64], "c_kaug_sel": [64, 32, 128], "c_kaug_win": [64, 32, 128],
    "c_qaug": [16, 64, 512], "c_cmpD": [12, 128, 512], "c_selvalid": [T, 64], "c_seladd": [T, 64],
}


def build(stop=None, dbg=(), rw_limit=None, skip=()):
    nc = bass.Bass("TRN2", target_bir_lowering=False)
    k = K(nc)
    cx = Ctx()
    if rw_limit is not None:
        cx.rw_limit = rw_limit
    cx.inp = {}
    for name, shape in INPUT_SHAPES.items():
        cx.inp[name] = k.dram(name, shape, F32, kind="ExternalInput")

    def scratch(name, shape, dtype=F32):
        return k.dram(name, shape, dtype, kind=("ExternalOutput" if name in dbg else "Internal"))

    out = k.dram("out", [T, D], F32, kind="ExternalOutput")
    cx.G_qk = scratch("G_qk", [512, T])
    cx.G_lr = scratch("G_lr", [16, T])
    cx.G_v = scratch("G_v", [T, 512], BF16)
    cx.G_og = scratch("G_og", [T, 512])
    cx.R_fm = scratch("R_fm", [1280, T])
    cx.R_v = scratch("R_v", [T, 512])
    cx.O_mix = scratch("O_mix", [T, D], BF16)
    cx.O_fm = scratch("O_fm", [D, T], BF16)
    cx.H_fm = scratch("H_fm", [FF, T], BF16)
    cx.XA = scratch("XA", [T, D])
    cx.XB = scratch("XB", [T, D])
    cx.XC = scratch("XC", [T, D])
    cx.N_q = scratch("N_q", [D, T], BF16)
    cx.N_c = scratch("N_c", [512, T], BF16)
    cx.N_ks = scratch("N_ks", [256, T], BF16)
    cx.N_kw = scratch("N_kw", [256, T], BF16)
    cx.N_gt = scratch("N_gt", [48, T])
    cx.N_vs = scratch("N_vs", [T, 256], BF16)
    cx.N_vw = scratch("N_vw", [T, 256], BF16)
    x = cx.inp["x"]

    def done(stage):
        return stop is not None and stage == stop

    phase_consts(k, cx)
    k.push()
    bc = phase_mod(k, cx, 0)
    if not done("mod0") and "l0" not in skip:
        fm = [(0, 512, cx.G_qk, 0, F32, 1.0), (1536, 16, cx.G_lr, 0, F32, 1.0),
              (1552, 1088, cx.R_fm, 0, F32, 1.0), (1552 + 1600, 192, cx.R_fm, 1088, F32, 1.0)]
        tm = [(512, 512, cx.G_v, 0, BF16), (1024, 512, cx.G_og, 0, F32), (1552 + 1088, 512, cx.R_v, 0, F32)]
        phase_proj(k, cx, x, cx.inp["ev_w_in"][0], EV_COLS, bc["gsc1"], bc["sh1"], fm, tm, "pj0")
    stages = ["mod0", "proj0", "gla", "rwkv", "wout0", "ffn0u", "ffn0d", "proj1", "nsa", "wout1", "ffn1u", "ffn1d"]
    def upto(stage):
        return stop is None or stop not in stages or stages.index(stop) >= stages.index(stage)
    if "l0" in skip:
        stages_l0_off = True
    if upto("gla") and "gla" not in skip and "l0" not in skip:
        phase_gla(k, cx)
    if upto("rwkv") and "l0" not in skip:
        phase_rwkv(k, cx)
    if upto("wout0") and "l0" not in skip:
        phase_wout(k, cx, x, cx.XA, cx.inp["ev_w_out"][0], bc["g1"], cx.O_mix, "tm", "wo0")
    if upto("ffn0u") and "l0" not in skip:
        phase_ffn_up(k, cx, 0, cx.XA, bc)
    if upto("ffn0d") and "l0" not in skip:
        phase_ffn_down(k, cx, 0, cx.XA, cx.XB, bc)
    k.pop()
    if upto("proj1"):
        xin1 = cx.XB if "l0" not in skip else x
        k.push()
        bc = phase_mod(k, cx, 1)
        fm = [(0, 1024, cx.N_q, 0, BF16, 0.125), (1024, 512, cx.N_c, 0, BF16, 1.0), (1536, 256, cx.N_ks, 0, BF16, 1.0),
              (2048, 256, cx.N_kw, 0, BF16, 1.0), (2560, 48, cx.N_gt, 0, F32, 1.0)]
        tm = [(1792, 256, cx.N_vs, 0, BF16), (2304, 256, cx.N_vw, 0, BF16)]
        phase_proj(k, cx, xin1, cx.inp["od_w_in"][0], OD_COLS, bc["gsc1"], bc["sh1"], fm, tm, "pj1")
        if upto("nsa"):
            phase_nsa(k, cx)
        if upto("wout1"):
            phase_wout(k, cx, xin1, cx.XC, cx.inp["od_w_out"][0], bc["g1"], cx.O_fm, "fm", "wo1")
        if upto("ffn1u"):
            phase_ffn_up(k, cx, 1, cx.XC, bc)
        if upto("ffn1d"):
            phase_ffn_down(k, cx, 1, cx.XC, out, bc, final=True)
        k.pop()
    build.stats = {e: len(k.ops[e]) for e in ENGS}
    k.emit()
    return nc


def host_inputs(inputs, b):
    f = np.float32
    m = {}
    m["x"] = np.ascontiguousarray(inputs["x"][b], dtype=f)
    m["c_col"] = np.ascontiguousarray(inputs["c"][b].reshape(NKC, 128).T, dtype=f)
    for nm in ("ada_w", "ada_b", "norm1_g", "norm2_g", "ffn_w_up", "ffn_w_down", "ev_w_in", "ev_w_out", "od_w_in", "od_w_out"):
        m[nm] = np.ascontiguousarray(inputs[nm], dtype=f)
    m["final_norm_g_bc"] = np.ascontiguousarray(np.broadcast_to(inputs["final_norm_g"][None, :], (128, D)), dtype=f)
    cw = np.concatenate([inputs["ffn_conv_w"], inputs["ffn_conv_b"][:, None, :]], axis=1)
    m["ffn_conv_col"] = np.ascontiguousarray(cw.reshape(2, 4, FF // 128, 128).transpose(0, 3, 2, 1), dtype=f)
    m["gla_a_up"] = np.ascontiguousarray(inputs["gla_a_up"][0], dtype=f)
    m["gla_a_b_col"] = np.ascontiguousarray(inputs["gla_a_b"][0].reshape(2, 128).T, dtype=f)
    m["gla_norm_g_bc"] = np.ascontiguousarray(np.broadcast_to(inputs["gla_norm_g"][0][None, :], (128, 128)), dtype=f)
    m["c_ident"] = np.eye(128, dtype=f)
    sc = np.ones((128, 512), f)
    sc[:, 0::128] = 0.0
    m["c_scan128"] = sc
    i = np.arange(128)
    m["c_tri_incl"] = (i[:, None] <= i[None, :]).astype(f)
    smu = inputs["ev_shift_mu"][0]
    mu_fm = np.concatenate([smu[0:1088], smu[1600:1792]])
    m["rw_mu_col"] = np.ascontiguousarray(mu_fm.reshape(10, 128).T, dtype=f)
    m["rw_mu_k_col"] = np.ascontiguousarray(smu[576:1088].reshape(4, 128).T, dtype=f)
    m["rw_mu_al_col"] = np.ascontiguousarray(smu[1600:1664].reshape(64, 1), dtype=f)
    m["rw_muv_bc"] = np.ascontiguousarray(np.broadcast_to(smu[1088:1600][None, :], (128, 512)), dtype=f)
    for nm in ("w0", "a0", "k_k", "k_a"):
        m[f"rw_{nm}_col"] = np.ascontiguousarray(inputs[f"rw_{nm}"][0].reshape(4, 128).T, dtype=f)
    m["rw_w2"] = np.ascontiguousarray(inputs["rw_w2"][0], dtype=f)
    m["rw_a2"] = np.ascontiguousarray(inputs["rw_a2"][0], dtype=f)
    m["rw_g2"] = np.ascontiguousarray(inputs["rw_g2"][0], dtype=f)
    rk = inputs["rw_r_k"][0]
    rkcol = np.zeros((128, 4, 2), f)
    for hp in range(4):
        rkcol[0:64, hp, 0] = rk[2 * hp]
        rkcol[64:128, hp, 1] = rk[2 * hp + 1]
    m["rw_rkcol"] = rkcol
    m["rw_gn_w_bc"] = np.ascontiguousarray(np.broadcast_to(inputs["rw_gn_w"][0][None, :], (128, 512)), dtype=f)
    m["rw_gn_b_bc"] = np.ascontiguousarray(np.broadcast_to(inputs["rw_gn_b"][0][None, :], (128, 512)), dtype=f)
    same = (i[:, None] // 64) == (i[None, :] // 64)
    m["c_blockones"] = same.astype(f)
    m["c_negblock"] = -same.astype(f)
    s64 = np.ones((128, 512), f)
    s64[:, 0::64] = 0.0
    m["c_scan64"] = s64
    mstrict = (same & (i[:, None] < i[None, :])).astype(f)
    mincl = (same & (i[:, None] <= i[None, :])).astype(f)
    m["c_mk1"] = np.concatenate([mstrict, mincl], axis=1)
    m["c_mk2"] = np.concatenate([-mstrict, mincl], axis=1)
    m["c_nmt"] = np.ascontiguousarray(-mstrict.T)
    s_ = np.arange(128)[:, None].astype(np.float64)
    q_ = np.arange(512)[None, :].astype(np.float64)
    NEG = -1.0e6
    dd = np.zeros((128, 4, 512), f)
    wd = np.zeros((128, 4, 512), f)
    for j in range(4):
        dd[:, j, :] = np.where(128 * j + s_ <= q_, 128 * j + s_ - 511.0, NEG)
        wd[:, j, :] = np.where(128 * j + s_ > q_, 128 * j - 512.0 + s_ - 511.0, NEG)
    m["c_diagD"] = dd
    m["c_winD"] = wd
    slopes = np.array(NSA_SLOPES, np.float64)
    kb = np.zeros((128, 16 * 28), f)
    for h in range(16):
        for jj in range(28):
            kb[:, h * 28 + jj] = (slopes[h] * (np.arange(128) + 128.0 * (jj - 28) - 511.0)).astype(f)
    m["c_kbias"] = kb
    n_ = np.arange(256)
    cstart = n_ * 16
    cend = cstart + 31
    sstart = np.arange(64) * 64
    ovl = ((cstart[:, None] <= sstart[None, :] + 63) & (cend[:, None] >= sstart[None, :])).astype(f)
    ovl[255] = 0.0
    ov = np.zeros((128, 2, 65), f)
    ov[:, 0, :64] = ovl[:128]
    ov[:, 1, :64] = ovl[128:]
    ov[:, :, 64] = 1.0
    ov[127, 1, :] = 0.0
    m["c_ov"] = ov
    sg = np.zeros((48, 48, 64), f)
    for i_ in range(48):
        sg[i_, i_, :] = 1.0
    m["c_selg"] = sg
    m["nsa_w2k"] = np.ascontiguousarray(inputs["cmp_w2_k"][0], dtype=f)
    m["nsa_w2v"] = np.ascontiguousarray(inputs["cmp_w2_v"][0], dtype=f)
    m["nsa_peT"] = np.ascontiguousarray(np.stack([inputs["cmp_pe_k"][0].T, inputs["cmp_pe_v"][0].T], axis=1), dtype=f)
    m["nsa_w1k"] = np.ascontiguousarray(inputs["cmp_w1_k"][0], dtype=f)
    m["nsa_w1v"] = np.ascontiguousarray(inputs["cmp_w1_v"][0], dtype=f)
    ka_s = np.zeros((64, 32, 128), f)
    ka_w = np.zeros((64, 32, 128), f)
    for kt in range(32):
        for half in range(2):
            b_ = 2 * kt + half
            if 1 <= b_ <= 62:
                ka_s[b_ - 1, kt, half * 64:(half + 1) * 64] = 1.0
    ka_s[62:64] = 1.0
    ka_w[62:64] = 1.0
    m["c_kaug_sel"] = ka_s
    m["c_kaug_win"] = ka_w
    import ml_dtypes
    qa = np.zeros((16, 64, 512), f)
    for h in range(16):
        rv = slopes[h] * (511.0 - np.arange(512))
        hi = rv.astype(f).astype(ml_dtypes.bfloat16).astype(np.float64)
        lo = (rv - hi).astype(f).astype(ml_dtypes.bfloat16).astype(np.float64)
        qa[h, 62] = hi
        qa[h, 63] = lo
    m["c_qaug"] = qa
    cD = np.zeros((12, 128, 512), f)
    for qb in range(8):
        for nt in range(2):
            if nt == 1 and qb < 4:
                continue
            e_n = 16.0 * (128 * nt + np.arange(128)[:, None]) + 31.0
            t_q = 512.0 * qb + np.arange(512)[None, :]
            cD[qb if nt == 0 else 8 + qb - 4] = np.where(t_q >= e_n, -(t_q - e_n), NEG)
    m["c_cmpD"] = cD
    t_ = np.arange(T)
    ahead = (t_ // 64)[:, None] - np.arange(64)[None, :]
    valid = ahead >= 0
    forced = (np.arange(64)[None, :] == 0) | (valid & (ahead < 2))
    m["c_selvalid"] = valid.astype(f)
    m["c_seladd"] = np.where(valid, np.where(forced, 100.0, 0.0), -100.0).astype(f)
    return m


_CACHE = {}


def kernel(**inputs):
    inputs = {k_: np.asarray(v) for k_, v in inputs.items()}
    if "nc" not in _CACHE:
        _CACHE["nc"] = build()
    nc = _CACHE["nc"]
    B = inputs["x"].shape[0]
    maps = [host_inputs(inputs, b) for b in range(B)]
    zero = dict(maps[0])
    zero["x"] = np.zeros_like(maps[0]["x"])
    slots = [0, 1, 4, 5][:B]
    in_maps = [zero] * 8
    for b, c_ in enumerate(slots):
        in_maps[c_] = maps[b]
    res = run_bass_kernel_spmd(nc, in_maps, core_ids=list(range(8)))
    out = np.stack([np.asarray(res.results[c_]["out"], dtype=np.float32) for c_ in slots], axis=0)
    return out
```
